# Optimizing a Trainium2 kernel written in Bass

```python
import jax, jax.numpy as jnp
from jax import lax
import numpy as np

D_MODEL = 1024
BATCH = 4
SEQ = 4096
DEPTH = 2

CTX_LEN = 256
GRID_W = 64
N_MIXERS = 2
EPS = 1e-6
N_MOD = 6
HG_EXPAND = 128
HG_HEADS = D_MODEL // HG_EXPAND
HG_DV = D_MODEL // HG_HEADS
HG_CHUNK = 64
N_HG_PROJ = 5
POOL_WINDOWS = (2, 4, 8, 16)
POOL_GROUPS = len(POOL_WINDOWS)
POOL_GDIM = D_MODEL // POOL_GROUPS
N_EXPERTS = 16
N_EXPERT_GROUPS = 4
EXPERTS_PER_GROUP = N_EXPERTS // N_EXPERT_GROUPS
TOP_K = 2
D_EXPERT = 256

kernel_name = "hybrid_hgrn2_pool_moe_dit"


def rms_norm(x, g):
    xf = x.astype(jnp.float32)
    y = xf * lax.rsqrt(jnp.mean(xf * xf, axis=-1, keepdims=True) + EPS)
    return (y * g.astype(jnp.float32)).astype(x.dtype)


def modulate(x, g, shift, scale):
    return rms_norm(x, g) * (1.0 + scale) + shift


def _heads(t):
    b, l, _ = t.shape
    return t.reshape(b, l, HG_HEADS, -1).transpose(0, 2, 1, 3).astype(jnp.float32)


def _rev(t):
    return jnp.flip(t, axis=2)


def _lower_bound(logits, slot):
    p = jax.nn.softmax(logits.astype(jnp.float32), axis=0)
    return jnp.cumsum(p, axis=0)[slot].reshape(HG_HEADS, 1, HG_EXPAND)


def _gate(z, lb):
    log_f = jnp.logaddexp(jnp.log(lb), jnp.log1p(-lb) + jax.nn.log_sigmoid(z))
    k = (1.0 - lb) * jax.nn.sigmoid(-z)
    return log_f, k


def _gla_scan(q, k, v, log_f, s0):
    b, h, l, _ = q.shape
    dv = v.shape[-1]
    n = l // HG_CHUNK
    rs = lambda t: t.reshape(b, h, n, HG_CHUNK, t.shape[-1])
    q, k, v, log_f = rs(q), rs(k), rs(v), rs(log_f)
    cum = jnp.cumsum(log_f, axis=3)
    ref = cum[:, :, :, HG_CHUNK // 2 - 1:HG_CHUNK // 2, :]
    scores = jnp.einsum('bhnck,bhnsk->bhncs', q * jnp.exp(cum - ref), k * jnp.exp(ref - cum))
    tri = jnp.tril(jnp.ones((HG_CHUNK, HG_CHUNK), dtype=bool))
    scores = jnp.where(tri, scores, 0.0)
    intra = jnp.einsum('bhncs,bhnsv->bhncv', scores, v)
    last = cum[:, :, :, -1:, :]
    chunk_kv = jnp.einsum('bhnck,bhncv->nbhkv', k * jnp.exp(last - cum), v)
    chunk_decay = jnp.moveaxis(jnp.exp(last[:, :, :, 0, :]), 2, 0)

    def step(s, inp):
        dec, kv = inp
        return dec[..., None] * s + kv, s

    s_final, s_prev = lax.scan(step, s0, (chunk_decay, chunk_kv))
    inter = jnp.einsum('bhnck,nbhkv->bhncv', q * jnp.exp(cum), s_prev)
    return (intra + inter).reshape(b, h, l, dv), s_final


def _hgrn_inputs(h, w_in):
    q, v, zf, zb, g = jnp.split(h @ w_in, N_HG_PROJ, axis=-1)
    return jax.nn.silu(_heads(q)), _heads(v), _heads(zf), _heads(zb), g


def _hgrn_readout(o, g, gnorm, w_out):
    b, h, l, dv = o.shape
    o = o * lax.rsqrt(jnp.mean(o * o, axis=-1, keepdims=True) + EPS) * gnorm.astype(jnp.float32)
    o = o.transpose(0, 2, 1, 3).reshape(b, l, h * dv)
    y = (o * jax.nn.silu(g.astype(jnp.float32))).astype(g.dtype)
    return y @ w_out


def hgrn2_mixer(h_lat, h_ctx, w_in, lb_fwd, lb_bwd, gnorm, w_out, slot, need_ctx):
    lbf = _lower_bound(lb_fwd, slot)
    lbb = _lower_bound(lb_bwd, slot)
    qc, vc, zfc, zbc, gc = _hgrn_inputs(h_ctx, w_in)
    ql, vl, zfl, zbl, gl = _hgrn_inputs(h_lat, w_in)
    s0 = jnp.zeros((h_lat.shape[0], HG_HEADS, HG_EXPAND, HG_DV), jnp.float32)
    lf, kf = _gate(zfc, lbf)
    o_cf, s_f = _gla_scan(qc, kf, vc, lf, s0)
    lf, kf = _gate(zfl, lbf)
    o_lf, _ = _gla_scan(ql, kf, vl, lf, s_f)
    lbk, kb = _gate(zbc, lbb)
    o_cb, s_b = _gla_scan(_rev(qc), _rev(kb), _rev(vc), _rev(lbk), s0)
    lbk, kb = _gate(zbl, lbb)
    o_lb, _ = _gla_scan(_rev(ql), _rev(kb), _rev(vl), _rev(lbk), s_b)
    y_lat = _hgrn_readout(o_lf + _rev(o_lb), gl, gnorm, w_out)
    y_ctx = _hgrn_readout(o_cf + _rev(o_cb), gc, gnorm, w_out) if need_ctx else None
    return y_lat, y_ctx


def _box_sum(u, win, axis):
    n = u.shape[axis]
    cs = jnp.cumsum(u, axis=axis)
    pad = [(0, 0)] * u.ndim
    pad[axis] = (1, 0)
    cs = jnp.pad(cs, pad)
    t = jnp.arange(n)
    lo = jnp.clip(t - win // 2, 0, n)
    hi = jnp.clip(t - win // 2 + win, 0, n)
    return jnp.take(cs, hi, axis=axis) - jnp.take(cs, lo, axis=axis), hi - lo


def _grid_mean(u, win):
    b, l, d = u.shape
    rows = l // GRID_W
    g = u.reshape(b, rows, GRID_W, d)
    s, cnt_c = _box_sum(g, win, 2)
    s, cnt_r = _box_sum(s, win, 1)
    cnt = (cnt_r[:, None] * cnt_c[None, :]).astype(u.dtype)
    return (s / cnt[None, :, :, None]).reshape(b, l, d)


def _seq_mean(u, win):
    s, cnt = _box_sum(u, win, 1)
    return s / cnt.astype(u.dtype)[None, :, None]


def _pool_mix(h, mean_fn, w_in, w_grp, p_scale, w_out):
    u = (h @ w_in).astype(jnp.float32)
    parts = []
    for gi, win in enumerate(POOL_WINDOWS):
        ug = u[..., gi * POOL_GDIM:(gi + 1) * POOL_GDIM]
        parts.append((mean_fn(ug, win) - ug) @ w_grp[gi].astype(jnp.float32))
    y = jnp.concatenate(parts, axis=-1) * p_scale.astype(jnp.float32)
    return y.astype(h.dtype) @ w_out


def pool_mixer(h_lat, h_ctx, w_in, w_grp, p_scale, w_out, need_ctx):
    y_lat = _pool_mix(h_lat, _grid_mean, w_in, w_grp, p_scale, w_out)
    y_ctx = _pool_mix(h_ctx, _seq_mean, w_in, w_grp, p_scale, w_out) if need_ctx else None
    return y_lat, y_ctx


def moe_ffn(h, router_w, router_b, w_gate, w_up, w_down):
    n = h.shape[0]
    s = jax.nn.sigmoid((h @ router_w).astype(jnp.float32))
    sel = (s + router_b.astype(jnp.float32)).reshape(n, N_EXPERT_GROUPS, EXPERTS_PER_GROUP)
    top_v, top_i = lax.top_k(sel, TOP_K)
    best = jnp.argmax(jnp.sum(top_v, axis=-1), axis=-1)
    loc = jnp.take_along_axis(top_i, best[:, None, None], axis=1)[:, 0]
    ids = best[:, None] * EXPERTS_PER_GROUP + loc
    w = jnp.take_along_axis(s, ids, axis=1)
    w = w / jnp.sum(w, axis=-1, keepdims=True)
    combine = jnp.sum(jax.nn.one_hot(ids, N_EXPERTS, dtype=jnp.float32) * w[..., None], axis=1)
    y = jnp.zeros(h.shape, jnp.float32)
    for e in range(N_EXPERTS):
        a = jax.nn.silu(h @ w_gate[e]) * (h @ w_up[e])
        y = y + combine[:, e:e + 1] * (a @ w_down[e]).astype(jnp.float32)
    return y.astype(h.dtype)


def setup_inputs(seed: int = 0) -> dict:
    key = jax.random.key(seed)
    ks = jax.random.split(key, 24)
    n_a = (DEPTH + N_MIXERS - 1) // N_MIXERS
    n_b = DEPTH // N_MIXERS
    nrm = lambda k, shape, sc: jax.random.normal(k, shape, jnp.float32) * sc
    d = D_MODEL
    return {
        "x": nrm(ks[0], (BATCH, SEQ, d), 1.0),
        "c": nrm(ks[1], (BATCH, d), 1.0),
        "ctx": nrm(ks[2], (BATCH, CTX_LEN, d), 1.0),
        "c_ctx": nrm(ks[3], (d,), 1.0),
        "w_mod": nrm(ks[4], (DEPTH, d, N_MOD * d), 0.5 * d ** -0.5),
        "b_mod": nrm(ks[5], (DEPTH, N_MOD * d), 0.01),
        "norm1_g": 1.0 + nrm(ks[6], (DEPTH, d), 0.05),
        "norm2_g": 1.0 + nrm(ks[7], (DEPTH, d), 0.05),
        "hg_w_in": nrm(ks[8], (n_a, d, N_HG_PROJ * d), d ** -0.5),
        "hg_lb_fwd": nrm(ks[9], (n_a + 1, d), 0.5),
        "hg_lb_bwd": nrm(ks[10], (n_a + 1, d), 0.5),
        "hg_gnorm": 1.0 + nrm(ks[11], (n_a, HG_DV), 0.05),
        "hg_w_out": nrm(ks[12], (n_a, d, d), d ** -0.5),
        "pool_w_in": nrm(ks[13], (n_b, d, d), d ** -0.5),
        "pool_w_grp": nrm(ks[14], (n_b, POOL_GROUPS, POOL_GDIM, POOL_GDIM), POOL_GDIM ** -0.5),
        "pool_scale": 1.0 + nrm(ks[15], (n_b, d), 0.1),
        "pool_w_out": nrm(ks[16], (n_b, d, d), d ** -0.5),
        "router_w": nrm(ks[17], (d, N_EXPERTS), d ** -0.5),
        "router_b": nrm(ks[18], (N_EXPERTS,), 0.01),
        "moe_w_gate": nrm(ks[19], (DEPTH, N_EXPERTS, d, D_EXPERT), d ** -0.5),
        "moe_w_up": nrm(ks[20], (DEPTH, N_EXPERTS, d, D_EXPERT), d ** -0.5),
        "moe_w_down": nrm(ks[21], (DEPTH, N_EXPERTS, D_EXPERT, d), D_EXPERT ** -0.5),
        "final_g": 1.0 + nrm(ks[22], (d,), 0.05),
    }


def reference(x, c, ctx, c_ctx, w_mod, b_mod, norm1_g, norm2_g, hg_w_in, hg_lb_fwd, hg_lb_bwd, hg_gnorm,
              hg_w_out, pool_w_in, pool_w_grp, pool_scale, pool_w_out, router_w, router_b, moe_w_gate,
              moe_w_up, moe_w_down, final_g):
    x_lat, x_ctx = x, ctx
    d = x.shape[-1]
    for i in range(DEPTH):
        need_ctx = i < DEPTH - 1
        slot = i // N_MIXERS
        mod = jax.nn.silu(c) @ w_mod[i] + b_mod[i]
        sh1, sc1, g1, sh2, sc2, g2 = jnp.split(mod[:, None, :], N_MOD, axis=-1)
        mod_c = jax.nn.silu(c_ctx) @ w_mod[i] + b_mod[i]
        csh1, csc1, cg1, csh2, csc2, cg2 = jnp.split(mod_c, N_MOD, axis=-1)
        h_lat = modulate(x_lat, norm1_g[i], sh1, sc1)
        h_ctx = modulate(x_ctx, norm1_g[i], csh1, csc1)
        if i % N_MIXERS == 0:
            y_lat, y_ctx = hgrn2_mixer(h_lat, h_ctx, hg_w_in[slot], hg_lb_fwd, hg_lb_bwd, hg_gnorm[slot],
                                       hg_w_out[slot], slot, need_ctx)
        else:
            y_lat, y_ctx = pool_mixer(h_lat, h_ctx, pool_w_in[slot], pool_w_grp[slot], pool_scale[slot],
                                      pool_w_out[slot], need_ctx)
        x_lat = x_lat + g1 * y_lat
        h2_lat = modulate(x_lat, norm2_g[i], sh2, sc2).reshape(-1, d)
        n_lat = h2_lat.shape[0]
        if need_ctx:
            x_ctx = x_ctx + cg1 * y_ctx
            h2_ctx = modulate(x_ctx, norm2_g[i], csh2, csc2).reshape(-1, d)
            ff = moe_ffn(jnp.concatenate([h2_lat, h2_ctx], axis=0), router_w, router_b,
                         moe_w_gate[i], moe_w_up[i], moe_w_down[i])
            x_ctx = x_ctx + cg2 * ff[n_lat:].reshape(x_ctx.shape)
            ff_lat = ff[:n_lat]
        else:
            ff_lat = moe_ffn(h2_lat, router_w, router_b, moe_w_gate[i], moe_w_up[i], moe_w_down[i])
        x_lat = x_lat + g2 * ff_lat.reshape(x_lat.shape)
    return rms_norm(x_lat, final_g)
```

```python
import numpy as np
from contextlib import ExitStack
import concourse.bass as bass
import concourse.mybir as mybir
from concourse.bass_utils import run_bass_kernel_spmd

F32 = mybir.dt.float32
BF16 = mybir.dt.bfloat16
AF = mybir.ActivationFunctionType
ALU = mybir.AluOpType

D = 1024
L = 4096
CTX = 256
OWN = 2048
HALO = 512
EXT = OWN + HALO
TB = 256
NB_EXT = EXT // TB
NB_OWN = OWN // TB
EPS = 1e-6
NE = 16
POOL_WINDOWS = (2, 4, 8, 16)


class Res:
    __slots__ = ("name", "w", "r", "tw", "tr")

    def __init__(self, name=""):
        self.name = name
        self.w = None
        self.r = []
        self.tw = 0.0
        self.tr = 0.0


class _Eng:
    def __init__(self, trk, name, handle, sem):
        self.trk = trk
        self.name = name
        self.h = handle
        self.sem = sem
        self.count = 0
        self.seen = {}

    def wait(self, ev):
        if ev is None:
            return
        key, val = ev[0], ev[1]
        if self.seen.get(id(key), 0) >= val:
            return
        self.seen[id(key)] = val
        self.h.wait_ge(key, val)
        self.trk.nwaits += 1


class Tracker:
    def __init__(self, nc, engines):
        self.nc = nc
        self.E = {n: _Eng(self, n, h, s) for n, (h, s) in engines.items()}
        self.nwaits = 0
        self.nops = 0
        self.dma_sems = {}
        self.dma_rr = {}
        self.rec = None
        self.t_eng = {}
        self.strict = True

    def add_dma_sems(self, qname, sems):
        self.dma_sems[qname] = [[s, 0] for s in sems]
        self.dma_rr[qname] = 0

    def _deps(self, eng, reads, writes):
        for r in reads:
            if r.w is not None:
                eng.wait(r.w)
        for w in writes:
            if w.w is not None and (self.strict or w.w[0] is not eng.sem):
                eng.wait(w.w)
            for rd in w.r:
                if self.strict or rd[0] is not eng.sem:
                    eng.wait(rd)

    def _commit(self, ev, reads, writes):
        for r in reads:
            r.r.append(ev)
            if len(r.r) > 48:
                best = {}
                for k, v in r.r:
                    if id(k) not in best or best[id(k)][1] < v:
                        best[id(k)] = (k, v)
                r.r = list(best.values())
        for w in writes:
            w.w = ev
            w.r = []

    LAT = 0.15
    DEF_COST = {"act": 0.4, "dve": 0.4, "pool": 0.7, "pe": 0.12, "sp": 0.05}

    def _est(self, ename, reads, writes):
        t = self.t_eng.get(ename, 0.0)
        for r in reads:
            t = max(t, r.tw + self.LAT)
        for w in writes:
            t = max(t, w.tw + self.LAT, w.tr + self.LAT)
        return t

    def _advance(self, ename, reads, writes, cost, async_cost=None):
        st = self._est(ename, reads, writes)
        fin = st + cost
        self.t_eng[ename] = fin
        done = fin if async_cost is None else st + async_cost
        for r in reads:
            r.tr = max(r.tr, done)
        for w in writes:
            w.tw = done
            w.tr = 0.0

    def _cost_of(self, kind, args):
        if kind == "op":
            return args[4] if args[4] is not None else self.DEF_COST[args[0]]
        if kind == "group":
            return args[4] if args[4] is not None else self.DEF_COST["pe"] * len(args[1])
        return 0.05

    def capture(self, fn):
        prev = self.rec
        self.rec = []
        fn()
        out = self.rec
        self.rec = prev
        return out

    def play(self, *lists):
        lists = [l for l in lists if l]
        if _DBG.get("seq"):
            for l in lists:
                for kind, args in l:
                    getattr(self, kind)(*args)
            return
        pos = [0] * len(lists)
        while True:
            best, bi = None, -1
            for i, l in enumerate(lists):
                if pos[i] < len(l):
                    kind, args = l[pos[i]]
                    ename = args[0]
                    if kind == "dma":
                        st = self._est(ename, args[3], args[4])
                    else:
                        st = self._est(ename, args[2], args[3])
                    if best is None or st < best - 1e-9:
                        best, bi = st, i
            if bi < 0:
                break
            kind, args = lists[bi][pos[bi]]
            pos[bi] += 1
            getattr(self, kind)(*args)

    def op(self, ename, fn, reads=(), writes=(), cost=None):
        if self.rec is not None:
            self.rec.append(("op", (ename, fn, list(reads), list(writes), cost)))
            return
        self._advance(ename, reads, writes, cost if cost is not None else self.DEF_COST[ename])
        eng = self.E[ename]
        self._deps(eng, reads, writes)
        ins = fn(eng.h)
        self.nops += 1
        eng.count += 1
        ins.then_inc(eng.sem, 1)
        self._commit((eng.sem, eng.count), reads, writes)

    def group(self, ename, fns, reads=(), writes=(), cost=None):
        if self.rec is not None:
            self.rec.append(("group", (ename, fns, list(reads), list(writes), cost)))
            return
        self._advance(ename, reads, writes, cost if cost is not None else self.DEF_COST["pe"] * len(fns))
        eng = self.E[ename]
        self._deps(eng, reads, writes)
        ins = None
        for f in fns:
            ins = f(eng.h)
            self.nops += 1
        eng.count += 1
        ins.then_inc(eng.sem, 1)
        self._commit((eng.sem, eng.count), reads, writes)

    def dma(self, qname, out, in_, reads=(), writes=()):
        if self.rec is not None:
            self.rec.append(("dma", (qname, out, in_, list(reads), list(writes))))
            return None
        self._advance(qname, reads, writes, 0.05, async_cost=3.0)
        eng = self.E[qname]
        pool = self.dma_sems[qname]
        i = self.dma_rr[qname]
        self.dma_rr[qname] = (i + 1) % len(pool)
        slot = pool[i]
        sem = slot[0]
        if slot[1] > 0:
            eng.wait((sem, 16 * slot[1]))
        self._deps(eng, reads, writes)
        slot[1] += 1
        eng.h.dma_start(out=out, in_=in_).then_inc(sem, 16)
        self.nops += 1
        ev = (sem, 16 * slot[1])
        self._commit(ev, reads, writes)
        return ev

    def barrier(self):
        tmax = max(self.t_eng.values()) if self.t_eng else 0.0
        for k in self.t_eng:
            self.t_eng[k] = tmax
        evs = [(e.sem, e.count) for e in self.E.values() if e.count > 0]
        for pool in self.dma_sems.values():
            for s, uses in pool:
                if uses > 0:
                    evs.append((s, 16 * uses))
        for e in self.E.values():
            for ev in evs:
                if ev[0] is not e.sem:
                    e.wait(ev)


def _col(v):
    return np.ascontiguousarray(np.asarray(v, np.float32).reshape(-1, 128).T)


VEC = dict(c=0, cctx=8, bmod0=16, bmod1=64, n1g0=112, n2g0=120, n1g1=128, n2g1=136,
           lbF0=144, lbF1=152, lbB0=160, lbB1=168, pscale=176, fing=184, gnorm=192, rb=193)
NV = 256

CST = dict(ident=0, ones=128, scanmask=256, triF=512, triB=576)
NCST = 640


def _pool_plan():
    plan = []
    for w in POOL_WINDOWS:
        reach = w // 2
        dt = (reach + 1) // 2 + (0 if reach % 2 == 0 else 0)
        dt = (reach + 1) // 2 if reach > 1 else 1
        plan.append((-dt, dt, dt))
    return plan


def _pool_mats(half):
    mats = []
    index = {}
    plan = _pool_plan()
    cache = {}
    for wi, w in enumerate(POOL_WINDOWS):
        dmin, dmax, nsp = plan[wi]
        for ti in range(OWN // 128):
            special = ti < nsp
            for d in range(dmin, dmax + 1):
                sj = ti + d
                if sj < 0 or sj >= EXT // 128:
                    continue
                key = (wi, ti if special else -1, d)
                if key in cache:
                    index[(wi, ti, d)] = cache[key]
                    continue
                M = np.zeros((128, 128), np.float64)
                for tl in range(128):
                    f = ti * 128 + tl
                    t = f if half == 0 else L - 1 - f
                    r, c = divmod(t, 64)
                    r0, r1 = max(r - w // 2, 0), min(r - w // 2 + w, 64)
                    c0, c1 = max(c - w // 2, 0), min(c - w // 2 + w, 64)
                    inv = 1.0 / ((r1 - r0) * (c1 - c0))
                    for sl in range(128):
                        fs = sj * 128 + sl
                        ts = fs if half == 0 else L - 1 - fs
                        rs, cs = divmod(ts, 64)
                        if r0 <= rs < r1 and c0 <= cs < c1:
                            M[sl, tl] += inv
                        if ts == t:
                            M[sl, tl] -= 1.0
                cache[key] = len(mats)
                index[(wi, ti, d)] = len(mats)
                mats.append(M.astype(np.float32))
    return np.stack(mats), index


_POOL_CACHE = {}


def _pool_mats_cached(half):
    if half not in _POOL_CACHE:
        _POOL_CACHE[half] = _pool_mats(half)
    return _POOL_CACHE[half]


def _consts():
    c = np.zeros((128, NCST), np.float32)
    c[:, CST["ident"]:CST["ident"] + 128] = np.eye(128, dtype=np.float32)
    c[:, CST["ones"]:CST["ones"] + 128] = 1.0
    m = np.ones((128, TB), np.float32)
    m[:, ::64] = 0.0
    c[:, CST["scanmask"]:CST["scanmask"] + TB] = m
    s = np.arange(64)[:, None]
    cc = np.arange(64)[None, :]
    tf = (s <= cc).astype(np.float32)
    tb = (s >= cc).astype(np.float32)
    c[0:64, CST["triF"]:CST["triF"] + 64] = tf
    c[64:128, CST["triF"]:CST["triF"] + 64] = tf
    c[0:64, CST["triB"]:CST["triB"] + 64] = tb
    c[64:128, CST["triB"]:CST["triB"] + 64] = tb
    sel = np.zeros((16, NE, 128), np.float32)
    for e in range(NE):
        sel[e, e, :] = 1.0
    return c, sel.reshape(16, NE * 128)


def _shared_weights(inp):
    f = lambda a: np.asarray(a, np.float32)
    out = {}
    wm = f(inp["w_mod"])
    out["w_mod_t"] = np.ascontiguousarray(
        wm.reshape(2, 8, 128, 12, 512).transpose(0, 3, 2, 1, 4))
    wg = f(inp["moe_w_gate"]).reshape(2, NE, 8, 128, 256)
    wu = f(inp["moe_w_up"]).reshape(2, NE, 8, 128, 256)
    wgu = np.concatenate([wg, wu], axis=-1)
    out["wgu_t"] = np.ascontiguousarray(wgu.transpose(0, 1, 3, 2, 4))
    wd = f(inp["moe_w_down"]).reshape(2, NE, 2, 128, 1024)
    out["wd_t"] = np.ascontiguousarray(wd.transpose(0, 1, 3, 2, 4))
    out["w_out_t"] = np.ascontiguousarray(
        f(inp["hg_w_out"][0]).reshape(4, 2, 128, 1024).transpose(0, 2, 1, 3))
    out["pw_in_t"] = np.ascontiguousarray(f(inp["pool_w_in"][0]).reshape(8, 128, 1024).transpose(1, 0, 2))
    out["pw_out_t"] = np.ascontiguousarray(f(inp["pool_w_out"][0]).reshape(8, 128, 1024).transpose(1, 0, 2))
    out["pw_grp_t"] = np.ascontiguousarray(
        f(inp["pool_w_grp"][0]).reshape(4, 2, 128, 256).transpose(2, 0, 1, 3))
    out["router_t"] = np.ascontiguousarray(f(inp["router_w"]).reshape(8, 128, NE).transpose(1, 0, 2))
    cst, sel = _consts()
    out["cst"] = cst
    out["sel"] = sel
    return out


def _w_in_tiled(w_in, half):
    q, v, zf, zb, g = [w_in[:, i * D:(i + 1) * D] for i in range(5)]
    zF, zB = (zf, zb) if half == 0 else (zb, zf)
    groups = []
    for gi in range(4):
        cols = []
        for kind in (q, zF, zB, g, v):
            for hh in range(2):
                h = 2 * gi + hh
                cols.append(kind[:, h * 128:(h + 1) * 128])
        Wg = np.concatenate(cols, axis=1)
        groups.append(Wg.reshape(8, 128, 1280).transpose(1, 0, 2))
    return np.ascontiguousarray(np.stack(groups))


def _core_inputs(inp, shared, w_in_by_half, core):
    b, half = core // 2, core % 2
    f = lambda a: np.asarray(a, np.float32)
    x = f(inp["x"][b])
    ctx = f(inp["ctx"][b])
    if half == 1:
        x = x[::-1]
        ctx = ctx[::-1]
    m = dict(shared)
    m["xT"] = np.ascontiguousarray(x.T)
    m["ctxT"] = np.ascontiguousarray(ctx.T)
    vec = np.zeros((128, NV), np.float32)
    vec[:, VEC["c"]:VEC["c"] + 8] = _col(inp["c"][b])
    vec[:, VEC["cctx"]:VEC["cctx"] + 8] = _col(inp["c_ctx"])
    for l in range(2):
        vec[:, VEC[f"bmod{l}"]:VEC[f"bmod{l}"] + 48] = _col(inp["b_mod"][l])
        vec[:, VEC[f"n1g{l}"]:VEC[f"n1g{l}"] + 8] = _col(inp["norm1_g"][l])
        vec[:, VEC[f"n2g{l}"]:VEC[f"n2g{l}"] + 8] = _col(inp["norm2_g"][l])
    lbF, lbB = (inp["hg_lb_fwd"], inp["hg_lb_bwd"]) if half == 0 else (inp["hg_lb_bwd"], inp["hg_lb_fwd"])
    vec[:, VEC["lbF0"]:VEC["lbF0"] + 8] = _col(lbF[0])
    vec[:, VEC["lbF1"]:VEC["lbF1"] + 8] = _col(lbF[1])
    vec[:, VEC["lbB0"]:VEC["lbB0"] + 8] = _col(lbB[0])
    vec[:, VEC["lbB1"]:VEC["lbB1"] + 8] = _col(lbB[1])
    vec[:, VEC["pscale"]:VEC["pscale"] + 8] = _col(inp["pool_scale"][0])
    vec[:, VEC["fing"]:VEC["fing"] + 8] = _col(inp["final_g"])
    vec[:, VEC["gnorm"]] = f(inp["hg_gnorm"][0])
    vec[:, VEC["rb"]:VEC["rb"] + NE] = f(inp["router_b"])[None, :]
    m["vec"] = vec
    m["w_in_t"] = w_in_by_half[half]
    m["pmat"] = _pool_mats_cached(half)[0]
    return m


_DBG = {"stage": None}


class _Prog:
    def __init__(self, stage=None):
        self.stage = stage
        self.nc = bass.Bass("TRN2", target_bir_lowering=False)
        self.es = ExitStack()
        self.nsb = 0

    def sb(self, name, shape, dt=F32, es=None):
        return (es or self.es).enter_context(self.nc.sbuf_tensor("sb_" + name, list(shape), dt))

    def ps(self, name, shape, dt=F32, es=None):
        return (es or self.es).enter_context(self.nc.psum_tensor("pp_" + name, list(shape), dt))

    def sem(self, name):
        return self.es.enter_context(self.nc.semaphore(name))

    def dram_in(self, name, shape, dt=F32):
        return self.nc.dram_tensor(name, list(shape), dt, kind="ExternalInput").ap()

    def dram_out(self, name, shape, dt=F32):
        return self.nc.dram_tensor(name, list(shape), dt, kind="ExternalOutput").ap()


def build_program(stage=None, n_pmat=1):
    P = _Prog(stage)
    nc = P.nc
    with P.es:
        _build(P, n_pmat)
    return nc


def _build(P, n_pmat):
    nc = P.nc
    stage = P.stage
    xT_d = P.dram_in("xT", [D, L])
    ctxT_d = P.dram_in("ctxT", [D, CTX])
    vec_d = P.dram_in("vec", [128, NV])
    cst_d = P.dram_in("cst", [128, NCST])
    sel_d = P.dram_in("sel", [16, NE * 128])
    w_mod_d = P.dram_in("w_mod_t", [2, 12, 128, 8, 512])
    w_in_d = P.dram_in("w_in_t", [4, 128, 8, 1280])
    w_out_d = P.dram_in("w_out_t", [4, 128, 2, 1024])
    wgu_d = P.dram_in("wgu_t", [2, NE, 128, 8, 512])
    wd_d = P.dram_in("wd_t", [2, NE, 128, 2, 1024])
    pw_in_d = P.dram_in("pw_in_t", [128, 8, 1024])
    pw_out_d = P.dram_in("pw_out_t", [128, 8, 1024])
    pw_grp_d = P.dram_in("pw_grp_t", [128, 4, 2, 256])
    router_d = P.dram_in("router_t", [128, 8, NE])
    pmat_d = P.dram_in("pmat", [n_pmat, 128, 128])
    out_d = P.dram_out("outT", [D, OWN])
    dbg_d = P.dram_out("dbg", [D, EXT]) if stage else None

    engs = {"pe": (nc.tensor, P.sem("s_pe")), "act": (nc.scalar, P.sem("s_act")),
            "dve": (nc.vector, P.sem("s_dve")), "pool": (nc.gpsimd, P.sem("s_pool")),
            "sp": (nc.sync, P.sem("s_sp"))}
    T = Tracker(nc, engs)
    T.add_dma_sems("sp", [P.sem(f"dsp{i}") for i in range(8)])
    T.add_dma_sems("pool", [P.sem(f"dpl{i}") for i in range(8)])

    XT = P.sb("XT", [128, 8, EXT], F32)
    XTr = [[Res(f"XT{j}_{dc}") for dc in range(8)] for j in range(NB_EXT)]
    XTall = [r for row in XTr for r in row]
    vec = P.sb("vec", [128, NV], F32)
    Rvec = Res("vec")
    cst32 = P.sb("cst32", [128, NCST], F32)
    Rcst = Res("cst")
    ident = P.sb("ident", [128, 128], BF16)
    ones = P.sb("ones", [128, 128], BF16)
    Rid = Res("ident")
    modv = P.sb("modv", [128, 2, 48, 2], F32)
    Rmod = Res("modv")
    drv = P.sb("drv", [128, 192], F32)
    Rdrv = Res("drv")
    PSB = [P.ps(f"psb{i}", [128, 512], F32) for i in range(7)]
    RPS = [Res(f"ps{i}") for i in range(7)]
    PSH = P.ps("psh", [128, 1024], BF16)
    _rpsh = Res("psh")
    RPSH = [_rpsh, _rpsh]

    def vcol(name, n=8, off=0):
        a = VEC[name] + off
        return vec[:, a:a + n]

    T.dma("sp", vec[:], vec_d[:, :], writes=[Rvec])
    T.dma("sp", cst32[:], cst_d[:, :], writes=[Rcst])
    T.dma("pool", ident[:], cst_d[:, CST["ident"]:CST["ident"] + 128], writes=[Rid])
    T.dma("pool", ones[:], cst_d[:, CST["ones"]:CST["ones"] + 128], writes=[Rid])
    for dc in range(8):
        T.dma("sp", XT[:, dc, :], xT_d[dc * 128:(dc + 1) * 128, 0:EXT], writes=[XTr[j][dc] for j in range(NB_EXT)])

    scanmask = cst32[:, CST["scanmask"]:CST["scanmask"] + TB]
    triF = cst32[:, CST["triF"]:CST["triF"] + 64]
    triB = cst32[:, CST["triB"]:CST["triB"] + 64]

    cs16 = P.sb("cs16", [128, 8, 2], BF16)
    Rcs = Res("cs")
    mps = PSB[0][:, 0:96].rearrange("p (n c) -> p n c", c=2)

    def mod_piece(l, j, wmp, Rwmp):
        buf = wmp[j % 2]
        T.dma("pool", buf[:], w_mod_d[l, j], writes=[Rwmp[j % 2]])
        fns = []
        for cg in range(4):
            n = j * 4 + cg
            for dc in range(8):
                fns.append(lambda e, n=n, cg=cg, dc=dc: e.matmul(
                    mps[:, n, :], buf[:, dc, cg * 128:(cg + 1) * 128], cs16[:, dc, :],
                    start=(dc == 0), stop=(dc == 7)))
        T.group("pe", fns, reads=[Rwmp[j % 2], Rcs], writes=[RPS[0]])

    def mv(l, comp, which=0):
        return modv[:, l, comp * 8:(comp + 1) * 8, which]

    def mod_finish(l):
        bm = vcol(f"bmod{l}", 48)
        T.op("dve", lambda e: e.tensor_tensor(
            out=modv[:, l], in0=mps, in1=bm.unsqueeze(2).to_broadcast([128, 48, 2]), op=ALU.add),
            reads=[RPS[0], Rvec], writes=[Rmod])
        base = l * 64
        n1 = vcol(f"n1g{l}")
        n2 = vcol(f"n2g{l}")

        def gs(dst, sc, ng):
            T.op("dve", lambda e: e.scalar_tensor_tensor(out=dst, in0=sc, scalar=1.0, in1=ng,
                                                          op0=ALU.add, op1=ALU.mult),
                 reads=[Rmod, Rvec], writes=[Rdrv])
        gs(drv[:, base + 0:base + 8], mv(l, 1), n1)
        gs(drv[:, base + 24:base + 32], mv(l, 4), n2)
        gs(drv[:, base + 48:base + 56], mv(l, 1, 1), n1)
        for dst, comp, which in ((8, 0, 0), (16, 2, 0), (32, 3, 0), (40, 5, 0), (56, 0, 1)):
            T.op("dve", lambda e, dst=dst, comp=comp, which=which: e.tensor_copy(
                out=drv[:, base + dst:base + dst + 8], in_=mv(l, comp, which)),
                reads=[Rmod], writes=[Rdrv])

    with ExitStack() as es0:
        cs32 = P.sb("cs32", [128, 16], F32, es0)
        wmp = [P.sb(f"wmp{i}", [128, 8, 512], BF16, es0) for i in range(2)]
        Rwmp = [Res("wmp0"), Res("wmp1")]
        T.op("act", lambda e: e.activation(out=cs32[:], in_=vec[:, 0:16], func=AF.Sigmoid),
             reads=[Rvec], writes=[Rcs])
        T.op("dve", lambda e: e.tensor_tensor(out=cs32[:], in0=cs32[:], in1=vec[:, 0:16], op=ALU.mult),
             reads=[Rcs, Rvec], writes=[Rcs])
        T.op("dve", lambda e: e.tensor_copy(out=cs16[:, :, 0], in_=cs32[:, 0:8]), reads=[Rcs], writes=[Rcs])
        T.op("dve", lambda e: e.tensor_copy(out=cs16[:, :, 1], in_=cs32[:, 8:16]), reads=[Rcs], writes=[Rcs])
        for j in range(12):
            mod_piece(0, j, wmp, Rwmp)
        mod_finish(0)
        for nm, off in (("F", 128), ("B", 144)):
            l0 = vcol(f"lb{nm}0")
            l1 = vcol(f"lb{nm}1")
            T.op("dve", lambda e, off=off, l0=l0, l1=l1: e.tensor_tensor(
                out=drv[:, off + 8:off + 16], in0=l1, in1=l0, op=ALU.subtract), reads=[Rvec], writes=[Rdrv])
            T.op("act", lambda e, off=off: e.activation(out=drv[:, off:off + 8], in_=drv[:, off + 8:off + 16],
                                                        func=AF.Sigmoid), reads=[Rdrv], writes=[Rdrv])
            T.op("dve", lambda e, off=off: e.tensor_scalar(out=drv[:, off + 8:off + 16], in0=drv[:, off:off + 8],
                                                           scalar1=-1.0, scalar2=None, op0=ALU.mult),
                 reads=[Rdrv], writes=[Rdrv])
            xo = 160 if nm == "F" else 168
            T.op("act", lambda e, off=off, xo=xo: e.activation(out=drv[:, xo:xo + 8], in_=drv[:, off:off + 8], func=AF.Ln),
                 reads=[Rdrv], writes=[Rdrv])
            T.op("dve", lambda e, off=off, xo=xo: e.tensor_scalar(out=drv[:, xo + 16:xo + 24], in0=drv[:, off:off + 8],
                                                                  scalar1=-1.0, scalar2=1.0, op0=ALU.mult, op1=ALU.add),
                 reads=[Rdrv], writes=[Rdrv])
        T.barrier()

    def dcol(l, off, dc):
        a = l * 64 + off + dc
        return drv[:, a:a + 1]

    T32ALT = {}
    def modulate(es, src, Rsrc, n, gs_off, sh_off, l, dst16, Rdst, dst32=None, Rdst32=None, tmp=None):
        sq, Rsq, rstd, Rrstd, t32, Rt32, bank, Rbank = tmp
        fns = []
        for dc in range(8):
            if dc % 4 == 3:
                T.op("act", lambda e, dc=dc: e.activation(out=sq[:, dc, 0:n], in_=src(dc), func=AF.Square),
                     reads=[Rsrc[dc] if isinstance(Rsrc, list) else Rsrc], writes=[Rsq])
            else:
                T.op("pool", lambda e, dc=dc: e.tensor_tensor(out=sq[:, dc, 0:n], in0=src(dc), in1=src(dc), op=ALU.mult),
                     reads=[Rsrc[dc] if isinstance(Rsrc, list) else Rsrc], writes=[Rsq])
        for dc in range(8):
            fns.append(lambda e, dc=dc: e.matmul(bank[:, 0:n], ones[:], sq[:, dc, 0:n],
                                                  start=(dc == 0), stop=(dc == 7)))
        T.group("pe", fns, reads=[Rsq, Rid], writes=[Rbank])
        T.op("act", lambda e: e.activation(out=rstd[:, 0:n], in_=bank[:, 0:n], func=AF.Ln,
                                           scale=1.0 / D, bias=EPS), reads=[Rbank], writes=[Rrstd])
        T.op("act", lambda e: e.activation(out=rstd[:, 0:n], in_=rstd[:, 0:n], func=AF.Exp, scale=-0.5),
             reads=[Rrstd], writes=[Rrstd])
        alt = T32ALT.get(id(t32))
        for dc in range(8):
            tb, Rtb = (t32, Rt32) if (alt is None or dc % 2 == 0) else alt
            T.op("dve", lambda e, dc=dc, tb=tb: e.tensor_tensor(out=tb[:, 0:n], in0=src(dc), in1=rstd[:, 0:n],
                                                                 op=ALU.mult),
                 reads=[Rsrc[dc] if isinstance(Rsrc, list) else Rsrc, Rrstd], writes=[Rtb])
            if dst32 is not None:
                Rd32 = Rdst32[dc] if isinstance(Rdst32, list) else Rdst32
                T.op("act", lambda e, dc=dc, tb=tb: e.activation(
                    out=dst32(dc), in_=tb[:, 0:n], func=AF.Identity,
                    scale=dcol(l, gs_off, dc), bias=dcol(l, sh_off, dc)), reads=[Rtb, Rdrv], writes=[Rd32])
                T.op("pool", lambda e, dc=dc: e.tensor_copy(out=dst16(dc), in_=dst32(dc)),
                     reads=[Rd32], writes=[Rdst])
            else:
                T.op("act", lambda e, dc=dc, tb=tb: e.activation(
                    out=dst16(dc), in_=tb[:, 0:n], func=AF.Identity,
                    scale=dcol(l, gs_off, dc), bias=dcol(l, sh_off, dc)), reads=[Rtb, Rdrv], writes=[Rdst])

    with ExitStack() as es1:
        hown_d = nc.dram_tensor("hown", [NB_EXT, 128, 8 * TB], BF16, kind="Internal").ap()
        HTr = [Res(f"HT{j}") for j in range(NB_EXT)]
        sq = P.sb("sq", [128, 8, TB], BF16, es1)
        Rsq = Res("sq")
        rstd = P.sb("rstd", [128, TB], F32, es1)
        Rrstd = Res("rstd")
        t32 = P.sb("t32", [128, TB], F32, es1)
        Rt32 = Res("t32")
        T32ALT[id(t32)] = (P.sb("t32_b", [128, TB], F32, es1), Res("t32_b"))
        mtmp = (sq, Rsq, rstd, Rrstd, t32, Rt32, PSB[0], RPS[0])
        NSO = 1 + (L - EXT) // TB
        hso_d = nc.dram_tensor("hso", [NSO, 128, 8 * TB], BF16, kind="Internal").ap()
        Rhso = [Res(f"hso{i}") for i in range(NSO)]
        hp2 = [P.sb(f"hp{i}", [128, 8, TB], BF16, es1) for i in range(2)]
        Rhp2 = [Res("hp0"), Res("hp1")]
        with ExitStack() as esi:
            NSET = 3
            sets = []
            for k in range(NSET):
                hpk = hp2[k] if k < 2 else P.sb("hp_x", [128, 8, TB], BF16, esi)
                Rhpk = Rhp2[k] if k < 2 else Res("hp_x")
                if k == 0:
                    sqk, Rsqk, rstdk, Rrstdk, t32k, Rt32k = sq, Rsq, rstd, Rrstd, t32, Rt32
                else:
                    sqk = P.sb(f"sq_i{k}", [128, 8, TB], BF16, esi)
                    rstdk = P.sb(f"rstd_i{k}", [128, TB], F32, esi)
                    t32k = P.sb(f"t32_i{k}", [128, TB], F32, esi)
                    Rsqk, Rrstdk, Rt32k = Res(f"sq_i{k}"), Res(f"rstd_i{k}"), Res(f"t32_i{k}")
                    T32ALT[id(t32k)] = (P.sb(f"t32_i{k}b", [128, TB], F32, esi), Res(f"t32_i{k}b"))
                xstk = P.sb(f"xst{k}", [128, 8, 128], F32, esi)
                sets.append(dict(hp=hpk, Rhp=Rhpk, xst=xstk, Rxst=Res(f"xst{k}"),
                                 tmp=(sqk, Rsqk, rstdk, Rrstdk, t32k, Rt32k, PSB[k], RPS[k])))

            def stage_task(k, kind, idx):
                st = sets[k]
                hp, Rhp = st["hp"], st["Rhp"]
                if kind == "so":
                    if idx == 0:
                        src_d, t0, gs_off, sh_off = ctxT_d, 0, 48, 56
                    else:
                        src_d, t0, gs_off, sh_off = xT_d, EXT + (idx - 1) * TB, 0, 8
                    xst, Rxst = st["xst"], st["Rxst"]
                    for hf in range(TB // 128):
                        for dc in range(8):
                            T.dma("sp", xst[:, dc, :],
                                  src_d[dc * 128:(dc + 1) * 128, t0 + hf * 128:t0 + (hf + 1) * 128], writes=[Rxst])
                        modulate(esi, lambda dc: xst[:, dc, :], Rxst, 128, gs_off, sh_off, 0,
                                 lambda dc, hf=hf: hp[:, dc, hf * 128:(hf + 1) * 128], Rhp, tmp=st["tmp"])
                    T.dma("sp", hso_d[idx], hp[:].rearrange("p a b -> p (a b)"), reads=[Rhp], writes=[Rhso[idx]])
                else:
                    sl = slice(idx * TB, (idx + 1) * TB)
                    modulate(esi, lambda dc: XT[:, dc, sl], XTr[idx], TB, 0, 8, 0,
                             lambda dc: hp[:, dc, :], Rhp, tmp=st["tmp"])
                    T.dma("sp", hown_d[idx], hp[:].rearrange("p a b -> p (a b)"), reads=[Rhp], writes=[HTr[idx]])

            tasks = [("own", j) for j in range(NB_EXT)] + [("so", bi) for bi in range(NSO)]
            for r in range(0, len(tasks), NSET):
                T.play(*[T.capture(lambda k=k, t=t: stage_task(k, t[0], t[1]))
                         for k, t in enumerate(tasks[r:r + NSET])])
            T.barrier()

        Wgb = [P.sb("Wg0", [128, 8, 1280], BF16, es1), None]
        RWgb = [Res("Wg0"), Res("Wg1")]
        cur = [0]

        class _WgView:
            def __getitem__(self, _):
                return Wgb[cur[0]]
        Wg = _WgView()

        Wo = P.sb("Wo", [128, 2, 1024], BF16, es1)
        RWo = Res("Wo")
        qd = [[P.sb(f"qd{h}{d}", [128, TB], BF16, es1) for d in range(2)] for h in range(2)]
        kd = [[P.sb(f"kd{h}{d}", [128, TB], BF16, es1) for d in range(2)] for h in range(2)]
        qc = [[P.sb(f"qc{h}{d}", [128, TB], F32, es1) for d in range(2)] for h in range(2)]
        klT = [[P.sb(f"klT{h}{d}", [128, TB], BF16, es1) for d in range(2)] for h in range(2)]
        Rops = [[Res(f"ops{h}{d}") for d in range(2)] for h in range(2)]
        RklT = [[Res(f"klT{h}{d}") for d in range(2)] for h in range(2)]
        decs = P.sb("decs", [128, 2, 2, TB // 64], F32, es1)
        Rdec = [[Res(f"dec{h}{d}") for d in range(2)] for h in range(2)]
        qT = [P.sb(f"qT{h}", [128, TB], F32, es1) for h in range(2)]
        RqT = [Res("qT0"), Res("qT1")]
        sg2 = [P.sb(f"sg{i}", [128, 2, TB], F32, es1) for i in range(3)]
        Rsg2 = [[Res(f"sg{i}{h}") for h in range(2)] for i in range(3)]
        tmpb = [[P.sb(f"gt{h}_{i}", [128, TB], F32, es1) for i in range(7)] for h in range(2)]
        tmpr = [[Res(f"gt{h}_{i}") for i in range(7)] for h in range(2)]
        kl_tm = [P.sb(f"kl_tm{d}", [128, TB // 128, 2, 128], BF16, es1) for d in range(2)]
        Rkl_tm = [Res("kl_tm0"), Res("kl_tm1")]
        v_tm = [P.sb(f"v_tm{i}", [128, TB // 128, 256], BF16, es1) for i in range(2)]
        Rv_tm = [Res("v_tm0"), Res("v_tm1")]
        sTm = [P.sb(f"sTm{i}", [128, 2, 128], BF16, es1) for i in range(2)]
        RsTm = [Res("sTm0"), Res("sTm1")]
        o_sb2 = [P.sb(f"o_sb{i}", [128, 2, TB], F32, es1) for i in range(2)]
        Ro_sb2 = [Res("o_sb0"), Res("o_sb1")]
        yT = P.sb("yT", [128, 2, TB], BF16, es1)
        RyT = Res("yT")
        S = P.sb("S", [128, 2, 2, 128], F32, es1)
        RS = [Res("S_F"), Res("S_B")]
        RS16 = [Res("S16_F"), Res("S16_B")]
        o_b = P.sb("o_b", [128, 2, EXT], BF16, es1)
        Ro_b = [Res(f"o_b{j}") for j in range(NB_EXT)]
        for i in range(2):
            T.op("pool", lambda e, i=i: e.memset(sTm[i][:], 0.0), writes=[RsTm[i]])

        B_PROJ = [1, 2]
        B_SC = 3
        B_KV = 4
        B_O = 5
        B_Y = 6
        proj_rr = [0]

        def next_proj_bank():
            b = B_PROJ[proj_rr[0] % 2]
            proj_rr[0] += 1
            return b

        OML = {0: 128, 1: 144}

        def gate_elementwise(gi, hh, d, hsrc, Rh, n, main, slot=None):
            slot = d if slot is None else slot
            sT, lf, cum, dA, ndL, ee0_, ee1_ = tmpb[hh]
            RsT, Rlf, Rcum, RdA, RndL, Ree0_, Ree1_ = tmpr[hh]
            ee = [ee0_, ee1_]
            Ree = [Ree0_, Ree1_]
            h = 2 * gi + hh
            zcol = (1 + d) * 256 + hh * 128
            pb = 1 + hh
            bank = PSB[pb]
            T.group("pe", [lambda e, dc=dc: e.matmul(bank[:, 0:n], Wg[0][:, dc, zcol:zcol + 128], hsrc(dc),
                                                      start=(dc == 0), stop=(dc == 7)) for dc in range(8)],
                    reads=[RWgb[cur[0]], Rh], writes=[RPS[pb]])
            lnoml = drv[:, 160 + 8 * d + h:160 + 8 * d + h + 1]
            lbc = drv[:, 176 + 8 * d + h:176 + 8 * d + h + 1]
            nch = n // 64
            l1 = ee[1]
            Rl1 = Ree[1]
            T.op("act", lambda e: e.activation(out=sT[:, 0:n], in_=bank[:, 0:n], func=AF.Exp),
                 reads=[RPS[pb]], writes=[RsT])
            T.op("act", lambda e: e.activation(out=l1[:, 0:n], in_=sT[:, 0:n], func=AF.Ln, bias=1.0),
                 reads=[RsT], writes=[Rl1])
            T.op("act", lambda e: e.activation(out=lf[:, 0:n], in_=sT[:, 0:n], func=AF.Ln, bias=lbc),
                 reads=[RsT, Rdrv], writes=[Rlf])
            T.op("pool", lambda e: e.tensor_tensor(out=lf[:, 0:n], in0=lf[:, 0:n], in1=l1[:, 0:n], op=ALU.subtract),
                 reads=[Rlf, Rl1], writes=[Rlf])
            T.op("dve", lambda e: e.tensor_tensor_scan(out=cum[:, 0:n], data0=scanmask[:, 0:n], data1=lf[:, 0:n],
                                                        initial=0.0, op0=ALU.mult, op1=ALU.add),
                 reads=[Rlf, Rcst], writes=[Rcum])
            cum3 = cum[:, 0:n].rearrange("p (c t) -> p c t", t=64)
            T.op("act", lambda e: e.activation(out=decs[:, hh, slot, 0:nch], in_=cum3[:, :, 63], func=AF.Exp),
                 reads=[Rcum], writes=[Rdec[hh][slot]])
            T.op("pool", lambda e: e.tensor_tensor(
                out=ndL[:, 0:n].rearrange("p (c t) -> p c t", t=64), in0=cum3,
                in1=cum3[:, :, 63:64].to_broadcast([128, nch, 64]), op=ALU.subtract),
                reads=[Rcum], writes=[RndL])
            if d == 1:
                T.op("pool", lambda e: e.tensor_tensor(out=ndL[:, 0:n], in0=ndL[:, 0:n], in1=lf[:, 0:n],
                                                       op=ALU.subtract), reads=[RndL, Rlf], writes=[RndL])
                T.op("pool", lambda e: e.tensor_tensor(out=cum[:, 0:n], in0=cum[:, 0:n], in1=lf[:, 0:n],
                                                       op=ALU.subtract), reads=[Rcum, Rlf], writes=[Rcum])
            A = cum
            RA = Rcum
            if d == 0:
                T.op("dve", lambda e: e.scalar_tensor_tensor(out=ee[0][:, 0:n], in0=ndL[:, 0:n], scalar=-1.0,
                                                             in1=l1[:, 0:n], op0=ALU.mult, op1=ALU.subtract),
                     reads=[RndL, Rl1], writes=[Ree[0]])
            else:
                T.op("dve", lambda e: e.tensor_tensor(out=ee[0][:, 0:n], in0=A[:, 0:n], in1=l1[:, 0:n], op=ALU.subtract),
                     reads=[RA, Rl1], writes=[Ree[0]])
            T.op("act", lambda e: e.activation(out=klT[hh][slot][:, 0:n], in_=ee[0][:, 0:n], func=AF.Exp, bias=lnoml),
                 reads=[Ree[0], Rdrv], writes=[RklT[hh][slot]])
            if not main:
                return
            refi = 31 if d == 0 else 32
            A3 = A[:, 0:n].rearrange("p (c t) -> p c t", t=64)
            T.op("pool", lambda e: e.tensor_tensor(
                out=dA[:, 0:n].rearrange("p (c t) -> p c t", t=64), in0=A3,
                in1=A3[:, :, refi:refi + 1].to_broadcast([128, nch, 64]), op=ALU.subtract),
                reads=[RA], writes=[RdA])
            sq_, sk_ = (1.0, -1.0) if d == 0 else (-1.0, 1.0)
            T.op("dve", lambda e: e.scalar_tensor_tensor(out=ee[0][:, 0:n], in0=dA[:, 0:n], scalar=sk_,
                                                         in1=l1[:, 0:n], op0=ALU.mult, op1=ALU.subtract),
                 reads=[RdA, Rl1], writes=[Ree[0]])
            T.op("act", lambda e: e.activation(out=kd[hh][slot][:, 0:n], in_=ee[0][:, 0:n], func=AF.Exp, bias=lnoml),
                 reads=[Ree[0], Rdrv], writes=[Rops[hh][slot]])
            T.op("act", lambda e: e.activation(out=sT[:, 0:n], in_=dA[:, 0:n], func=AF.Exp, scale=sq_),
                 reads=[RdA], writes=[RsT])
            T.op("dve", lambda e: e.tensor_tensor(out=qd[hh][slot][:, 0:n], in0=qT[hh][:, 0:n], in1=sT[:, 0:n],
                                                  op=ALU.mult), reads=[RqT[hh], RsT], writes=[Rops[hh][slot]])
            if d == 0:
                T.op("act", lambda e: e.activation(out=ndL[:, 0:n], in_=A[:, 0:n], func=AF.Exp),
                     reads=[RA], writes=[RndL])
            else:
                T.op("act", lambda e: e.activation(out=ndL[:, 0:n], in_=ndL[:, 0:n], func=AF.Exp, scale=-1.0),
                     reads=[RndL], writes=[RndL])
            T.op("pool", lambda e: e.tensor_tensor(out=qc[hh][slot][:, 0:n], in0=qT[hh][:, 0:n], in1=ndL[:, 0:n],
                                                   op=ALU.mult), reads=[RqT[hh], RndL], writes=[Rops[hh][slot]])

        def v_project(hsrc, Rh, n, vs=0):
            nt = n // 128
            pb = 1
            bank = PSB[pb]
            fns = []
            for ti in range(nt):
                for dc in range(8):
                    fns.append(lambda e, ti=ti, dc=dc: e.matmul(
                        bank[:, ti * 256:(ti + 1) * 256], hsrc(dc)[:, ti * 128:(ti + 1) * 128],
                        Wg[0][:, dc, 1024:1280], start=(dc == 0), stop=(dc == 7)))
            T.group("pe", fns, reads=[RWgb[cur[0]], Rh], writes=[RPS[pb]])
            T.op("act", lambda e: e.activation(
                out=v_tm[vs][:, 0:nt, :], in_=bank[:, 0:nt * 256].rearrange("p (a b) -> p a b", b=256), func=AF.Copy),
                reads=[RPS[pb]], writes=[Rv_tm[vs]])

        def kl_transpose(d, n):
            nt = n // 128
            half = RPSH[d]
            base = d * 512
            fns = []
            for ti in range(nt):
                for hh in range(2):
                    o0 = base + (ti * 2 + hh) * 128
                    fns.append(lambda e, ti=ti, hh=hh, o0=o0: e.transpose(
                        PSH[:, o0:o0 + 128], klT[hh][d][:, ti * 128:(ti + 1) * 128], ident[:]))
            T.group("pe", fns, reads=[RklT[0][d], RklT[1][d], Rid], writes=[half])
            T.op("act", lambda e: e.activation(
                out=kl_tm[d][:, 0:nt].rearrange("p a h k -> p (a h k)"), in_=PSH[:, base:base + nt * 256],
                func=AF.Copy), reads=[half], writes=[Rkl_tm[d]])

        def kv_bank(chunk):
            if chunk % 2 == 0:
                return PSB[B_KV][:, 0:256].rearrange("p (h v) -> p h v", v=128), RPS[B_KV]
            return PSB[0][:, 256:512].rearrange("p (h v) -> p h v", v=128), RPS[0]

        def kv_mm(ti, j, chunk, slot, vs):
            kvb, Rkv = kv_bank(chunk)
            fns = []
            for hh in range(2):
                fns.append(lambda e, hh=hh: e.matmul(
                    kvb[:, hh, :], kl_tm[slot][64 * j:64 * j + 64, ti, hh, :],
                    v_tm[vs][64 * j:64 * j + 64, ti, hh * 128:(hh + 1) * 128], start=True, stop=True))
            T.group("pe", fns, reads=[Rkl_tm[slot], Rv_tm[vs]], writes=[Rkv])

        def s_update(d, chunk, slot):
            kvb, Rkv = kv_bank(chunk)
            for hh in range(2):
                T.op("dve", lambda e, hh=hh: e.scalar_tensor_tensor(
                    out=S[:, d, hh, :], in0=S[:, d, hh, :], scalar=decs[:, hh, slot, chunk:chunk + 1],
                    in1=kvb[:, hh, :], op0=ALU.mult, op1=ALU.add),
                    reads=[RS[d], Rkv, Rdec[hh][slot]], writes=[RS[d]])

        def state_step(d, ti, j, chunk, slot=None, vs=0):
            slot = d if slot is None else slot
            kv_mm(ti, j, chunk, slot, vs)
            s_update(d, chunk, slot)

        def zero_state(d):
            T.op("pool", lambda e: e.memset(S[:, d].rearrange("p h v -> p (h v)"), 0.0), writes=[RS[d]])

        def so_prep(gi, d, slot, bi_so, part):
            hp, Rhp = hp2[slot], Rhp2[slot]
            hsrc = lambda dc: hp[:, dc, :]
            if part == "pre":
                T.dma("sp", hp[:].rearrange("p a b -> p (a b)"), hso_d[bi_so], reads=[Rhso[bi_so]], writes=[Rhp])
            elif part == "post":
                kl_transpose(slot, TB)
            elif part == 0:
                v_project(hsrc, Rhp, TB, slot)
                gate_elementwise(gi, 0, d, hsrc, Rhp, TB, main=False, slot=slot)
            else:
                gate_elementwise(gi, 1, d, hsrc, Rhp, TB, main=False, slot=slot)

        def so_steps(d, slot):
            nch = TB // 64
            order = range(nch) if d == 0 else range(nch - 1, -1, -1)
            for chunk in order:
                state_step(d, chunk // 2, chunk % 2, chunk, slot=slot, vs=slot)

        def m_prep(gi, j, dd, slot, part):
            hp, Rh = hp2[slot], Rhp2[slot]
            hsrc = lambda dc: hp[:, dc, :]
            n = TB
            sg = sg2[j % 3]
            if part == "pre":
                T.dma("sp", hp[:].rearrange("p a b -> p (a b)"), hown_d[j], reads=[HTr[j]], writes=[Rh])
                return
            if part == "post":
                kl_transpose(slot, n)
                return
            hh = part
            if part == 0:
                v_project(hsrc, Rh, n, slot)
            kinds = [0] if dd == 1 else [0, 3]
            for kind in kinds:
                dstT, Rdst = (qT[hh], RqT[hh]) if kind == 0 else (None, Rsg2[j % 3][hh])
                col = kind * 256 + hh * 128
                pb = 1 + hh
                bank = PSB[pb]
                T.group("pe", [lambda e, dc=dc, col=col, bank=bank: e.matmul(
                    bank[:, 0:n], Wg[0][:, dc, col:col + 128], hsrc(dc), start=(dc == 0), stop=(dc == 7))
                    for dc in range(8)], reads=[RWgb[cur[0]], Rh], writes=[RPS[pb]])
                dst = dstT[:, 0:n] if dstT is not None else sg[:, hh, 0:n]
                T.op("act", lambda e, dst=dst, bank=bank: e.activation(out=dst, in_=bank[:, 0:n],
                                                                        func=AF.Exp, scale=-1.0),
                     reads=[RPS[pb]], writes=[Rdst])
                T.op("act", lambda e, dst=dst: e.activation(out=dst, in_=dst, func=AF.Ln, bias=1.0),
                     reads=[Rdst], writes=[Rdst])
                T.op("act", lambda e, dst=dst: e.activation(out=dst, in_=dst, func=AF.Exp, scale=-1.0),
                     reads=[Rdst], writes=[Rdst])
                T.op("dve", lambda e, dst=dst, bank=bank: e.tensor_tensor(out=dst, in0=dst, in1=bank[:, 0:n],
                                                                           op=ALU.mult),
                     reads=[Rdst, RPS[pb]], writes=[Rdst])
            gate_elementwise(gi, hh, dd, hsrc, Rh, n, main=True, slot=slot)

        def m_loop(gi, j, dd, slot):
            o_sb, Ro_sb = o_sb2[j % 2], Ro_sb2[j % 2]
            n = TB
            nch = n // 64
            vs = slot
            tri = triF if dd == 0 else triB
            order = list(range(nch)) if dd == 0 else list(range(nch - 1, -1, -1))
            for ti in ([0, 1] if dd == 0 else [1, 0]):
                sb_i = ti % 2
                if True:
                    scb = PSB[B_SC][:, sb_i * 256:(sb_i + 1) * 256].rearrange("p (h c) -> p h c", c=128)
                    T.group("pe", [lambda e, hh=hh, ti=ti, scb=scb: e.matmul(
                        scb[:, hh, :], kd[hh][slot][:, ti * 128:(ti + 1) * 128],
                        qd[hh][slot][:, ti * 128:(ti + 1) * 128], start=True, stop=True) for hh in range(2)],
                        reads=[Rops[0][slot], Rops[1][slot]], writes=[RPS[B_SC]])
                    for blk in range(2):
                        p0 = blk * 64
                        T.op("dve", lambda e, p0=p0, sb_i=sb_i, scb=scb: e.tensor_tensor(
                            out=sTm[sb_i][p0:p0 + 64, :, p0:p0 + 64], in0=scb[p0:p0 + 64, :, p0:p0 + 64],
                            in1=tri[p0:p0 + 64, :].unsqueeze(1).to_broadcast([64, 2, 64]), op=ALU.mult),
                            reads=[RPS[B_SC], Rcst], writes=[RsTm[sb_i]])
            for chunk in order:
                ti, jj = chunk // 2, chunk % 2
                sb_i = ti % 2
                kv_mm(ti, jj, chunk, slot, vs)
                qtr = chunk % 4
                ob = PSB[B_O][:, qtr * 128:(qtr + 1) * 128].rearrange("p (h c) -> p h c", c=64)
                fns = []
                p0 = jj * 64
                for hh in range(2):
                    fns.append(lambda e, hh=hh, p0=p0, ti=ti, sb_i=sb_i, ob=ob: e.matmul(
                        ob[:, hh, :], v_tm[vs][p0:p0 + 64, ti, hh * 128:(hh + 1) * 128],
                        sTm[sb_i][p0:p0 + 64, hh, p0:p0 + 64], start=True, stop=False))
                    fns.append(lambda e, hh=hh, chunk=chunk, ob=ob: e.matmul(
                        ob[:, hh, :], S[:, dd, hh, :], qc[hh][slot][:, chunk * 64:(chunk + 1) * 64],
                        start=False, stop=True))
                T.group("pe", fns, reads=[Rv_tm[vs], RsTm[sb_i], RS[dd], Rops[0][slot], Rops[1][slot]],
                        writes=[RPS[B_O]])
                if dd == 0:
                    osl = o_sb[:, :, chunk * 64:(chunk + 1) * 64]
                    T.op("act", lambda e, osl=osl, ob=ob: e.activation(out=osl, in_=ob, func=AF.Copy),
                         reads=[RPS[B_O]], writes=[Ro_sb])
                else:
                    osl = o_b[:, :, j * TB + chunk * 64:j * TB + (chunk + 1) * 64]
                    T.op("act", lambda e, osl=osl, ob=ob: e.activation(out=osl, in_=ob, func=AF.Copy),
                         reads=[RPS[B_O]], writes=[Ro_b[j]])
                s_update(dd, chunk, slot)

        def m_readout(gi, j):
            o_sb, Ro_sb = o_sb2[j % 2], Ro_sb2[j % 2]
            n = TB
            sl = slice(j * TB, (j + 1) * TB)
            sg = sg2[j % 3]
            T.op("dve", lambda e: e.tensor_tensor(out=o_sb[:], in0=o_sb[:], in1=o_b[:, :, sl], op=ALU.add),
                 reads=[Ro_sb, Ro_b[j]], writes=[Ro_sb])
            for hh in range(2):
                T.op("act", lambda e, hh=hh: e.activation(out=sq[:, hh, 0:n], in_=o_sb[:, hh, :], func=AF.Square),
                     reads=[Ro_sb], writes=[Rsq])
            for hh in range(2):
                bank = PSB[B_Y]
                T.group("pe", [lambda e, hh=hh, bank=bank: e.matmul(bank[:, 0:n], ones[:], sq[:, hh, 0:n],
                                                                     start=True, stop=True)],
                        reads=[Rsq, Rid], writes=[RPS[B_Y]])
                T.op("act", lambda e, bank=bank: e.activation(out=rstd[:, 0:n], in_=bank[:, 0:n], func=AF.Ln,
                                                               scale=1.0 / 128, bias=EPS),
                     reads=[RPS[B_Y]], writes=[Rrstd])
                T.op("act", lambda e: e.activation(out=rstd[:, 0:n], in_=rstd[:, 0:n], func=AF.Exp, scale=-0.5),
                     reads=[Rrstd], writes=[Rrstd])
                T.op("dve", lambda e, hh=hh: e.tensor_tensor(out=t32[:, 0:n], in0=o_sb[:, hh, :], in1=rstd[:, 0:n],
                                                              op=ALU.mult), reads=[Ro_sb, Rrstd], writes=[Rt32])
                T.op("dve", lambda e, hh=hh: e.scalar_tensor_tensor(
                    out=yT[:, hh, :], in0=t32[:, 0:n], scalar=vec[:, VEC["gnorm"]:VEC["gnorm"] + 1],
                    in1=sg[:, hh, 0:n], op0=ALU.mult, op1=ALU.mult), reads=[Rt32, Rsg2[j % 3][hh], Rvec], writes=[RyT])
            for oc in range(8):
                by = B_Y
                bank = PSB[by][:, (oc % 2) * 256:(oc % 2) * 256 + 256]
                T.group("pe", [lambda e, hh=hh, oc=oc, bank=bank: e.matmul(
                    bank[:, 0:n], Wo[:, hh, oc * 128:(oc + 1) * 128], yT[:, hh, :], start=(hh == 0), stop=(hh == 1))
                    for hh in range(2)], reads=[RWo, RyT], writes=[RPS[by]])
                T.op("dve", lambda e, oc=oc, bank=bank: e.scalar_tensor_tensor(
                    out=XT[:, oc, sl], in0=bank[:, 0:n], scalar=dcol(0, 16, oc), in1=XT[:, oc, sl],
                    op0=ALU.mult, op1=ALU.add), reads=[RPS[by], XTr[j][oc], Rdrv], writes=[XTr[j][oc]])

        def zero_state(d):
            T.op("pool", lambda e: e.memset(S[:, d].rearrange("p h v -> p (h v)"), 0.0), writes=[RS[d]])

        cap = T.capture
        esw = ExitStack()
        wmp1 = [P.sb(f"wmpL1_{i}", [128, 8, 512], BF16, esw) for i in range(2)]
        Rwmp1 = [Res("wmpL1_0"), Res("wmpL1_1")]
        T.dma("pool", Wgb[0][:], w_in_d[0], writes=[RWgb[0]])
        for gi in range(4):
            cur[0] = gi % 2
            if gi >= 1 and gi + 1 < 4:
                T.dma("pool", Wgb[(gi + 1) % 2][:], w_in_d[gi + 1], writes=[RWgb[(gi + 1) % 2]])
            T.dma("pool", Wo[:], w_out_d[gi], writes=[RWo])
            for d in (1, 0):
                if gi == 0 and d == 0:
                    T.barrier()
                    esw.close()
                    Wgb[1] = P.sb("Wg1", [128, 8, 1280], BF16, es1)
                    T.dma("pool", Wgb[1][:], w_in_d[1], writes=[RWgb[1]])
                zero_state(d)
                stages = [("so", 0)]
                if d == 1:
                    stages += [("so", 1 + (j - NB_EXT)) for j in range(L // TB - 1, NB_EXT - 1, -1)]
                    stages += [("m", j) for j in range(NB_EXT - 1, -1, -1)]
                else:
                    stages += [("m", j) for j in range(NB_EXT)]

                def prep(i, part):
                    kind, a = stages[i]
                    slot = i % 2
                    if kind == "so":
                        so_prep(gi, d, slot, a, part)
                    else:
                        m_prep(gi, a, d, slot, part)

                def body(i):
                    kind, a = stages[i]
                    slot = i % 2
                    if kind == "so":
                        so_steps(d, slot)
                    else:
                        m_loop(gi, a, d, slot)

                prep(0, "pre")
                T.play(cap(lambda: prep(0, 0)), cap(lambda: prep(0, 1)))
                prep(0, "post")
                pending = None
                for i in range(len(stages)):
                    streams = [cap(lambda: body(i))]
                    if pending is not None:
                        streams.append(cap(lambda: m_readout(gi, pending)))
                    if i + 1 < len(stages):
                        prep(i + 1, "pre")
                        streams += [cap(lambda: prep(i + 1, 0)), cap(lambda: prep(i + 1, 1))]
                        if gi == 0 and d == 1 and i < 12:
                            streams.append(cap(lambda: mod_piece(1, i, wmp1, Rwmp1)))
                    T.play(*streams)
                    if i + 1 < len(stages):
                        prep(i + 1, "post")
                    if gi == 0 and d == 1 and i == 11:
                        mod_finish(1)
                    pending = stages[i][1] if (d == 0 and stages[i][0] == "m") else None
                if pending is not None:
                    m_readout(gi, pending)
        T.barrier()

    if stage == "x1":
        return _finish(P, T, XT, XTr, dbg_d, out_d, debug=True)

    def moe_phase(l, ntok):
        nb = ntok // TB
        MB = 512
        nmb = ntok // MB
        with ExitStack() as es2:
            H2 = P.sb(f"H2_{l}", [128, 8, ntok], BF16, es2)
            RH2 = [Res(f"H2_{j}") for j in range(nmb)]
            h32b = [P.sb(f"h32_{l}_{i}", [128, 8, TB], F32, es2) for i in range(2)]
            Rh32b = [[Res(f"h32_{i}_{dc}") for dc in range(8)] for i in range(2)]
            sq = P.sb(f"msq_{l}", [128, 8, TB], BF16, es2)
            rstd = P.sb(f"mrstd_{l}", [128, TB], F32, es2)
            t32 = P.sb(f"mt32_{l}", [128, TB], F32, es2)
            T32ALT[id(t32)] = (P.sb(f"mt32b_{l}", [128, TB], F32, es2), Res("mt32b"))
            mtmp = (sq, Res("msq"), rstd, Res("mrstd"), t32, Res("mt32"), PSB[0], RPS[0])
            rw = P.sb(f"rw_{l}", [128, 8, NE], F32, es2)
            Rrw = Res("rw")
            combT = P.sb(f"combT_{l}", [16, ntok], F32, es2)
            RcombT = [Res(f"combT{j}") for j in range(nmb)]
            rs = P.sb(f"rs_{l}", [128, 2, NE], F32, es2)
            rsel = P.sb(f"rsel_{l}", [128, 2, NE], F32, es2)
            req = P.sb(f"req_{l}", [128, 2, NE], F32, es2)
            rsel2 = P.sb(f"rsel2_{l}", [128, 2, NE], F32, es2)
            rmk = P.sb(f"rmk_{l}", [128, 2, NE], F32, es2)
            rm1 = P.sb(f"rm1_{l}", [128, 8], F32, es2)
            rm2 = P.sb(f"rm2_{l}", [128, 8], F32, es2)
            rgs = P.sb(f"rgs_{l}", [128, 8], F32, es2)
            rgm = P.sb(f"rgm_{l}", [128, 8], F32, es2)
            rgx = P.sb(f"rgx_{l}", [128, 2], F32, es2)
            rws = P.sb(f"rws_{l}", [128, 2], F32, es2)
            comb = P.sb(f"comb_{l}", [128, 2, NE], F32, es2)
            Rr = Res("route")
            T.dma("sp", rw[:], router_d[:, :, :], writes=[Rrw])
            gs_off, sh_off = 24, 32
            rb = vec[:, VEC["rb"]:VEC["rb"] + NE]
            def rt_mod(j):
                sl = slice(j * TB, (j + 1) * TB)
                mbi = (j * TB) // MB
                h32, Rh32 = h32b[j % 2], Rh32b[j % 2]
                modulate(es2, lambda dc: XT[:, dc, sl], XTr[j], TB, gs_off, sh_off, l,
                         lambda dc: H2[:, dc, sl], RH2[mbi],
                         dst32=lambda dc: h32[:, dc, :], Rdst32=Rh32, tmp=mtmp)

            def rt_route(j):
                sl = slice(j * TB, (j + 1) * TB)
                mbi = (j * TB) // MB
                h32, Rh32 = h32b[j % 2], Rh32b[j % 2]
                lg = PSB[1][:, 0:2 * NE].rearrange("p (a e) -> p a e", e=NE)
                fns = []
                for ti in range(2):
                    for dc in range(8):
                        fns.append(lambda e, ti=ti, dc=dc: e.matmul(
                            lg[:, ti, :], h32[:, dc, ti * 128:(ti + 1) * 128], rw[:, dc, :],
                            start=(dc == 0), stop=(dc == 7)))
                T.group("pe", fns, reads=Rh32 + [Rrw], writes=[RPS[1]])
                T.op("act", lambda e: e.activation(out=rs[:], in_=lg, func=AF.Exp, scale=-1.0), reads=[RPS[1]], writes=[Rr])
                T.op("dve", lambda e: e.tensor_scalar(out=rs[:], in0=rs[:], scalar1=1.0, scalar2=None, op0=ALU.add),
                     reads=[Rr], writes=[Rr])
                T.op("dve", lambda e: e.reciprocal(out=rs[:], in_=rs[:]), reads=[Rr], writes=[Rr])
                dv = lambda fn, rd=(), : T.op("dve", fn, reads=[Rr] + list(rd), writes=[Rr])
                g4 = lambda t: t[:].rearrange("p a (g k) -> p (a g) k", k=4)
                dv(lambda e: e.tensor_tensor(out=rsel[:], in0=rs[:], in1=rb.unsqueeze(1).to_broadcast([128, 2, NE]),
                                             op=ALU.add), rd=[Rvec])
                dv(lambda e: e.tensor_reduce(out=rm1[:], in_=g4(rsel), axis=mybir.AxisListType.X, op=ALU.max))
                dv(lambda e: e.tensor_tensor(out=g4(req), in0=g4(rsel), in1=rm1[:].unsqueeze(2).to_broadcast([128, 8, 4]),
                                             op=ALU.is_equal))
                dv(lambda e: e.scalar_tensor_tensor(out=rsel2[:], in0=req[:], scalar=-1.0e9, in1=rsel[:],
                                                    op0=ALU.mult, op1=ALU.add))
                dv(lambda e: e.tensor_reduce(out=rm2[:], in_=g4(rsel2), axis=mybir.AxisListType.X, op=ALU.max))
                dv(lambda e: e.tensor_tensor(out=rgs[:], in0=rm1[:], in1=rm2[:], op=ALU.add))
                dv(lambda e: e.tensor_reduce(out=rgx[:], in_=rgs[:].rearrange("p (a g) -> p a g", g=4),
                                             axis=mybir.AxisListType.X, op=ALU.max))
                dv(lambda e: e.tensor_tensor(out=rgm[:].rearrange("p (a g) -> p a g", g=4),
                                             in0=rgs[:].rearrange("p (a g) -> p a g", g=4),
                                             in1=rgx[:].unsqueeze(2).to_broadcast([128, 2, 4]), op=ALU.is_equal))
                dv(lambda e: e.tensor_tensor(out=g4(rmk), in0=g4(rsel), in1=rm2[:].unsqueeze(2).to_broadcast([128, 8, 4]),
                                             op=ALU.is_ge))
                dv(lambda e: e.tensor_tensor(out=g4(rmk), in0=g4(rmk), in1=rgm[:].unsqueeze(2).to_broadcast([128, 8, 4]),
                                             op=ALU.mult))
                dv(lambda e: e.tensor_tensor(out=rmk[:], in0=rmk[:], in1=rs[:], op=ALU.mult))
                dv(lambda e: e.tensor_reduce(out=rws[:], in_=rmk[:], axis=mybir.AxisListType.X, op=ALU.add))
                dv(lambda e: e.reciprocal(out=rws[:], in_=rws[:]))
                dv(lambda e: e.tensor_tensor(out=comb[:], in0=rmk[:], in1=rws[:].unsqueeze(2).to_broadcast([128, 2, NE]),
                                             op=ALU.mult))
                tp = PSB[2][0:16, 0:256]
                T.group("pe", [lambda e, ti=ti: e.transpose(tp[:, ti * 128:(ti + 1) * 128], comb[:, ti, :],
                                                            cst32[:, CST["ident"]:CST["ident"] + 128])
                               for ti in range(2)], reads=[Rr, Rcst], writes=[RPS[2]])
                T.op("act", lambda e: e.activation(out=combT[:, sl], in_=tp, func=AF.Copy),
                     reads=[RPS[2]], writes=[RcombT[mbi]])
            rt_mod(0)
            for j in range(nb):
                nxt = T.capture(lambda: rt_mod(j + 1)) if j + 1 < nb else []
                T.play(T.capture(lambda: rt_route(j)), nxt)
            if stage == f"comb{l}":
                return "comb", combT
            cmb_d = nc.dram_tensor(f"cmb_scratch{l}", [16, ntok], F32, kind="Internal").ap()
            Rcmb_d = Res("cmb_d")
            T.dma("sp", cmb_d[:, :], combT[:, :], reads=RcombT, writes=[Rcmb_d])
            wgu = [P.sb(f"wgu{i}_{l}", [128, 8, 512], BF16, es2) for i in range(2)]
            wdn = [P.sb(f"wdn{i}_{l}", [128, 2, 1024], BF16, es2) for i in range(2)]
            Rwgu = [Res("wgu0"), Res("wgu1")]
            Rwdn = [Res("wdn0"), Res("wdn1")]
            cmb_sb = P.sb(f"cmb_sb_{l}", [128, MB], F32, es2)
            Rcmb = Res("cmb")
            s_sb = [P.sb(f"s_sb{i}_{l}", [128, MB], F32, es2) for i in range(2)]
            Rs_sb = [Res("s_sb0"), Res("s_sb1")]
            t_sb = [P.sb(f"t_sb{i}_{l}", [128, MB], F32, es2) for i in range(2)]
            Rt_sb = [Res("t_sb0"), Res("t_sb1")]
            a_sb = [P.sb(f"a_sb{i}_{l}", [128, 2, MB], BF16, es2) for i in range(2)]
            Ra_sb = [[Res(f"a{i}{f}") for f in range(2)] for i in range(2)]
            g2_off = 40
            cmb2 = [cmb_sb, P.sb(f"cmb_sb2_{l}", [128, MB], F32, es2)]
            Rcmb2 = [Rcmb, Res("cmb2")]
            iters = [(ex, mb) for ex in range(NE) for mb in range(nmb)]

            def load_w(ex):
                wb = ex % 2
                T.dma("pool", wgu[wb][:], wgu_d[l, ex], writes=[Rwgu[wb]])
                T.dma("pool", wdn[wb][:], wd_d[l, ex], writes=[Rwdn[wb]])

            def emit_gu(it):
                ex, mb = iters[it]
                wb = ex % 2
                ab = it % 2
                msl = slice(mb * MB, (mb + 1) * MB)
                T.dma("sp", cmb2[ab][:], cmb_d[ex:ex + 1, msl].to_broadcast([128, MB]),
                      reads=[Rcmb_d], writes=[Rcmb2[ab]])
                for fc in range(2):
                    bg, bu = 1 + fc, 3 + fc
                    T.group("pe", [lambda e, dc=dc, fc=fc, bg=bg: e.matmul(
                        PSB[bg][:, :], wgu[wb][:, dc, fc * 128:(fc + 1) * 128], H2[:, dc, msl],
                        start=(dc == 0), stop=(dc == 7)) for dc in range(8)],
                        reads=[Rwgu[wb], RH2[mb]], writes=[RPS[bg]], cost=1.8)
                    T.group("pe", [lambda e, dc=dc, fc=fc, bu=bu: e.matmul(
                        PSB[bu][:, :], wgu[wb][:, dc, 256 + fc * 128:256 + (fc + 1) * 128], H2[:, dc, msl],
                        start=(dc == 0), stop=(dc == 7)) for dc in range(8)],
                        reads=[Rwgu[wb], RH2[mb]], writes=[RPS[bu]], cost=1.8)
                    T.op("act", lambda e, fc=fc, bg=bg: e.activation(out=s_sb[fc][:], in_=PSB[bg][:, :], func=AF.Silu),
                         reads=[RPS[bg]], writes=[Rs_sb[fc]], cost=0.65)
                    T.op("pool", lambda e, fc=fc: e.tensor_tensor(out=t_sb[fc][:], in0=s_sb[fc][:], in1=cmb2[ab][:], op=ALU.mult),
                         reads=[Rs_sb[fc], Rcmb2[ab]], writes=[Rt_sb[fc]], cost=1.3)
                    T.op("dve", lambda e, fc=fc, bu=bu: e.tensor_tensor(out=a_sb[ab][:, fc, :], in0=PSB[bu][:, :], in1=t_sb[fc][:],
                                                          op=ALU.mult),
                         reads=[RPS[bu], Rt_sb[fc]], writes=[Ra_sb[ab][fc]], cost=0.7)

            ysb = [P.sb(f"ysb{i}_{l}", [128, MB], F32, es2) for i in range(2)]
            Rysb = [Res("ysb0"), Res("ysb1")]

            def emit_down(it):
                ex, mb = iters[it]
                wb = ex % 2
                ab = it % 2
                msl = slice(mb * MB, (mb + 1) * MB)
                for oc in range(8):
                    by = (5, 6, 0)[oc % 3]
                    T.group("pe", [lambda e, fc=fc, oc=oc, by=by: e.matmul(
                        PSB[by][:, :], wdn[wb][:, fc, oc * 128:(oc + 1) * 128], a_sb[ab][:, fc, :],
                        start=(fc == 0), stop=(fc == 1)) for fc in range(2)],
                        reads=[Rwdn[wb], Ra_sb[ab][0], Ra_sb[ab][1]], writes=[RPS[by]], cost=0.45)
                    xr = [XTr[mb * 2][oc], XTr[mb * 2 + 1][oc]]
                    if oc not in (1, 5):
                        T.op("dve", lambda e, oc=oc, by=by: e.scalar_tensor_tensor(
                            out=XT[:, oc, msl], in0=PSB[by][:, :], scalar=dcol(l, g2_off, oc),
                            in1=XT[:, oc, msl], op0=ALU.mult, op1=ALU.add),
                            reads=[RPS[by], Rdrv] + xr, writes=xr, cost=0.8)
                    else:
                        yi = (oc // 2) % 2
                        T.op("act", lambda e, oc=oc, by=by, yi=yi: e.activation(
                            out=ysb[yi][:], in_=PSB[by][:, :], func=AF.Copy, scale=dcol(l, g2_off, oc)),
                            reads=[RPS[by], Rdrv], writes=[Rysb[yi]], cost=0.7)
                        T.op("pool", lambda e, oc=oc, yi=yi: e.tensor_tensor(
                            out=XT[:, oc, msl], in0=XT[:, oc, msl], in1=ysb[yi][:], op=ALU.add),
                            reads=[Rysb[yi]] + xr, writes=xr, cost=1.3)

            load_w(0)
            load_w(1)
            emit_gu(0)
            for it in range(len(iters)):
                nxt = T.capture(lambda: emit_gu(it + 1)) if it + 1 < len(iters) else []
                T.play(nxt, T.capture(lambda: emit_down(it)))
                ex, mb = iters[it]
                if mb == nmb - 1 and ex + 2 < NE:
                    load_w(ex + 2)
            T.barrier()
        return None

    r = moe_phase(0, EXT)
    if stage == "comb0":
        return _finish_comb(P, T, r[1], dbg_d, out_d, XT, XTr)
    if stage == "x2":
        return _finish(P, T, XT, XTr, dbg_d, out_d, debug=True)

    pidx = _pool_mats_cached(0)[1]
    plan = _pool_plan()
    with ExitStack() as es3:
        u_tm = P.sb("u_tm", [128, EXT // 128, D], BF16, es3)
        Ru = [Res(f"u{t}") for t in range(EXT // 128)]
        pwi = P.sb("pwi", [128, 8, D], BF16, es3)
        pwo = P.sb("pwo", [128, 8, D], BF16, es3)
        pwg = P.sb("pwg", [128, 4, 2, 256], BF16, es3)
        pm = P.sb("pm", [128, n_pmat, 128], BF16, es3)
        Rpw = Res("pw")
        hb = P.sb("hb", [128, 8, TB], BF16, es3)
        Rhb = Res("hb")
        sq = P.sb("psq", [128, 8, TB], BF16, es3)
        rstd = P.sb("prstd", [128, TB], F32, es3)
        t32 = P.sb("pt32", [128, TB], F32, es3)
        T32ALT[id(t32)] = (P.sb("pt32b", [128, TB], F32, es3), Res("pt32b"))
        mtmp = (sq, Res("psq"), rstd, Res("prstd"), t32, Res("pt32"), PSB[0], RPS[0])
        dT = P.sb("dT", [128, 8, TB], BF16, es3)
        RdT = Res("dT")
        yin = P.sb("yin", [128, 8, TB], BF16, es3)
        Ryin = Res("yin")
        T.dma("pool", pwi[:], pw_in_d[:, :, :], writes=[Rpw])
        T.dma("pool", pwo[:], pw_out_d[:, :, :], writes=[Rpw])
        T.dma("pool", pwg[:], pw_grp_d[:, :, :, :], writes=[Rpw])
        T.dma("pool", pm[:], pmat_d.rearrange("m s t -> s m t"), writes=[Rpw])
        def pl_u(j):
            sl = slice(j * TB, (j + 1) * TB)
            modulate(es3, lambda dc, sl=sl: XT[:, dc, sl], XTr[j], TB, 0, 8, 1,
                     lambda dc: hb[:, dc, :], Rhb, tmp=mtmp)
            for ti in range(2):
                tile_i = j * 2 + ti
                for hf in range(2):
                    pb = 1 + (ti * 2 + hf) % 4
                    T.group("pe", [lambda e, dc=dc, ti=ti, hf=hf, pb=pb: e.matmul(
                        PSB[pb][:, :], hb[:, dc, ti * 128:(ti + 1) * 128], pwi[:, dc, hf * 512:(hf + 1) * 512],
                        start=(dc == 0), stop=(dc == 7)) for dc in range(8)], reads=[Rhb, Rpw], writes=[RPS[pb]])
                    eng = "act" if hf == 0 else "dve"
                    if eng == "act":
                        T.op("act", lambda e, tile_i=tile_i, hf=hf, pb=pb: e.activation(
                            out=u_tm[:, tile_i, hf * 512:(hf + 1) * 512], in_=PSB[pb][:, :], func=AF.Copy),
                            reads=[RPS[pb]], writes=[Ru[tile_i]])
                    else:
                        T.op("dve", lambda e, tile_i=tile_i, hf=hf, pb=pb: e.tensor_copy(
                            out=u_tm[:, tile_i, hf * 512:(hf + 1) * 512], in_=PSB[pb][:, :]),
                            reads=[RPS[pb]], writes=[Ru[tile_i]])
        def pl_mix(j):
            sl = slice(j * TB, (j + 1) * TB)
            for ti in range(2):
                tile_i = j * 2 + ti
                for wi in range(4):
                    dmin, dmax, nsp = plan[wi]
                    ds = [d for d in range(dmin, dmax + 1) if (wi, tile_i, d) in pidx]
                    pb = 1 + wi % 2
                    dps = PSB[pb][:, 0:256].rearrange("p (f t) -> p f t", t=128)
                    fns = []
                    srcs = []
                    for fc in range(2):
                        for k, d in enumerate(ds):
                            mi = pidx[(wi, tile_i, d)]
                            fns.append(lambda e, fc=fc, d=d, mi=mi, k=k, wi=wi, tile_i=tile_i, dps=dps, nds=len(ds): e.matmul(
                                dps[:, fc, :], u_tm[:, tile_i + d, wi * 256 + fc * 128:wi * 256 + (fc + 1) * 128],
                                pm[:, mi, :], start=(k == 0), stop=(k == nds - 1)))
                    for d in ds:
                        srcs.append(Ru[tile_i + d])
                    T.group("pe", fns, reads=srcs + [Rpw], writes=[RPS[pb]])
                    T.op("act", lambda e, wi=wi, ti=ti, dps=dps: e.activation(
                        out=dT[:, wi * 2:wi * 2 + 2, ti * 128:(ti + 1) * 128], in_=dps, func=AF.Copy),
                        reads=[RPS[pb]], writes=[RdT])
            for wi in range(4):
                for fo in range(2):
                    pb = 3 + (wi * 2 + fo) % 2
                    T.group("pe", [lambda e, fc=fc, wi=wi, fo=fo, pb=pb: e.matmul(
                        PSB[pb][:, 0:TB], pwg[:, wi, fc, fo * 128:(fo + 1) * 128], dT[:, wi * 2 + fc, :],
                        start=(fc == 0), stop=(fc == 1)) for fc in range(2)], reads=[RdT, Rpw], writes=[RPS[pb]])
                    ci = wi * 2 + fo
                    T.op("act", lambda e, ci=ci, pb=pb: e.activation(
                        out=yin[:, ci, :], in_=PSB[pb][:, 0:TB], func=AF.Copy,
                        scale=vec[:, VEC["pscale"] + ci:VEC["pscale"] + ci + 1]),
                        reads=[RPS[pb], Rvec], writes=[Ryin])
            for oc in range(8):
                pb = 5 + oc % 2
                T.group("pe", [lambda e, fcx=fcx, oc=oc, pb=pb: e.matmul(
                    PSB[pb][:, 0:TB], pwo[:, fcx, oc * 128:(oc + 1) * 128], yin[:, fcx, :],
                    start=(fcx == 0), stop=(fcx == 7)) for fcx in range(8)], reads=[Ryin, Rpw], writes=[RPS[pb]])
                T.op("dve", lambda e, oc=oc, pb=pb, sl=sl: e.scalar_tensor_tensor(
                    out=XT[:, oc, sl], in0=PSB[pb][:, 0:TB], scalar=dcol(1, 16, oc), in1=XT[:, oc, sl],
                    op0=ALU.mult, op1=ALU.add), reads=[RPS[pb], XTr[j][oc], Rdrv], writes=[XTr[j][oc]])
        for j in range(NB_EXT):
            pl_u(j)
        for j in range(NB_OWN):
            pl_mix(j)
        T.barrier()
    if stage == "x3":
        return _finish(P, T, XT, XTr, dbg_d, out_d, debug=True)

    moe_phase(1, OWN)
    if stage == "x4":
        return _finish(P, T, XT, XTr, dbg_d, out_d, debug=True)

    out_evs = []
    with ExitStack() as es4:
        sq2 = [P.sb(f"fsq{i}", [128, 8, TB], BF16, es4) for i in range(2)]
        Rsq2 = [Res("fsq0"), Res("fsq1")]
        rstd2 = [P.sb(f"frstd{i}", [128, TB], F32, es4) for i in range(2)]
        Rrstd2 = [Res("frstd0"), Res("frstd1")]
        out_v = out_d.rearrange("(dc p) t -> p dc t", p=128)

        def fin_block(j):
            sl = slice(j * TB, (j + 1) * TB)
            sq, Rsq, rstd, Rrstd = sq2[j % 2], Rsq2[j % 2], rstd2[j % 2], Rrstd2[j % 2]
            for dc in range(8):
                if dc % 2 == 0:
                    T.op("act", lambda e, dc=dc: e.activation(out=sq[:, dc, :], in_=XT[:, dc, sl], func=AF.Square),
                         reads=[XTr[j][dc]], writes=[Rsq])
                else:
                    T.op("pool", lambda e, dc=dc: e.tensor_tensor(out=sq[:, dc, :], in0=XT[:, dc, sl], in1=XT[:, dc, sl],
                                                                  op=ALU.mult), reads=[XTr[j][dc]], writes=[Rsq])
            pb = 1 + j % 2
            T.group("pe", [lambda e, dc=dc: e.matmul(PSB[pb][:, 0:TB], ones[:], sq[:, dc, :],
                                                      start=(dc == 0), stop=(dc == 7)) for dc in range(8)],
                    reads=[Rsq, Rid], writes=[RPS[pb]])
            T.op("act", lambda e: e.activation(out=rstd[:], in_=PSB[pb][:, 0:TB], func=AF.Ln, scale=1.0 / D, bias=EPS),
                 reads=[RPS[pb]], writes=[Rrstd])
            T.op("act", lambda e: e.activation(out=rstd[:], in_=rstd[:], func=AF.Exp, scale=-0.5),
                 reads=[Rrstd], writes=[Rrstd])
            for dc in range(8):
                T.op("dve", lambda e, dc=dc: e.scalar_tensor_tensor(
                    out=XT[:, dc, sl], in0=XT[:, dc, sl], scalar=vec[:, VEC["fing"] + dc:VEC["fing"] + dc + 1],
                    in1=rstd[:], op0=ALU.mult, op1=ALU.mult), reads=[XTr[j][dc], Rrstd, Rvec], writes=[XTr[j][dc]])

        for j in range(0, NB_OWN, 2):
            T.play(T.capture(lambda: fin_block(j)), T.capture(lambda: fin_block(j + 1)))
            if not stage:
                for jj in (j, j + 1):
                    sl = slice(jj * TB, (jj + 1) * TB)
                    out_evs.append(T.dma("sp", out_v[:, :, sl], XT[:, :, sl], reads=XTr[jj]))
        T.barrier()
    if not stage:
        for ev in out_evs:
            T.E["sp"].wait(ev)
        print(f"[kernel] ops={T.nops} waits={T.nwaits}", flush=True)
        return
    return _finish(P, T, XT, XTr, dbg_d, out_d, debug=bool(stage))


def _finish_comb(P, T, combT, dbg_d, out_d, XT, XTr):
    T.barrier()
    ev = T.dma("sp", dbg_d[0:16, :], combT[:, :])
    T.E["sp"].wait(ev)
    _finish(P, T, XT, XTr, dbg_d, out_d, debug=False)


def _finish(P, T, XT, XTr, dbg_d, out_d, debug):
    evs = []
    if debug:
        for dc in range(8):
            evs.append(T.dma("sp", dbg_d[dc * 128:(dc + 1) * 128, :], XT[:, dc, :], reads=[r[dc] for r in XTr]))
    for dc in range(8):
        evs.append(T.dma("sp", out_d[dc * 128:(dc + 1) * 128, :], XT[:, dc, 0:OWN], reads=[r[dc] for r in XTr]))
    for ev in evs:
        T.E["sp"].wait(ev)
    print(f"[kernel] ops={T.nops} waits={T.nwaits}", flush=True)


def _prepare(inputs):
    shared = _shared_weights(inputs)
    w_in = np.asarray(inputs["hg_w_in"][0], np.float32)
    w_in_by_half = [_w_in_tiled(w_in, 0), _w_in_tiled(w_in, 1)]
    return shared, w_in_by_half


def kernel(**inputs):
    inputs = {k: np.asarray(v) for k, v in inputs.items()}
    shared, w_in_by_half = _prepare(inputs)
    n_pmat = _pool_mats_cached(0)[0].shape[0]
    nc = build_program(stage=_DBG["stage"], n_pmat=n_pmat)
    in_maps = [_core_inputs(inputs, shared, w_in_by_half, c) for c in range(8)]
    res = run_bass_kernel_spmd(nc, in_maps, core_ids=list(range(8)))
    out = np.zeros((4, L, D), np.float32)
    for c in range(8):
        b, half = c // 2, c % 2
        o = res.results[c]["outT"].T
        if half == 0:
            out[b, 0:OWN] = o
        else:
            out[b, OWN:] = o[::-1]
    return out
```

```python
import numpy as np
from contextlib import ExitStack
import concourse.bass as bass
import concourse.mybir as mybir
from concourse.bass_utils import run_bass_kernel_spmd

F32 = mybir.dt.float32
BF16 = mybir.dt.bfloat16
AF = mybir.ActivationFunctionType
ALU = mybir.AluOpType

D = 1024
L = 4096
CTX = 256
OWN = 2048
HALO = 512
EXT = OWN + HALO
TB = 256
NB_EXT = EXT // TB
NB_OWN = OWN // TB
EPS = 1e-6
NE = 16
POOL_WINDOWS = (2, 4, 8, 16)


class Res:
    __slots__ = ("name", "w", "r", "tw", "tr")

    def __init__(self, name=""):
        self.name = name
        self.w = None
        self.r = []
        self.tw = 0.0
        self.tr = 0.0


class _Eng:
    def __init__(self, trk, name, handle, sem):
        self.trk = trk
        self.name = name
        self.h = handle
        self.sem = sem
        self.count = 0
        self.seen = {}

    def wait(self, ev):
        if ev is None:
            return
        key, val = ev[0], ev[1]
        if self.seen.get(id(key), 0) >= val:
            return
        self.seen[id(key)] = val
        self.h.wait_ge(key, val)
        self.trk.nwaits += 1


class Tracker:
    def __init__(self, nc, engines):
        self.nc = nc
        self.E = {n: _Eng(self, n, h, s) for n, (h, s) in engines.items()}
        self.nwaits = 0
        self.nops = 0
        self.dma_sems = {}
        self.dma_rr = {}
        self.rec = None
        self.t_eng = {}
        self.strict = True

    def add_dma_sems(self, qname, sems):
        self.dma_sems[qname] = [[s, 0] for s in sems]
        self.dma_rr[qname] = 0

    def _deps(self, eng, reads, writes):
        for r in reads:
            if r.w is not None:
                eng.wait(r.w)
        for w in writes:
            if w.w is not None and (self.strict or w.w[0] is not eng.sem):
                eng.wait(w.w)
            for rd in w.r:
                if self.strict or rd[0] is not eng.sem:
                    eng.wait(rd)

    def _commit(self, ev, reads, writes):
        for r in reads:
            r.r.append(ev)
            if len(r.r) > 48:
                best = {}
                for k, v in r.r:
                    if id(k) not in best or best[id(k)][1] < v:
                        best[id(k)] = (k, v)
                r.r = list(best.values())
        for w in writes:
            w.w = ev
            w.r = []

    LAT = 0.22
    DEF_COST = {"act": 0.42, "dve": 0.38, "pool": 0.68, "pe": 0.2, "sp": 0.05}

    def _est(self, ename, reads, writes):
        t = self.t_eng.get(ename, 0.0)
        for r in reads:
            t = max(t, r.tw + self.LAT)
        for w in writes:
            t = max(t, w.tw + self.LAT, w.tr + self.LAT)
        return t

    def _advance(self, ename, reads, writes, cost, async_cost=None):
        st = self._est(ename, reads, writes)
        fin = st + cost
        self.t_eng[ename] = fin
        done = fin if async_cost is None else st + async_cost
        for r in reads:
            r.tr = max(r.tr, done)
        for w in writes:
            w.tw = done
            w.tr = 0.0

    def _cost_of(self, kind, args):
        if kind == "op":
            return args[4] if args[4] is not None else self.DEF_COST[args[0]]
        if kind == "group":
            return args[4] if args[4] is not None else self.DEF_COST["pe"] * len(args[1])
        return 0.05

    def capture(self, fn):
        prev = self.rec
        self.rec = []
        fn()
        out = self.rec
        self.rec = prev
        return out

    def play(self, *lists):
        lists = [l for l in lists if l]
        if _DBG.get("seq"):
            for l in lists:
                for kind, args in l:
                    getattr(self, kind)(*args)
            return
        pos = [0] * len(lists)
        while True:
            best, bi = None, -1
            for i, l in enumerate(lists):
                if pos[i] < len(l):
                    kind, args = l[pos[i]]
                    ename = args[0]
                    if kind == "dma":
                        st = self._est(ename, args[3], args[4])
                    else:
                        st = self._est(ename, args[2], args[3])
                    if best is None or st < best - 1e-9:
                        best, bi = st, i
            if bi < 0:
                break
            kind, args = lists[bi][pos[bi]]
            pos[bi] += 1
            getattr(self, kind)(*args)

    def op(self, ename, fn, reads=(), writes=(), cost=None):
        if self.rec is not None:
            self.rec.append(("op", (ename, fn, list(reads), list(writes), cost)))
            return
        self._advance(ename, reads, writes, cost if cost is not None else self.DEF_COST[ename])
        eng = self.E[ename]
        self._deps(eng, reads, writes)
        ins = fn(eng.h)
        self.nops += 1
        eng.count += 1
        ins.then_inc(eng.sem, 1)
        self._commit((eng.sem, eng.count), reads, writes)

    def group(self, ename, fns, reads=(), writes=(), cost=None):
        if self.rec is not None:
            self.rec.append(("group", (ename, fns, list(reads), list(writes), cost)))
            return
        self._advance(ename, reads, writes, cost if cost is not None else self.DEF_COST["pe"] * len(fns))
        eng = self.E[ename]
        self._deps(eng, reads, writes)
        ins = None
        for f in fns:
            ins = f(eng.h)
            self.nops += 1
        eng.count += 1
        ins.then_inc(eng.sem, 1)
        self._commit((eng.sem, eng.count), reads, writes)

    def dma(self, qname, out, in_, reads=(), writes=()):
        if self.rec is not None:
            self.rec.append(("dma", (qname, out, in_, list(reads), list(writes))))
            return None
        self._advance(qname, reads, writes, 0.05, async_cost=3.0)
        eng = self.E[qname]
        pool = self.dma_sems[qname]
        i = self.dma_rr[qname]
        self.dma_rr[qname] = (i + 1) % len(pool)
        slot = pool[i]
        sem = slot[0]
        if slot[1] > 0:
            eng.wait((sem, 16 * slot[1]))
        self._deps(eng, reads, writes)
        slot[1] += 1
        eng.h.dma_start(out=out, in_=in_).then_inc(sem, 16)
        self.nops += 1
        ev = (sem, 16 * slot[1])
        self._commit(ev, reads, writes)
        return ev

    def barrier(self):
        tmax = max(self.t_eng.values()) if self.t_eng else 0.0
        for k in self.t_eng:
            self.t_eng[k] = tmax
        evs = [(e.sem, e.count) for e in self.E.values() if e.count > 0]
        for pool in self.dma_sems.values():
            for s, uses in pool:
                if uses > 0:
                    evs.append((s, 16 * uses))
        for e in self.E.values():
            for ev in evs:
                if ev[0] is not e.sem:
                    e.wait(ev)


def _col(v):
    return np.ascontiguousarray(np.asarray(v, np.float32).reshape(-1, 128).T)


VEC = dict(c=0, cctx=8, bmod0=16, bmod1=64, n1g0=112, n2g0=120, n1g1=128, n2g1=136,
           lbF0=144, lbF1=152, lbB0=160, lbB1=168, pscale=176, fing=184, gnorm=192, rb=193)
NV = 256

CST = dict(ident=0, ones=128, scanmask=256, triF=512, triB=576)
NCST = 640


def _pool_plan():
    plan = []
    for w in POOL_WINDOWS:
        reach = w // 2
        dt = (reach + 1) // 2 + (0 if reach % 2 == 0 else 0)
        dt = (reach + 1) // 2 if reach > 1 else 1
        plan.append((-dt, dt, dt))
    return plan


def _pool_mats(half):
    mats = []
    index = {}
    plan = _pool_plan()
    cache = {}
    for wi, w in enumerate(POOL_WINDOWS):
        dmin, dmax, nsp = plan[wi]
        for ti in range(OWN // 128):
            special = ti < nsp
            for d in range(dmin, dmax + 1):
                sj = ti + d
                if sj < 0 or sj >= EXT // 128:
                    continue
                key = (wi, ti if special else -1, d)
                if key in cache:
                    index[(wi, ti, d)] = cache[key]
                    continue
                M = np.zeros((128, 128), np.float64)
                for tl in range(128):
                    f = ti * 128 + tl
                    t = f if half == 0 else L - 1 - f
                    r, c = divmod(t, 64)
                    r0, r1 = max(r - w // 2, 0), min(r - w // 2 + w, 64)
                    c0, c1 = max(c - w // 2, 0), min(c - w // 2 + w, 64)
                    inv = 1.0 / ((r1 - r0) * (c1 - c0))
                    for sl in range(128):
                        fs = sj * 128 + sl
                        ts = fs if half == 0 else L - 1 - fs
                        rs, cs = divmod(ts, 64)
                        if r0 <= rs < r1 and c0 <= cs < c1:
                            M[sl, tl] += inv
                        if ts == t:
                            M[sl, tl] -= 1.0
                cache[key] = len(mats)
                index[(wi, ti, d)] = len(mats)
                mats.append(M.astype(np.float32))
    return np.stack(mats), index


_POOL_CACHE = {}


def _pool_mats_cached(half):
    if half not in _POOL_CACHE:
        _POOL_CACHE[half] = _pool_mats(half)
    return _POOL_CACHE[half]


def _consts():
    c = np.zeros((128, NCST), np.float32)
    c[:, CST["ident"]:CST["ident"] + 128] = np.eye(128, dtype=np.float32)
    c[:, CST["ones"]:CST["ones"] + 128] = 1.0
    m = np.ones((128, TB), np.float32)
    m[:, ::64] = 0.0
    c[:, CST["scanmask"]:CST["scanmask"] + TB] = m
    s = np.arange(64)[:, None]
    cc = np.arange(64)[None, :]
    tf = (s <= cc).astype(np.float32)
    tb = (s >= cc).astype(np.float32)
    c[0:64, CST["triF"]:CST["triF"] + 64] = tf
    c[64:128, CST["triF"]:CST["triF"] + 64] = tf
    c[0:64, CST["triB"]:CST["triB"] + 64] = tb
    c[64:128, CST["triB"]:CST["triB"] + 64] = tb
    sel = np.zeros((16, NE, 128), np.float32)
    for e in range(NE):
        sel[e, e, :] = 1.0
    return c, sel.reshape(16, NE * 128)


def _shared_weights(inp):
    f = lambda a: np.asarray(a, np.float32)
    out = {}
    wm = f(inp["w_mod"])
    out["w_mod_t"] = np.ascontiguousarray(
        wm.reshape(2, 8, 128, 12, 512).transpose(0, 3, 2, 1, 4))
    wg = f(inp["moe_w_gate"]).reshape(2, NE, 8, 128, 256)
    wu = f(inp["moe_w_up"]).reshape(2, NE, 8, 128, 256)
    wgu = np.concatenate([wg, wu], axis=-1)
    out["wgu_t"] = np.ascontiguousarray(wgu.transpose(0, 1, 3, 2, 4))
    wd = f(inp["moe_w_down"]).reshape(2, NE, 2, 128, 1024)
    out["wd_t"] = np.ascontiguousarray(wd.transpose(0, 1, 3, 2, 4))
    out["w_out_t"] = np.ascontiguousarray(
        f(inp["hg_w_out"][0]).reshape(4, 2, 128, 1024).transpose(0, 2, 1, 3))
    out["pw_in_t"] = np.ascontiguousarray(f(inp["pool_w_in"][0]).reshape(8, 128, 1024).transpose(1, 0, 2))
    out["pw_out_t"] = np.ascontiguousarray(f(inp["pool_w_out"][0]).reshape(8, 128, 1024).transpose(1, 0, 2))
    out["pw_grp_t"] = np.ascontiguousarray(
        f(inp["pool_w_grp"][0]).reshape(4, 2, 128, 256).transpose(2, 0, 1, 3))
    out["router_t"] = np.ascontiguousarray(f(inp["router_w"]).reshape(8, 128, NE).transpose(1, 0, 2))
    cst, sel = _consts()
    out["cst"] = cst
    out["sel"] = sel
    return out


def _w_in_tiled(w_in, half):
    q, v, zf, zb, g = [w_in[:, i * D:(i + 1) * D] for i in range(5)]
    zF, zB = (zf, zb) if half == 0 else (zb, zf)
    groups = []
    for gi in range(4):
        cols = []
        for kind in (q, zF, zB, g, v):
            for hh in range(2):
                h = 2 * gi + hh
                cols.append(kind[:, h * 128:(h + 1) * 128])
        Wg = np.concatenate(cols, axis=1)
        groups.append(Wg.reshape(8, 128, 1280).transpose(1, 0, 2))
    return np.ascontiguousarray(np.stack(groups))


def _core_inputs(inp, shared, w_in_by_half, core):
    b, half = core // 2, core % 2
    f = lambda a: np.asarray(a, np.float32)
    x = f(inp["x"][b])
    ctx = f(inp["ctx"][b])
    if half == 1:
        x = x[::-1]
        ctx = ctx[::-1]
    m = dict(shared)
    m["xT"] = np.ascontiguousarray(x.T)
    m["ctxT"] = np.ascontiguousarray(ctx.T)
    vec = np.zeros((128, NV), np.float32)
    vec[:, VEC["c"]:VEC["c"] + 8] = _col(inp["c"][b])
    vec[:, VEC["cctx"]:VEC["cctx"] + 8] = _col(inp["c_ctx"])
    for l in range(2):
        vec[:, VEC[f"bmod{l}"]:VEC[f"bmod{l}"] + 48] = _col(inp["b_mod"][l])
        vec[:, VEC[f"n1g{l}"]:VEC[f"n1g{l}"] + 8] = _col(inp["norm1_g"][l])
        vec[:, VEC[f"n2g{l}"]:VEC[f"n2g{l}"] + 8] = _col(inp["norm2_g"][l])
    lbF, lbB = (inp["hg_lb_fwd"], inp["hg_lb_bwd"]) if half == 0 else (inp["hg_lb_bwd"], inp["hg_lb_fwd"])
    vec[:, VEC["lbF0"]:VEC["lbF0"] + 8] = _col(lbF[0])
    vec[:, VEC["lbF1"]:VEC["lbF1"] + 8] = _col(lbF[1])
    vec[:, VEC["lbB0"]:VEC["lbB0"] + 8] = _col(lbB[0])
    vec[:, VEC["lbB1"]:VEC["lbB1"] + 8] = _col(lbB[1])
    vec[:, VEC["pscale"]:VEC["pscale"] + 8] = _col(inp["pool_scale"][0])
    vec[:, VEC["fing"]:VEC["fing"] + 8] = _col(inp["final_g"])
    vec[:, VEC["gnorm"]] = f(inp["hg_gnorm"][0])
    vec[:, VEC["rb"]:VEC["rb"] + NE] = f(inp["router_b"])[None, :]
    m["vec"] = vec
    m["w_in_t"] = w_in_by_half[half]
    m["pmat"] = _pool_mats_cached(half)[0]
    return m


_DBG = {"stage": None}


class _Prog:
    def __init__(self, stage=None):
        self.stage = stage
        self.nc = bass.Bass("TRN2", target_bir_lowering=False)
        self.es = ExitStack()
        self.nsb = 0

    def sb(self, name, shape, dt=F32, es=None):
        return (es or self.es).enter_context(self.nc.sbuf_tensor("sb_" + name, list(shape), dt))

    def ps(self, name, shape, dt=F32, es=None):
        return (es or self.es).enter_context(self.nc.psum_tensor("pp_" + name, list(shape), dt))

    def sem(self, name):
        return self.es.enter_context(self.nc.semaphore(name))

    def dram_in(self, name, shape, dt=F32):
        return self.nc.dram_tensor(name, list(shape), dt, kind="ExternalInput").ap()

    def dram_out(self, name, shape, dt=F32):
        return self.nc.dram_tensor(name, list(shape), dt, kind="ExternalOutput").ap()


def build_program(stage=None, n_pmat=1):
    P = _Prog(stage)
    nc = P.nc
    with P.es:
        _build(P, n_pmat)
    return nc


def _build(P, n_pmat):
    nc = P.nc
    stage = P.stage
    xT_d = P.dram_in("xT", [D, L])
    ctxT_d = P.dram_in("ctxT", [D, CTX])
    vec_d = P.dram_in("vec", [128, NV])
    cst_d = P.dram_in("cst", [128, NCST])
    sel_d = P.dram_in("sel", [16, NE * 128])
    w_mod_d = P.dram_in("w_mod_t", [2, 12, 128, 8, 512])
    w_in_d = P.dram_in("w_in_t", [4, 128, 8, 1280])
    w_out_d = P.dram_in("w_out_t", [4, 128, 2, 1024])
    wgu_d = P.dram_in("wgu_t", [2, NE, 128, 8, 512])
    wd_d = P.dram_in("wd_t", [2, NE, 128, 2, 1024])
    pw_in_d = P.dram_in("pw_in_t", [128, 8, 1024])
    pw_out_d = P.dram_in("pw_out_t", [128, 8, 1024])
    pw_grp_d = P.dram_in("pw_grp_t", [128, 4, 2, 256])
    router_d = P.dram_in("router_t", [128, 8, NE])
    pmat_d = P.dram_in("pmat", [n_pmat, 128, 128])
    out_d = P.dram_out("outT", [D, OWN])
    dbg_d = P.dram_out("dbg", [D, EXT]) if stage else None

    engs = {"pe": (nc.tensor, P.sem("s_pe")), "act": (nc.scalar, P.sem("s_act")),
            "dve": (nc.vector, P.sem("s_dve")), "pool": (nc.gpsimd, P.sem("s_pool")),
            "sp": (nc.sync, P.sem("s_sp"))}
    T = Tracker(nc, engs)
    T.add_dma_sems("sp", [P.sem(f"dsp{i}") for i in range(8)])
    T.add_dma_sems("pool", [P.sem(f"dpl{i}") for i in range(8)])

    XT = P.sb("XT", [128, 8, EXT], F32)
    XTr = [[Res(f"XT{j}_{dc}") for dc in range(8)] for j in range(NB_EXT)]
    XTall = [r for row in XTr for r in row]
    vec = P.sb("vec", [128, NV], F32)
    Rvec = Res("vec")
    cst32 = P.sb("cst32", [128, NCST], F32)
    Rcst = Res("cst")
    ident = P.sb("ident", [128, 128], BF16)
    ones = P.sb("ones", [128, 128], BF16)
    Rid = Res("ident")
    modv = P.sb("modv", [128, 2, 48, 2], F32)
    Rmod = Res("modv")
    drv = P.sb("drv", [128, 192], F32)
    Rdrv = Res("drv")
    PSB = [P.ps(f"psb{i}", [128, 512], F32) for i in range(7)]
    RPS = [Res(f"ps{i}") for i in range(7)]
    PSH = P.ps("psh", [128, 1024], BF16)
    _rpsh = Res("psh")
    RPSH = [_rpsh, _rpsh]

    def vcol(name, n=8, off=0):
        a = VEC[name] + off
        return vec[:, a:a + n]

    T.dma("sp", vec[:], vec_d[:, :], writes=[Rvec])
    T.dma("sp", cst32[:], cst_d[:, :], writes=[Rcst])
    T.dma("pool", ident[:], cst_d[:, CST["ident"]:CST["ident"] + 128], writes=[Rid])
    T.dma("pool", ones[:], cst_d[:, CST["ones"]:CST["ones"] + 128], writes=[Rid])
    for dc in range(8):
        T.dma("sp", XT[:, dc, :], xT_d[dc * 128:(dc + 1) * 128, 0:EXT], writes=[XTr[j][dc] for j in range(NB_EXT)])

    scanmask = cst32[:, CST["scanmask"]:CST["scanmask"] + TB]
    triF = cst32[:, CST["triF"]:CST["triF"] + 64]
    triB = cst32[:, CST["triB"]:CST["triB"] + 64]

    cs16 = P.sb("cs16", [128, 8, 2], BF16)
    Rcs = Res("cs")
    mps = PSB[0][:, 0:96].rearrange("p (n c) -> p n c", c=2)

    def mod_piece(l, j, wmp, Rwmp):
        buf = wmp[j % 2]
        T.dma("pool", buf[:], w_mod_d[l, j], writes=[Rwmp[j % 2]])
        fns = []
        for cg in range(4):
            n = j * 4 + cg
            for dc in range(8):
                fns.append(lambda e, n=n, cg=cg, dc=dc: e.matmul(
                    mps[:, n, :], buf[:, dc, cg * 128:(cg + 1) * 128], cs16[:, dc, :],
                    start=(dc == 0), stop=(dc == 7)))
        T.group("pe", fns, reads=[Rwmp[j % 2], Rcs], writes=[RPS[0]])

    def mv(l, comp, which=0):
        return modv[:, l, comp * 8:(comp + 1) * 8, which]

    def mod_finish(l):
        bm = vcol(f"bmod{l}", 48)
        T.op("dve", lambda e: e.tensor_tensor(
            out=modv[:, l], in0=mps, in1=bm.unsqueeze(2).to_broadcast([128, 48, 2]), op=ALU.add),
            reads=[RPS[0], Rvec], writes=[Rmod])
        base = l * 64
        n1 = vcol(f"n1g{l}")
        n2 = vcol(f"n2g{l}")

        def gs(dst, sc, ng):
            T.op("dve", lambda e: e.scalar_tensor_tensor(out=dst, in0=sc, scalar=1.0, in1=ng,
                                                          op0=ALU.add, op1=ALU.mult),
                 reads=[Rmod, Rvec], writes=[Rdrv])
        gs(drv[:, base + 0:base + 8], mv(l, 1), n1)
        gs(drv[:, base + 24:base + 32], mv(l, 4), n2)
        gs(drv[:, base + 48:base + 56], mv(l, 1, 1), n1)
        for dst, comp, which in ((8, 0, 0), (16, 2, 0), (32, 3, 0), (40, 5, 0), (56, 0, 1)):
            T.op("dve", lambda e, dst=dst, comp=comp, which=which: e.tensor_copy(
                out=drv[:, base + dst:base + dst + 8], in_=mv(l, comp, which)),
                reads=[Rmod], writes=[Rdrv])

    with ExitStack() as es0:
        cs32 = P.sb("cs32", [128, 16], F32, es0)
        wmp = [P.sb(f"wmp{i}", [128, 8, 512], BF16, es0) for i in range(2)]
        Rwmp = [Res("wmp0"), Res("wmp1")]
        T.op("act", lambda e: e.activation(out=cs32[:], in_=vec[:, 0:16], func=AF.Sigmoid),
             reads=[Rvec], writes=[Rcs])
        T.op("dve", lambda e: e.tensor_tensor(out=cs32[:], in0=cs32[:], in1=vec[:, 0:16], op=ALU.mult),
             reads=[Rcs, Rvec], writes=[Rcs])
        T.op("dve", lambda e: e.tensor_copy(out=cs16[:, :, 0], in_=cs32[:, 0:8]), reads=[Rcs], writes=[Rcs])
        T.op("dve", lambda e: e.tensor_copy(out=cs16[:, :, 1], in_=cs32[:, 8:16]), reads=[Rcs], writes=[Rcs])
        for j in range(12):
            mod_piece(0, j, wmp, Rwmp)
        mod_finish(0)
        for nm, off in (("F", 128), ("B", 144)):
            l0 = vcol(f"lb{nm}0")
            l1 = vcol(f"lb{nm}1")
            T.op("dve", lambda e, off=off, l0=l0, l1=l1: e.tensor_tensor(
                out=drv[:, off + 8:off + 16], in0=l1, in1=l0, op=ALU.subtract), reads=[Rvec], writes=[Rdrv])
            T.op("act", lambda e, off=off: e.activation(out=drv[:, off:off + 8], in_=drv[:, off + 8:off + 16],
                                                        func=AF.Sigmoid), reads=[Rdrv], writes=[Rdrv])
            T.op("dve", lambda e, off=off: e.tensor_scalar(out=drv[:, off + 8:off + 16], in0=drv[:, off:off + 8],
                                                           scalar1=-1.0, scalar2=None, op0=ALU.mult),
                 reads=[Rdrv], writes=[Rdrv])
            xo = 160 if nm == "F" else 168
            T.op("act", lambda e, off=off, xo=xo: e.activation(out=drv[:, xo:xo + 8], in_=drv[:, off:off + 8], func=AF.Ln),
                 reads=[Rdrv], writes=[Rdrv])
            T.op("dve", lambda e, off=off, xo=xo: e.tensor_scalar(out=drv[:, xo + 16:xo + 24], in0=drv[:, off:off + 8],
                                                                  scalar1=-1.0, scalar2=1.0, op0=ALU.mult, op1=ALU.add),
                 reads=[Rdrv], writes=[Rdrv])
        T.barrier()

    def dcol(l, off, dc):
        a = l * 64 + off + dc
        return drv[:, a:a + 1]

    T32ALT = {}
    def modulate(es, src, Rsrc, n, gs_off, sh_off, l, dst16, Rdst, dst32=None, Rdst32=None, tmp=None):
        sq, Rsq, rstd, Rrstd, t32, Rt32, bank, Rbank = tmp
        fns = []
        for dc in range(8):
            if dc % 4 == 3:
                T.op("act", lambda e, dc=dc: e.activation(out=sq[:, dc, 0:n], in_=src(dc), func=AF.Square),
                     reads=[Rsrc[dc] if isinstance(Rsrc, list) else Rsrc], writes=[Rsq])
            else:
                T.op("pool", lambda e, dc=dc: e.tensor_tensor(out=sq[:, dc, 0:n], in0=src(dc), in1=src(dc), op=ALU.mult),
                     reads=[Rsrc[dc] if isinstance(Rsrc, list) else Rsrc], writes=[Rsq])
        for dc in range(8):
            fns.append(lambda e, dc=dc: e.matmul(bank[:, 0:n], ones[:], sq[:, dc, 0:n],
                                                  start=(dc == 0), stop=(dc == 7)))
        T.group("pe", fns, reads=[Rsq, Rid], writes=[Rbank])
        T.op("act", lambda e: e.activation(out=rstd[:, 0:n], in_=bank[:, 0:n], func=AF.Ln,
                                           scale=1.0 / D, bias=EPS), reads=[Rbank], writes=[Rrstd])
        T.op("act", lambda e: e.activation(out=rstd[:, 0:n], in_=rstd[:, 0:n], func=AF.Exp, scale=-0.5),
             reads=[Rrstd], writes=[Rrstd])
        alt = T32ALT.get(id(t32))
        for dc in range(8):
            tb, Rtb = (t32, Rt32) if (alt is None or dc % 2 == 0) else alt
            T.op("dve", lambda e, dc=dc, tb=tb: e.tensor_tensor(out=tb[:, 0:n], in0=src(dc), in1=rstd[:, 0:n],
                                                                 op=ALU.mult),
                 reads=[Rsrc[dc] if isinstance(Rsrc, list) else Rsrc, Rrstd], writes=[Rtb])
            if dst32 is not None:
                Rd32 = Rdst32[dc] if isinstance(Rdst32, list) else Rdst32
                T.op("act", lambda e, dc=dc, tb=tb: e.activation(
                    out=dst32(dc), in_=tb[:, 0:n], func=AF.Identity,
                    scale=dcol(l, gs_off, dc), bias=dcol(l, sh_off, dc)), reads=[Rtb, Rdrv], writes=[Rd32])
                T.op("pool", lambda e, dc=dc: e.tensor_copy(out=dst16(dc), in_=dst32(dc)),
                     reads=[Rd32], writes=[Rdst])
            else:
                T.op("act", lambda e, dc=dc, tb=tb: e.activation(
                    out=dst16(dc), in_=tb[:, 0:n], func=AF.Identity,
                    scale=dcol(l, gs_off, dc), bias=dcol(l, sh_off, dc)), reads=[Rtb, Rdrv], writes=[Rdst])

    with ExitStack() as es1:
        hown_d = nc.dram_tensor("hown", [NB_EXT, 128, 8 * TB], BF16, kind="Internal").ap()
        HTr = [Res(f"HT{j}") for j in range(NB_EXT)]
        sq = P.sb("sq", [128, 8, TB], BF16, es1)
        Rsq = Res("sq")
        rstd = P.sb("rstd", [128, TB], F32, es1)
        Rrstd = Res("rstd")
        t32 = P.sb("t32", [128, TB], F32, es1)
        Rt32 = Res("t32")
        T32ALT[id(t32)] = (P.sb("t32_b", [128, TB], F32, es1), Res("t32_b"))
        mtmp = (sq, Rsq, rstd, Rrstd, t32, Rt32, PSB[0], RPS[0])
        NSO = 1 + (L - EXT) // TB
        hso_d = nc.dram_tensor("hso", [NSO, 128, 8 * TB], BF16, kind="Internal").ap()
        Rhso = [Res(f"hso{i}") for i in range(NSO)]
        hp2 = [P.sb(f"hp{i}", [128, 8, TB], BF16, es1) for i in range(2)]
        Rhp2 = [Res("hp0"), Res("hp1")]
        with ExitStack() as esi:
            NSET = 3
            sets = []
            for k in range(NSET):
                hpk = hp2[k] if k < 2 else P.sb("hp_x", [128, 8, TB], BF16, esi)
                Rhpk = Rhp2[k] if k < 2 else Res("hp_x")
                if k == 0:
                    sqk, Rsqk, rstdk, Rrstdk, t32k, Rt32k = sq, Rsq, rstd, Rrstd, t32, Rt32
                else:
                    sqk = P.sb(f"sq_i{k}", [128, 8, TB], BF16, esi)
                    rstdk = P.sb(f"rstd_i{k}", [128, TB], F32, esi)
                    t32k = P.sb(f"t32_i{k}", [128, TB], F32, esi)
                    Rsqk, Rrstdk, Rt32k = Res(f"sq_i{k}"), Res(f"rstd_i{k}"), Res(f"t32_i{k}")
                    T32ALT[id(t32k)] = (P.sb(f"t32_i{k}b", [128, TB], F32, esi), Res(f"t32_i{k}b"))
                xstk = P.sb(f"xst{k}", [128, 8, 128], F32, esi)
                sets.append(dict(hp=hpk, Rhp=Rhpk, xst=xstk, Rxst=Res(f"xst{k}"),
                                 tmp=(sqk, Rsqk, rstdk, Rrstdk, t32k, Rt32k, PSB[k], RPS[k])))

            def stage_task(k, kind, idx):
                st = sets[k]
                hp, Rhp = st["hp"], st["Rhp"]
                if kind == "so":
                    if idx == 0:
                        src_d, t0, gs_off, sh_off = ctxT_d, 0, 48, 56
                    else:
                        src_d, t0, gs_off, sh_off = xT_d, EXT + (idx - 1) * TB, 0, 8
                    xst, Rxst = st["xst"], st["Rxst"]
                    for hf in range(TB // 128):
                        for dc in range(8):
                            T.dma("sp", xst[:, dc, :],
                                  src_d[dc * 128:(dc + 1) * 128, t0 + hf * 128:t0 + (hf + 1) * 128], writes=[Rxst])
                        modulate(esi, lambda dc: xst[:, dc, :], Rxst, 128, gs_off, sh_off, 0,
                                 lambda dc, hf=hf: hp[:, dc, hf * 128:(hf + 1) * 128], Rhp, tmp=st["tmp"])
                    T.dma("sp", hso_d[idx], hp[:].rearrange("p a b -> p (a b)"), reads=[Rhp], writes=[Rhso[idx]])
                else:
                    sl = slice(idx * TB, (idx + 1) * TB)
                    modulate(esi, lambda dc: XT[:, dc, sl], XTr[idx], TB, 0, 8, 0,
                             lambda dc: hp[:, dc, :], Rhp, tmp=st["tmp"])
                    T.dma("sp", hown_d[idx], hp[:].rearrange("p a b -> p (a b)"), reads=[Rhp], writes=[HTr[idx]])

            tasks = [("own", j) for j in range(NB_EXT)] + [("so", bi) for bi in range(NSO)]
            for r in range(0, len(tasks), NSET):
                T.play(*[T.capture(lambda k=k, t=t: stage_task(k, t[0], t[1]))
                         for k, t in enumerate(tasks[r:r + NSET])])
            T.barrier()

        Wgb = [P.sb("Wg0", [128, 8, 1280], BF16, es1), None]
        RWgb = [Res("Wg0"), Res("Wg1")]
        cur = [0]

        class _WgView:
            def __getitem__(self, _):
                return Wgb[cur[0]]
        Wg = _WgView()

        Wo = P.sb("Wo", [128, 2, 1024], BF16, es1)
        RWo = Res("Wo")
        qd = [[P.sb(f"qd{h}{d}", [128, TB], BF16, es1) for d in range(2)] for h in range(2)]
        kd = [[P.sb(f"kd{h}{d}", [128, TB], BF16, es1) for d in range(2)] for h in range(2)]
        qc = [[P.sb(f"qc{h}{d}", [128, TB], F32, es1) for d in range(2)] for h in range(2)]
        klT = [[P.sb(f"klT{h}{d}", [128, TB], BF16, es1) for d in range(2)] for h in range(2)]
        Rops = [[Res(f"ops{h}{d}") for d in range(2)] for h in range(2)]
        RklT = [[Res(f"klT{h}{d}") for d in range(2)] for h in range(2)]
        decs = P.sb("decs", [128, 2, 2, TB // 64], F32, es1)
        Rdec = [[Res(f"dec{h}{d}") for d in range(2)] for h in range(2)]
        qT = [P.sb(f"qT{h}", [128, TB], F32, es1) for h in range(2)]
        RqT = [Res("qT0"), Res("qT1")]
        sg2 = [P.sb(f"sg{i}", [128, 2, TB], F32, es1) for i in range(3)]
        Rsg2 = [[Res(f"sg{i}{h}") for h in range(2)] for i in range(3)]
        tmpb = [[P.sb(f"gt{h}_{i}", [128, TB], F32, es1) for i in range(7)] for h in range(2)]
        tmpr = [[Res(f"gt{h}_{i}") for i in range(7)] for h in range(2)]
        kl_tm = [P.sb(f"kl_tm{d}", [128, TB // 128, 2, 128], BF16, es1) for d in range(2)]
        Rkl_tm = [Res("kl_tm0"), Res("kl_tm1")]
        v_tm = [P.sb(f"v_tm{i}", [128, TB // 128, 256], BF16, es1) for i in range(2)]
        Rv_tm = [Res("v_tm0"), Res("v_tm1")]
        sTm = [P.sb(f"sTm{i}", [128, 2, 128], BF16, es1) for i in range(2)]
        RsTm = [Res("sTm0"), Res("sTm1")]
        o_sb2 = [P.sb(f"o_sb{i}", [128, 2, TB], F32, es1) for i in range(2)]
        Ro_sb2 = [Res("o_sb0"), Res("o_sb1")]
        yT = P.sb("yT", [128, 2, TB], BF16, es1)
        RyT = Res("yT")
        S = P.sb("S", [128, 2, 2, 128], F32, es1)
        RS = [Res("S_F"), Res("S_B")]
        RS16 = [Res("S16_F"), Res("S16_B")]
        o_b = P.sb("o_b", [128, 2, EXT], BF16, es1)
        Ro_b = [Res(f"o_b{j}") for j in range(NB_EXT)]
        for i in range(2):
            T.op("pool", lambda e, i=i: e.memset(sTm[i][:], 0.0), writes=[RsTm[i]])

        B_PROJ = [1, 2]
        B_SC = 3
        B_KV = 4
        B_O = 5
        B_Y = 6
        proj_rr = [0]

        def next_proj_bank():
            b = B_PROJ[proj_rr[0] % 2]
            proj_rr[0] += 1
            return b

        OML = {0: 128, 1: 144}

        def gate_elementwise(gi, hh, d, hsrc, Rh, n, main, slot=None):
            slot = d if slot is None else slot
            sT, lf, cum, dA, ndL, ee0_, ee1_ = tmpb[hh]
            RsT, Rlf, Rcum, RdA, RndL, Ree0_, Ree1_ = tmpr[hh]
            ee = [ee0_, ee1_]
            Ree = [Ree0_, Ree1_]
            h = 2 * gi + hh
            zcol = (1 + d) * 256 + hh * 128
            pb = 1 + hh
            bank = PSB[pb]
            T.group("pe", [lambda e, dc=dc: e.matmul(bank[:, 0:n], Wg[0][:, dc, zcol:zcol + 128], hsrc(dc),
                                                      start=(dc == 0), stop=(dc == 7)) for dc in range(8)],
                    reads=[RWgb[cur[0]], Rh], writes=[RPS[pb]])
            lnoml = drv[:, 160 + 8 * d + h:160 + 8 * d + h + 1]
            lbc = drv[:, 176 + 8 * d + h:176 + 8 * d + h + 1]
            nch = n // 64
            l1 = ee[1]
            Rl1 = Ree[1]
            T.op("act", lambda e: e.activation(out=sT[:, 0:n], in_=bank[:, 0:n], func=AF.Exp),
                 reads=[RPS[pb]], writes=[RsT])
            T.op("act", lambda e: e.activation(out=l1[:, 0:n], in_=sT[:, 0:n], func=AF.Ln, bias=1.0),
                 reads=[RsT], writes=[Rl1])
            T.op("act", lambda e: e.activation(out=lf[:, 0:n], in_=sT[:, 0:n], func=AF.Ln, bias=lbc),
                 reads=[RsT, Rdrv], writes=[Rlf])
            T.op("pool", lambda e: e.tensor_tensor(out=lf[:, 0:n], in0=lf[:, 0:n], in1=l1[:, 0:n], op=ALU.subtract),
                 reads=[Rlf, Rl1], writes=[Rlf])
            T.op("dve", lambda e: e.tensor_tensor_scan(out=cum[:, 0:n], data0=scanmask[:, 0:n], data1=lf[:, 0:n],
                                                        initial=0.0, op0=ALU.mult, op1=ALU.add),
                 reads=[Rlf, Rcst], writes=[Rcum])
            cum3 = cum[:, 0:n].rearrange("p (c t) -> p c t", t=64)
            T.op("act", lambda e: e.activation(out=decs[:, hh, slot, 0:nch], in_=cum3[:, :, 63], func=AF.Exp),
                 reads=[Rcum], writes=[Rdec[hh][slot]])
            T.op("pool", lambda e: e.tensor_tensor(
                out=ndL[:, 0:n].rearrange("p (c t) -> p c t", t=64), in0=cum3,
                in1=cum3[:, :, 63:64].to_broadcast([128, nch, 64]), op=ALU.subtract),
                reads=[Rcum], writes=[RndL])
            if d == 1:
                T.op("pool", lambda e: e.tensor_tensor(out=ndL[:, 0:n], in0=ndL[:, 0:n], in1=lf[:, 0:n],
                                                       op=ALU.subtract), reads=[RndL, Rlf], writes=[RndL])
                T.op("pool", lambda e: e.tensor_tensor(out=cum[:, 0:n], in0=cum[:, 0:n], in1=lf[:, 0:n],
                                                       op=ALU.subtract), reads=[Rcum, Rlf], writes=[Rcum])
            A = cum
            RA = Rcum
            if d == 0:
                T.op("dve", lambda e: e.scalar_tensor_tensor(out=ee[0][:, 0:n], in0=ndL[:, 0:n], scalar=-1.0,
                                                             in1=l1[:, 0:n], op0=ALU.mult, op1=ALU.subtract),
                     reads=[RndL, Rl1], writes=[Ree[0]])
            else:
                T.op("dve", lambda e: e.tensor_tensor(out=ee[0][:, 0:n], in0=A[:, 0:n], in1=l1[:, 0:n], op=ALU.subtract),
                     reads=[RA, Rl1], writes=[Ree[0]])
            T.op("act", lambda e: e.activation(out=klT[hh][slot][:, 0:n], in_=ee[0][:, 0:n], func=AF.Exp, bias=lnoml),
                 reads=[Ree[0], Rdrv], writes=[RklT[hh][slot]])
            if not main:
                return
            refi = 31 if d == 0 else 32
            A3 = A[:, 0:n].rearrange("p (c t) -> p c t", t=64)
            T.op("pool", lambda e: e.tensor_tensor(
                out=dA[:, 0:n].rearrange("p (c t) -> p c t", t=64), in0=A3,
                in1=A3[:, :, refi:refi + 1].to_broadcast([128, nch, 64]), op=ALU.subtract),
                reads=[RA], writes=[RdA])
            sq_, sk_ = (1.0, -1.0) if d == 0 else (-1.0, 1.0)
            T.op("dve", lambda e: e.scalar_tensor_tensor(out=ee[0][:, 0:n], in0=dA[:, 0:n], scalar=sk_,
                                                         in1=l1[:, 0:n], op0=ALU.mult, op1=ALU.subtract),
                 reads=[RdA, Rl1], writes=[Ree[0]])
            T.op("act", lambda e: e.activation(out=kd[hh][slot][:, 0:n], in_=ee[0][:, 0:n], func=AF.Exp, bias=lnoml),
                 reads=[Ree[0], Rdrv], writes=[Rops[hh][slot]])
            T.op("act", lambda e: e.activation(out=sT[:, 0:n], in_=dA[:, 0:n], func=AF.Exp, scale=sq_),
                 reads=[RdA], writes=[RsT])
            T.op("dve", lambda e: e.tensor_tensor(out=qd[hh][slot][:, 0:n], in0=qT[hh][:, 0:n], in1=sT[:, 0:n],
                                                  op=ALU.mult), reads=[RqT[hh], RsT], writes=[Rops[hh][slot]])
            if d == 0:
                T.op("act", lambda e: e.activation(out=ndL[:, 0:n], in_=A[:, 0:n], func=AF.Exp),
                     reads=[RA], writes=[RndL])
            else:
                T.op("act", lambda e: e.activation(out=ndL[:, 0:n], in_=ndL[:, 0:n], func=AF.Exp, scale=-1.0),
                     reads=[RndL], writes=[RndL])
            T.op("pool", lambda e: e.tensor_tensor(out=qc[hh][slot][:, 0:n], in0=qT[hh][:, 0:n], in1=ndL[:, 0:n],
                                                   op=ALU.mult), reads=[RqT[hh], RndL], writes=[Rops[hh][slot]])

        def v_project(hsrc, Rh, n, vs=0):
            nt = n // 128
            pb = 1
            bank = PSB[pb]
            fns = []
            for ti in range(nt):
                for dc in range(8):
                    fns.append(lambda e, ti=ti, dc=dc: e.matmul(
                        bank[:, ti * 256:(ti + 1) * 256], hsrc(dc)[:, ti * 128:(ti + 1) * 128],
                        Wg[0][:, dc, 1024:1280], start=(dc == 0), stop=(dc == 7)))
            T.group("pe", fns, reads=[RWgb[cur[0]], Rh], writes=[RPS[pb]])
            T.op("act", lambda e: e.activation(
                out=v_tm[vs][:, 0:nt, :], in_=bank[:, 0:nt * 256].rearrange("p (a b) -> p a b", b=256), func=AF.Copy),
                reads=[RPS[pb]], writes=[Rv_tm[vs]])

        def kl_transpose(d, n):
            nt = n // 128
            half = RPSH[d]
            base = d * 512
            fns = []
            for ti in range(nt):
                for hh in range(2):
                    o0 = base + (ti * 2 + hh) * 128
                    fns.append(lambda e, ti=ti, hh=hh, o0=o0: e.transpose(
                        PSH[:, o0:o0 + 128], klT[hh][d][:, ti * 128:(ti + 1) * 128], ident[:]))
            T.group("pe", fns, reads=[RklT[0][d], RklT[1][d], Rid], writes=[half])
            T.op("act", lambda e: e.activation(
                out=kl_tm[d][:, 0:nt].rearrange("p a h k -> p (a h k)"), in_=PSH[:, base:base + nt * 256],
                func=AF.Copy), reads=[half], writes=[Rkl_tm[d]])

        def kv_bank(chunk):
            if chunk % 2 == 0:
                return PSB[B_KV][:, 0:256].rearrange("p (h v) -> p h v", v=128), RPS[B_KV]
            return PSB[0][:, 256:512].rearrange("p (h v) -> p h v", v=128), RPS[0]

        def kv_mm(ti, j, chunk, slot, vs):
            kvb, Rkv = kv_bank(chunk)
            fns = []
            for hh in range(2):
                fns.append(lambda e, hh=hh: e.matmul(
                    kvb[:, hh, :], kl_tm[slot][64 * j:64 * j + 64, ti, hh, :],
                    v_tm[vs][64 * j:64 * j + 64, ti, hh * 128:(hh + 1) * 128], start=True, stop=True))
            T.group("pe", fns, reads=[Rkl_tm[slot], Rv_tm[vs]], writes=[Rkv])

        def s_update(d, chunk, slot):
            kvb, Rkv = kv_bank(chunk)
            for hh in range(2):
                T.op("dve", lambda e, hh=hh: e.scalar_tensor_tensor(
                    out=S[:, d, hh, :], in0=S[:, d, hh, :], scalar=decs[:, hh, slot, chunk:chunk + 1],
                    in1=kvb[:, hh, :], op0=ALU.mult, op1=ALU.add),
                    reads=[RS[d], Rkv, Rdec[hh][slot]], writes=[RS[d]])

        def state_step(d, ti, j, chunk, slot=None, vs=0):
            slot = d if slot is None else slot
            kv_mm(ti, j, chunk, slot, vs)
            s_update(d, chunk, slot)

        def zero_state(d):
            T.op("pool", lambda e: e.memset(S[:, d].rearrange("p h v -> p (h v)"), 0.0), writes=[RS[d]])

        def so_prep(gi, d, slot, bi_so, part):
            hp, Rhp = hp2[slot], Rhp2[slot]
            hsrc = lambda dc: hp[:, dc, :]
            if part == "pre":
                T.dma("sp", hp[:].rearrange("p a b -> p (a b)"), hso_d[bi_so], reads=[Rhso[bi_so]], writes=[Rhp])
            elif part == "post":
                kl_transpose(slot, TB)
            elif part == 0:
                v_project(hsrc, Rhp, TB, slot)
                gate_elementwise(gi, 0, d, hsrc, Rhp, TB, main=False, slot=slot)
            else:
                gate_elementwise(gi, 1, d, hsrc, Rhp, TB, main=False, slot=slot)

        def so_steps(d, slot):
            nch = TB // 64
            order = range(nch) if d == 0 else range(nch - 1, -1, -1)
            for chunk in order:
                state_step(d, chunk // 2, chunk % 2, chunk, slot=slot, vs=slot)

        def m_prep(gi, j, dd, slot, part):
            hp, Rh = hp2[slot], Rhp2[slot]
            hsrc = lambda dc: hp[:, dc, :]
            n = TB
            sg = sg2[j % 3]
            if part == "pre":
                T.dma("sp", hp[:].rearrange("p a b -> p (a b)"), hown_d[j], reads=[HTr[j]], writes=[Rh])
                return
            if part == "post":
                kl_transpose(slot, n)
                return
            hh = part
            if part == 0:
                v_project(hsrc, Rh, n, slot)
            kinds = [0] if dd == 1 else [0, 3]
            for kind in kinds:
                dstT, Rdst = (qT[hh], RqT[hh]) if kind == 0 else (None, Rsg2[j % 3][hh])
                col = kind * 256 + hh * 128
                pb = 1 + hh
                bank = PSB[pb]
                T.group("pe", [lambda e, dc=dc, col=col, bank=bank: e.matmul(
                    bank[:, 0:n], Wg[0][:, dc, col:col + 128], hsrc(dc), start=(dc == 0), stop=(dc == 7))
                    for dc in range(8)], reads=[RWgb[cur[0]], Rh], writes=[RPS[pb]])
                dst = dstT[:, 0:n] if dstT is not None else sg[:, hh, 0:n]
                T.op("act", lambda e, dst=dst, bank=bank: e.activation(out=dst, in_=bank[:, 0:n],
                                                                        func=AF.Exp, scale=-1.0),
                     reads=[RPS[pb]], writes=[Rdst])
                T.op("act", lambda e, dst=dst: e.activation(out=dst, in_=dst, func=AF.Ln, bias=1.0),
                     reads=[Rdst], writes=[Rdst])
                T.op("act", lambda e, dst=dst: e.activation(out=dst, in_=dst, func=AF.Exp, scale=-1.0),
                     reads=[Rdst], writes=[Rdst])
                T.op("dve", lambda e, dst=dst, bank=bank: e.tensor_tensor(out=dst, in0=dst, in1=bank[:, 0:n],
                                                                           op=ALU.mult),
                     reads=[Rdst, RPS[pb]], writes=[Rdst])
            gate_elementwise(gi, hh, dd, hsrc, Rh, n, main=True, slot=slot)

        def m_loop(gi, j, dd, slot):
            o_sb, Ro_sb = o_sb2[j % 2], Ro_sb2[j % 2]
            n = TB
            nch = n // 64
            vs = slot
            tri = triF if dd == 0 else triB
            order = list(range(nch)) if dd == 0 else list(range(nch - 1, -1, -1))
            done_tiles = set()
            for chunk in order:
                ti, jj = chunk // 2, chunk % 2
                sb_i = ti % 2
                if ti not in done_tiles:
                    done_tiles.add(ti)
                    scb = PSB[B_SC][:, sb_i * 256:(sb_i + 1) * 256].rearrange("p (h c) -> p h c", c=128)
                    T.group("pe", [lambda e, hh=hh, ti=ti, scb=scb: e.matmul(
                        scb[:, hh, :], kd[hh][slot][:, ti * 128:(ti + 1) * 128],
                        qd[hh][slot][:, ti * 128:(ti + 1) * 128], start=True, stop=True) for hh in range(2)],
                        reads=[Rops[0][slot], Rops[1][slot]], writes=[RPS[B_SC]])
                    for blk in range(2):
                        p0 = blk * 64
                        T.op("dve", lambda e, p0=p0, sb_i=sb_i, scb=scb: e.tensor_tensor(
                            out=sTm[sb_i][p0:p0 + 64, :, p0:p0 + 64], in0=scb[p0:p0 + 64, :, p0:p0 + 64],
                            in1=tri[p0:p0 + 64, :].unsqueeze(1).to_broadcast([64, 2, 64]), op=ALU.mult),
                            reads=[RPS[B_SC], Rcst], writes=[RsTm[sb_i]])
                kv_mm(ti, jj, chunk, slot, vs)
                qtr = chunk % 4
                ob = PSB[B_O][:, qtr * 128:(qtr + 1) * 128].rearrange("p (h c) -> p h c", c=64)
                fns = []
                p0 = jj * 64
                for hh in range(2):
                    fns.append(lambda e, hh=hh, p0=p0, ti=ti, sb_i=sb_i, ob=ob: e.matmul(
                        ob[:, hh, :], v_tm[vs][p0:p0 + 64, ti, hh * 128:(hh + 1) * 128],
                        sTm[sb_i][p0:p0 + 64, hh, p0:p0 + 64], start=True, stop=False))
                    fns.append(lambda e, hh=hh, chunk=chunk, ob=ob: e.matmul(
                        ob[:, hh, :], S[:, dd, hh, :], qc[hh][slot][:, chunk * 64:(chunk + 1) * 64],
                        start=False, stop=True))
                T.group("pe", fns, reads=[Rv_tm[vs], RsTm[sb_i], RS[dd], Rops[0][slot], Rops[1][slot]],
                        writes=[RPS[B_O]])
                if dd == 0:
                    osl = o_sb[:, :, chunk * 64:(chunk + 1) * 64]
                    T.op("act", lambda e, osl=osl, ob=ob: e.activation(out=osl, in_=ob, func=AF.Copy),
                         reads=[RPS[B_O]], writes=[Ro_sb])
                else:
                    osl = o_b[:, :, j * TB + chunk * 64:j * TB + (chunk + 1) * 64]
                    T.op("act", lambda e, osl=osl, ob=ob: e.activation(out=osl, in_=ob, func=AF.Copy),
                         reads=[RPS[B_O]], writes=[Ro_b[j]])
                s_update(dd, chunk, slot)

        def m_readout(gi, j):
            o_sb, Ro_sb = o_sb2[j % 2], Ro_sb2[j % 2]
            n = TB
            sl = slice(j * TB, (j + 1) * TB)
            sg = sg2[j % 3]
            T.op("dve", lambda e: e.tensor_tensor(out=o_sb[:], in0=o_sb[:], in1=o_b[:, :, sl], op=ALU.add),
                 reads=[Ro_sb, Ro_b[j]], writes=[Ro_sb])
            for hh in range(2):
                T.op("act", lambda e, hh=hh: e.activation(out=sq[:, hh, 0:n], in_=o_sb[:, hh, :], func=AF.Square),
                     reads=[Ro_sb], writes=[Rsq])
            for hh in range(2):
                bank = PSB[B_Y]
                T.group("pe", [lambda e, hh=hh, bank=bank: e.matmul(bank[:, 0:n], ones[:], sq[:, hh, 0:n],
                                                                     start=True, stop=True)],
                        reads=[Rsq, Rid], writes=[RPS[B_Y]])
                T.op("act", lambda e, bank=bank: e.activation(out=rstd[:, 0:n], in_=bank[:, 0:n], func=AF.Ln,
                                                               scale=1.0 / 128, bias=EPS),
                     reads=[RPS[B_Y]], writes=[Rrstd])
                T.op("act", lambda e: e.activation(out=rstd[:, 0:n], in_=rstd[:, 0:n], func=AF.Exp, scale=-0.5),
                     reads=[Rrstd], writes=[Rrstd])
                T.op("dve", lambda e, hh=hh: e.tensor_tensor(out=t32[:, 0:n], in0=o_sb[:, hh, :], in1=rstd[:, 0:n],
                                                              op=ALU.mult), reads=[Ro_sb, Rrstd], writes=[Rt32])
                T.op("dve", lambda e, hh=hh: e.scalar_tensor_tensor(
                    out=yT[:, hh, :], in0=t32[:, 0:n], scalar=vec[:, VEC["gnorm"]:VEC["gnorm"] + 1],
                    in1=sg[:, hh, 0:n], op0=ALU.mult, op1=ALU.mult), reads=[Rt32, Rsg2[j % 3][hh], Rvec], writes=[RyT])
            for oc in range(8):
                by = B_Y
                bank = PSB[by][:, (oc % 2) * 256:(oc % 2) * 256 + 256]
                T.group("pe", [lambda e, hh=hh, oc=oc, bank=bank: e.matmul(
                    bank[:, 0:n], Wo[:, hh, oc * 128:(oc + 1) * 128], yT[:, hh, :], start=(hh == 0), stop=(hh == 1))
                    for hh in range(2)], reads=[RWo, RyT], writes=[RPS[by]])
                T.op("dve", lambda e, oc=oc, bank=bank: e.scalar_tensor_tensor(
                    out=XT[:, oc, sl], in0=bank[:, 0:n], scalar=dcol(0, 16, oc), in1=XT[:, oc, sl],
                    op0=ALU.mult, op1=ALU.add), reads=[RPS[by], XTr[j][oc], Rdrv], writes=[XTr[j][oc]])

        def zero_state(d):
            T.op("pool", lambda e: e.memset(S[:, d].rearrange("p h v -> p (h v)"), 0.0), writes=[RS[d]])

        cap = T.capture
        esw = ExitStack()
        wmp1 = [P.sb(f"wmpL1_{i}", [128, 8, 512], BF16, esw) for i in range(2)]
        Rwmp1 = [Res("wmpL1_0"), Res("wmpL1_1")]
        T.dma("pool", Wgb[0][:], w_in_d[0], writes=[RWgb[0]])
        for gi in range(4):
            cur[0] = gi % 2
            if gi >= 1 and gi + 1 < 4:
                T.dma("pool", Wgb[(gi + 1) % 2][:], w_in_d[gi + 1], writes=[RWgb[(gi + 1) % 2]])
            T.dma("pool", Wo[:], w_out_d[gi], writes=[RWo])
            for d in (1, 0):
                if gi == 0 and d == 0:
                    T.barrier()
                    esw.close()
                    Wgb[1] = P.sb("Wg1", [128, 8, 1280], BF16, es1)
                    T.dma("pool", Wgb[1][:], w_in_d[1], writes=[RWgb[1]])
                zero_state(d)
                stages = [("so", 0)]
                if d == 1:
                    stages += [("so", 1 + (j - NB_EXT)) for j in range(L // TB - 1, NB_EXT - 1, -1)]
                    stages += [("m", j) for j in range(NB_EXT - 1, -1, -1)]
                else:
                    stages += [("m", j) for j in range(NB_EXT)]

                def prep(i, part):
                    kind, a = stages[i]
                    slot = i % 2
                    if kind == "so":
                        so_prep(gi, d, slot, a, part)
                    else:
                        m_prep(gi, a, d, slot, part)

                def body(i):
                    kind, a = stages[i]
                    slot = i % 2
                    if kind == "so":
                        so_steps(d, slot)
                    else:
                        m_loop(gi, a, d, slot)

                prep(0, "pre")
                T.play(cap(lambda: prep(0, 0)), cap(lambda: prep(0, 1)))
                prep(0, "post")
                pending = None
                for i in range(len(stages)):
                    streams = [cap(lambda: body(i))]
                    if pending is not None:
                        streams.append(cap(lambda: m_readout(gi, pending)))
                    if i + 1 < len(stages):
                        prep(i + 1, "pre")
                        streams += [cap(lambda: prep(i + 1, 0)), cap(lambda: prep(i + 1, 1))]
                        if gi == 0 and d == 1 and i < 12:
                            streams.append(cap(lambda: mod_piece(1, i, wmp1, Rwmp1)))
                    T.play(*streams)
                    if i + 1 < len(stages):
                        prep(i + 1, "post")
                    if gi == 0 and d == 1 and i == 11:
                        mod_finish(1)
                    pending = stages[i][1] if (d == 0 and stages[i][0] == "m") else None
                if pending is not None:
                    m_readout(gi, pending)
        T.barrier()

    if stage == "x1":
        return _finish(P, T, XT, XTr, dbg_d, out_d, debug=True)

    def moe_phase(l, ntok):
        nb = ntok // TB
        MB = 512
        nmb = ntok // MB
        with ExitStack() as es2:
            H2 = P.sb(f"H2_{l}", [128, 8, ntok], BF16, es2)
            RH2 = [Res(f"H2_{j}") for j in range(nmb)]
            h32b = [P.sb(f"h32_{l}_{i}", [128, 8, TB], F32, es2) for i in range(2)]
            Rh32b = [[Res(f"h32_{i}_{dc}") for dc in range(8)] for i in range(2)]
            sq = P.sb(f"msq_{l}", [128, 8, TB], BF16, es2)
            rstd = P.sb(f"mrstd_{l}", [128, TB], F32, es2)
            t32 = P.sb(f"mt32_{l}", [128, TB], F32, es2)
            T32ALT[id(t32)] = (P.sb(f"mt32b_{l}", [128, TB], F32, es2), Res("mt32b"))
            mtmp = (sq, Res("msq"), rstd, Res("mrstd"), t32, Res("mt32"), PSB[0], RPS[0])
            rw = P.sb(f"rw_{l}", [128, 8, NE], F32, es2)
            Rrw = Res("rw")
            combT = P.sb(f"combT_{l}", [16, ntok], F32, es2)
            RcombT = [Res(f"combT{j}") for j in range(nmb)]
            rs = P.sb(f"rs_{l}", [128, 2, NE], F32, es2)
            rsel = P.sb(f"rsel_{l}", [128, 2, NE], F32, es2)
            req = P.sb(f"req_{l}", [128, 2, NE], F32, es2)
            rsel2 = P.sb(f"rsel2_{l}", [128, 2, NE], F32, es2)
            rmk = P.sb(f"rmk_{l}", [128, 2, NE], F32, es2)
            rm1 = P.sb(f"rm1_{l}", [128, 8], F32, es2)
            rm2 = P.sb(f"rm2_{l}", [128, 8], F32, es2)
            rgs = P.sb(f"rgs_{l}", [128, 8], F32, es2)
            rgm = P.sb(f"rgm_{l}", [128, 8], F32, es2)
            rgx = P.sb(f"rgx_{l}", [128, 2], F32, es2)
            rws = P.sb(f"rws_{l}", [128, 2], F32, es2)
            comb = P.sb(f"comb_{l}", [128, 2, NE], F32, es2)
            Rr = Res("route")
            T.dma("sp", rw[:], router_d[:, :, :], writes=[Rrw])
            gs_off, sh_off = 24, 32
            rb = vec[:, VEC["rb"]:VEC["rb"] + NE]
            def rt_mod(j):
                sl = slice(j * TB, (j + 1) * TB)
                mbi = (j * TB) // MB
                h32, Rh32 = h32b[j % 2], Rh32b[j % 2]
                modulate(es2, lambda dc: XT[:, dc, sl], XTr[j], TB, gs_off, sh_off, l,
                         lambda dc: H2[:, dc, sl], RH2[mbi],
                         dst32=lambda dc: h32[:, dc, :], Rdst32=Rh32, tmp=mtmp)

            def rt_route(j):
                sl = slice(j * TB, (j + 1) * TB)
                mbi = (j * TB) // MB
                h32, Rh32 = h32b[j % 2], Rh32b[j % 2]
                lg = PSB[1][:, 0:2 * NE].rearrange("p (a e) -> p a e", e=NE)
                fns = []
                for ti in range(2):
                    for dc in range(8):
                        fns.append(lambda e, ti=ti, dc=dc: e.matmul(
                            lg[:, ti, :], h32[:, dc, ti * 128:(ti + 1) * 128], rw[:, dc, :],
                            start=(dc == 0), stop=(dc == 7)))
                T.group("pe", fns, reads=Rh32 + [Rrw], writes=[RPS[1]])
                T.op("act", lambda e: e.activation(out=rs[:], in_=lg, func=AF.Exp, scale=-1.0), reads=[RPS[1]], writes=[Rr])
                T.op("dve", lambda e: e.tensor_scalar(out=rs[:], in0=rs[:], scalar1=1.0, scalar2=None, op0=ALU.add),
                     reads=[Rr], writes=[Rr])
                T.op("dve", lambda e: e.reciprocal(out=rs[:], in_=rs[:]), reads=[Rr], writes=[Rr])
                dv = lambda fn, rd=(), : T.op("dve", fn, reads=[Rr] + list(rd), writes=[Rr])
                g4 = lambda t: t[:].rearrange("p a (g k) -> p (a g) k", k=4)
                dv(lambda e: e.tensor_tensor(out=rsel[:], in0=rs[:], in1=rb.unsqueeze(1).to_broadcast([128, 2, NE]),
                                             op=ALU.add), rd=[Rvec])
                dv(lambda e: e.tensor_reduce(out=rm1[:], in_=g4(rsel), axis=mybir.AxisListType.X, op=ALU.max))
                dv(lambda e: e.tensor_tensor(out=g4(req), in0=g4(rsel), in1=rm1[:].unsqueeze(2).to_broadcast([128, 8, 4]),
                                             op=ALU.is_equal))
                dv(lambda e: e.scalar_tensor_tensor(out=rsel2[:], in0=req[:], scalar=-1.0e9, in1=rsel[:],
                                                    op0=ALU.mult, op1=ALU.add))
                dv(lambda e: e.tensor_reduce(out=rm2[:], in_=g4(rsel2), axis=mybir.AxisListType.X, op=ALU.max))
                dv(lambda e: e.tensor_tensor(out=rgs[:], in0=rm1[:], in1=rm2[:], op=ALU.add))
                dv(lambda e: e.tensor_reduce(out=rgx[:], in_=rgs[:].rearrange("p (a g) -> p a g", g=4),
                                             axis=mybir.AxisListType.X, op=ALU.max))
                dv(lambda e: e.tensor_tensor(out=rgm[:].rearrange("p (a g) -> p a g", g=4),
                                             in0=rgs[:].rearrange("p (a g) -> p a g", g=4),
                                             in1=rgx[:].unsqueeze(2).to_broadcast([128, 2, 4]), op=ALU.is_equal))
                dv(lambda e: e.tensor_tensor(out=g4(rmk), in0=g4(rsel), in1=rm2[:].unsqueeze(2).to_broadcast([128, 8, 4]),
                                             op=ALU.is_ge))
                dv(lambda e: e.tensor_tensor(out=g4(rmk), in0=g4(rmk), in1=rgm[:].unsqueeze(2).to_broadcast([128, 8, 4]),
                                             op=ALU.mult))
                dv(lambda e: e.tensor_tensor(out=rmk[:], in0=rmk[:], in1=rs[:], op=ALU.mult))
                dv(lambda e: e.tensor_reduce(out=rws[:], in_=rmk[:], axis=mybir.AxisListType.X, op=ALU.add))
                dv(lambda e: e.reciprocal(out=rws[:], in_=rws[:]))
                dv(lambda e: e.tensor_tensor(out=comb[:], in0=rmk[:], in1=rws[:].unsqueeze(2).to_broadcast([128, 2, NE]),
                                             op=ALU.mult))
                tp = PSB[2][0:16, 0:256]
                T.group("pe", [lambda e, ti=ti: e.transpose(tp[:, ti * 128:(ti + 1) * 128], comb[:, ti, :],
                                                            cst32[:, CST["ident"]:CST["ident"] + 128])
                               for ti in range(2)], reads=[Rr, Rcst], writes=[RPS[2]])
                T.op("act", lambda e: e.activation(out=combT[:, sl], in_=tp, func=AF.Copy),
                     reads=[RPS[2]], writes=[RcombT[mbi]])
            rt_mod(0)
            for j in range(nb):
                nxt = T.capture(lambda: rt_mod(j + 1)) if j + 1 < nb else []
                T.play(T.capture(lambda: rt_route(j)), nxt)
            if stage == f"comb{l}":
                return "comb", combT
            cmb_d = nc.dram_tensor(f"cmb_scratch{l}", [16, ntok], F32, kind="Internal").ap()
            Rcmb_d = Res("cmb_d")
            T.dma("sp", cmb_d[:, :], combT[:, :], reads=RcombT, writes=[Rcmb_d])
            wgu = [P.sb(f"wgu{i}_{l}", [128, 8, 512], BF16, es2) for i in range(2)]
            wdn = [P.sb(f"wdn{i}_{l}", [128, 2, 1024], BF16, es2) for i in range(2)]
            Rwgu = [Res("wgu0"), Res("wgu1")]
            Rwdn = [Res("wdn0"), Res("wdn1")]
            cmb_sb = P.sb(f"cmb_sb_{l}", [128, MB], F32, es2)
            Rcmb = Res("cmb")
            s_sb = [P.sb(f"s_sb{i}_{l}", [128, MB], F32, es2) for i in range(2)]
            Rs_sb = [Res("s_sb0"), Res("s_sb1")]
            t_sb = [P.sb(f"t_sb{i}_{l}", [128, MB], F32, es2) for i in range(2)]
            Rt_sb = [Res("t_sb0"), Res("t_sb1")]
            a_sb = [P.sb(f"a_sb{i}_{l}", [128, 2, MB], BF16, es2) for i in range(2)]
            Ra_sb = [[Res(f"a{i}{f}") for f in range(2)] for i in range(2)]
            g2_off = 40
            cmb2 = [cmb_sb, P.sb(f"cmb_sb2_{l}", [128, MB], F32, es2)]
            Rcmb2 = [Rcmb, Res("cmb2")]
            iters = [(ex, mb) for ex in range(NE) for mb in range(nmb)]

            def load_w(ex):
                wb = ex % 2
                T.dma("pool", wgu[wb][:], wgu_d[l, ex], writes=[Rwgu[wb]])
                T.dma("pool", wdn[wb][:], wd_d[l, ex], writes=[Rwdn[wb]])

            def emit_gu(it):
                ex, mb = iters[it]
                wb = ex % 2
                ab = it % 2
                msl = slice(mb * MB, (mb + 1) * MB)
                T.dma("sp", cmb2[ab][:], cmb_d[ex:ex + 1, msl].to_broadcast([128, MB]),
                      reads=[Rcmb_d], writes=[Rcmb2[ab]])
                for fc in range(2):
                    bg, bu = 1 + fc, 3 + fc
                    T.group("pe", [lambda e, dc=dc, fc=fc, bg=bg: e.matmul(
                        PSB[bg][:, :], wgu[wb][:, dc, fc * 128:(fc + 1) * 128], H2[:, dc, msl],
                        start=(dc == 0), stop=(dc == 7)) for dc in range(8)],
                        reads=[Rwgu[wb], RH2[mb]], writes=[RPS[bg]], cost=1.8)
                    T.group("pe", [lambda e, dc=dc, fc=fc, bu=bu: e.matmul(
                        PSB[bu][:, :], wgu[wb][:, dc, 256 + fc * 128:256 + (fc + 1) * 128], H2[:, dc, msl],
                        start=(dc == 0), stop=(dc == 7)) for dc in range(8)],
                        reads=[Rwgu[wb], RH2[mb]], writes=[RPS[bu]], cost=1.8)
                    T.op("act", lambda e, fc=fc, bg=bg: e.activation(out=s_sb[fc][:], in_=PSB[bg][:, :], func=AF.Silu),
                         reads=[RPS[bg]], writes=[Rs_sb[fc]], cost=0.65)
                    T.op("pool", lambda e, fc=fc: e.tensor_tensor(out=t_sb[fc][:], in0=s_sb[fc][:], in1=cmb2[ab][:], op=ALU.mult),
                         reads=[Rs_sb[fc], Rcmb2[ab]], writes=[Rt_sb[fc]], cost=1.3)
                    T.op("dve", lambda e, fc=fc, bu=bu: e.tensor_tensor(out=a_sb[ab][:, fc, :], in0=PSB[bu][:, :], in1=t_sb[fc][:],
                                                          op=ALU.mult),
                         reads=[RPS[bu], Rt_sb[fc]], writes=[Ra_sb[ab][fc]], cost=0.7)

            ysb = [P.sb(f"ysb{i}_{l}", [128, MB], F32, es2) for i in range(2)]
            Rysb = [Res("ysb0"), Res("ysb1")]

            def emit_down(it):
                ex, mb = iters[it]
                wb = ex % 2
                ab = it % 2
                msl = slice(mb * MB, (mb + 1) * MB)
                for oc in range(8):
                    by = (5, 6, 0)[oc % 3]
                    T.group("pe", [lambda e, fc=fc, oc=oc, by=by: e.matmul(
                        PSB[by][:, :], wdn[wb][:, fc, oc * 128:(oc + 1) * 128], a_sb[ab][:, fc, :],
                        start=(fc == 0), stop=(fc == 1)) for fc in range(2)],
                        reads=[Rwdn[wb], Ra_sb[ab][0], Ra_sb[ab][1]], writes=[RPS[by]], cost=0.45)
                    xr = [XTr[mb * 2][oc], XTr[mb * 2 + 1][oc]]
                    if oc not in (1, 5):
                        T.op("dve", lambda e, oc=oc, by=by: e.scalar_tensor_tensor(
                            out=XT[:, oc, msl], in0=PSB[by][:, :], scalar=dcol(l, g2_off, oc),
                            in1=XT[:, oc, msl], op0=ALU.mult, op1=ALU.add),
                            reads=[RPS[by], Rdrv] + xr, writes=xr, cost=0.8)
                    else:
                        yi = (oc // 2) % 2
                        T.op("act", lambda e, oc=oc, by=by, yi=yi: e.activation(
                            out=ysb[yi][:], in_=PSB[by][:, :], func=AF.Copy, scale=dcol(l, g2_off, oc)),
                            reads=[RPS[by], Rdrv], writes=[Rysb[yi]], cost=0.7)
                        T.op("pool", lambda e, oc=oc, yi=yi: e.tensor_tensor(
                            out=XT[:, oc, msl], in0=XT[:, oc, msl], in1=ysb[yi][:], op=ALU.add),
                            reads=[Rysb[yi]] + xr, writes=xr, cost=1.3)

            load_w(0)
            load_w(1)
            emit_gu(0)
            for it in range(len(iters)):
                nxt = T.capture(lambda: emit_gu(it + 1)) if it + 1 < len(iters) else []
                T.play(nxt, T.capture(lambda: emit_down(it)))
                ex, mb = iters[it]
                if mb == nmb - 1 and ex + 2 < NE:
                    load_w(ex + 2)
            T.barrier()
        return None

    r = moe_phase(0, EXT)
    if stage == "comb0":
        return _finish_comb(P, T, r[1], dbg_d, out_d, XT, XTr)
    if stage == "x2":
        return _finish(P, T, XT, XTr, dbg_d, out_d, debug=True)

    pidx = _pool_mats_cached(0)[1]
    plan = _pool_plan()
    with ExitStack() as es3:
        u_tm = P.sb("u_tm", [128, EXT // 128, D], BF16, es3)
        Ru = [Res(f"u{t}") for t in range(EXT // 128)]
        pwi = P.sb("pwi", [128, 8, D], BF16, es3)
        pwo = P.sb("pwo", [128, 8, D], BF16, es3)
        pwg = P.sb("pwg", [128, 4, 2, 256], BF16, es3)
        pm = P.sb("pm", [128, n_pmat, 128], BF16, es3)
        Rpw = Res("pw")
        hb = P.sb("hb", [128, 8, TB], BF16, es3)
        Rhb = Res("hb")
        sq = P.sb("psq", [128, 8, TB], BF16, es3)
        rstd = P.sb("prstd", [128, TB], F32, es3)
        t32 = P.sb("pt32", [128, TB], F32, es3)
        T32ALT[id(t32)] = (P.sb("pt32b", [128, TB], F32, es3), Res("pt32b"))
        mtmp = (sq, Res("psq"), rstd, Res("prstd"), t32, Res("pt32"), PSB[0], RPS[0])
        dT = P.sb("dT", [128, 8, TB], BF16, es3)
        RdT = Res("dT")
        yin = P.sb("yin", [128, 8, TB], BF16, es3)
        Ryin = Res("yin")
        T.dma("pool", pwi[:], pw_in_d[:, :, :], writes=[Rpw])
        T.dma("pool", pwo[:], pw_out_d[:, :, :], writes=[Rpw])
        T.dma("pool", pwg[:], pw_grp_d[:, :, :, :], writes=[Rpw])
        T.dma("pool", pm[:], pmat_d.rearrange("m s t -> s m t"), writes=[Rpw])
        def pl_u(j):
            sl = slice(j * TB, (j + 1) * TB)
            modulate(es3, lambda dc, sl=sl: XT[:, dc, sl], XTr[j], TB, 0, 8, 1,
                     lambda dc: hb[:, dc, :], Rhb, tmp=mtmp)
            for ti in range(2):
                tile_i = j * 2 + ti
                for hf in range(2):
                    pb = 1 + (ti * 2 + hf) % 4
                    T.group("pe", [lambda e, dc=dc, ti=ti, hf=hf, pb=pb: e.matmul(
                        PSB[pb][:, :], hb[:, dc, ti * 128:(ti + 1) * 128], pwi[:, dc, hf * 512:(hf + 1) * 512],
                        start=(dc == 0), stop=(dc == 7)) for dc in range(8)], reads=[Rhb, Rpw], writes=[RPS[pb]])
                    eng = "act" if hf == 0 else "dve"
                    if eng == "act":
                        T.op("act", lambda e, tile_i=tile_i, hf=hf, pb=pb: e.activation(
                            out=u_tm[:, tile_i, hf * 512:(hf + 1) * 512], in_=PSB[pb][:, :], func=AF.Copy),
                            reads=[RPS[pb]], writes=[Ru[tile_i]])
                    else:
                        T.op("dve", lambda e, tile_i=tile_i, hf=hf, pb=pb: e.tensor_copy(
                            out=u_tm[:, tile_i, hf * 512:(hf + 1) * 512], in_=PSB[pb][:, :]),
                            reads=[RPS[pb]], writes=[Ru[tile_i]])
        def pl_mix(j):
            sl = slice(j * TB, (j + 1) * TB)
            for ti in range(2):
                tile_i = j * 2 + ti
                for wi in range(4):
                    dmin, dmax, nsp = plan[wi]
                    ds = [d for d in range(dmin, dmax + 1) if (wi, tile_i, d) in pidx]
                    pb = 1 + wi % 2
                    dps = PSB[pb][:, 0:256].rearrange("p (f t) -> p f t", t=128)
                    fns = []
                    srcs = []
                    for fc in range(2):
                        for k, d in enumerate(ds):
                            mi = pidx[(wi, tile_i, d)]
                            fns.append(lambda e, fc=fc, d=d, mi=mi, k=k, wi=wi, tile_i=tile_i, dps=dps, nds=len(ds): e.matmul(
                                dps[:, fc, :], u_tm[:, tile_i + d, wi * 256 + fc * 128:wi * 256 + (fc + 1) * 128],
                                pm[:, mi, :], start=(k == 0), stop=(k == nds - 1)))
                    for d in ds:
                        srcs.append(Ru[tile_i + d])
                    T.group("pe", fns, reads=srcs + [Rpw], writes=[RPS[pb]])
                    T.op("act", lambda e, wi=wi, ti=ti, dps=dps: e.activation(
                        out=dT[:, wi * 2:wi * 2 + 2, ti * 128:(ti + 1) * 128], in_=dps, func=AF.Copy),
                        reads=[RPS[pb]], writes=[RdT])
            for wi in range(4):
                for fo in range(2):
                    pb = 3 + (wi * 2 + fo) % 2
                    T.group("pe", [lambda e, fc=fc, wi=wi, fo=fo, pb=pb: e.matmul(
                        PSB[pb][:, 0:TB], pwg[:, wi, fc, fo * 128:(fo + 1) * 128], dT[:, wi * 2 + fc, :],
                        start=(fc == 0), stop=(fc == 1)) for fc in range(2)], reads=[RdT, Rpw], writes=[RPS[pb]])
                    ci = wi * 2 + fo
                    T.op("act", lambda e, ci=ci, pb=pb: e.activation(
                        out=yin[:, ci, :], in_=PSB[pb][:, 0:TB], func=AF.Copy,
                        scale=vec[:, VEC["pscale"] + ci:VEC["pscale"] + ci + 1]),
                        reads=[RPS[pb], Rvec], writes=[Ryin])
            for oc in range(8):
                pb = 5 + oc % 2
                T.group("pe", [lambda e, fcx=fcx, oc=oc, pb=pb: e.matmul(
                    PSB[pb][:, 0:TB], pwo[:, fcx, oc * 128:(oc + 1) * 128], yin[:, fcx, :],
                    start=(fcx == 0), stop=(fcx == 7)) for fcx in range(8)], reads=[Ryin, Rpw], writes=[RPS[pb]])
                T.op("dve", lambda e, oc=oc, pb=pb, sl=sl: e.scalar_tensor_tensor(
                    out=XT[:, oc, sl], in0=PSB[pb][:, 0:TB], scalar=dcol(1, 16, oc), in1=XT[:, oc, sl],
                    op0=ALU.mult, op1=ALU.add), reads=[RPS[pb], XTr[j][oc], Rdrv], writes=[XTr[j][oc]])
        for j in range(NB_EXT):
            pl_u(j)
        for j in range(NB_OWN):
            pl_mix(j)
        T.barrier()
    if stage == "x3":
        return _finish(P, T, XT, XTr, dbg_d, out_d, debug=True)

    moe_phase(1, OWN)
    if stage == "x4":
        return _finish(P, T, XT, XTr, dbg_d, out_d, debug=True)

    out_evs = []
    with ExitStack() as es4:
        sq2 = [P.sb(f"fsq{i}", [128, 8, TB], BF16, es4) for i in range(2)]
        Rsq2 = [Res("fsq0"), Res("fsq1")]
        rstd2 = [P.sb(f"frstd{i}", [128, TB], F32, es4) for i in range(2)]
        Rrstd2 = [Res("frstd0"), Res("frstd1")]
        out_v = out_d.rearrange("(dc p) t -> p dc t", p=128)

        def fin_block(j):
            sl = slice(j * TB, (j + 1) * TB)
            sq, Rsq, rstd, Rrstd = sq2[j % 2], Rsq2[j % 2], rstd2[j % 2], Rrstd2[j % 2]
            for dc in range(8):
                if dc % 2 == 0:
                    T.op("act", lambda e, dc=dc: e.activation(out=sq[:, dc, :], in_=XT[:, dc, sl], func=AF.Square),
                         reads=[XTr[j][dc]], writes=[Rsq])
                else:
                    T.op("pool", lambda e, dc=dc: e.tensor_tensor(out=sq[:, dc, :], in0=XT[:, dc, sl], in1=XT[:, dc, sl],
                                                                  op=ALU.mult), reads=[XTr[j][dc]], writes=[Rsq])
            pb = 1 + j % 2
            T.group("pe", [lambda e, dc=dc: e.matmul(PSB[pb][:, 0:TB], ones[:], sq[:, dc, :],
                                                      start=(dc == 0), stop=(dc == 7)) for dc in range(8)],
                    reads=[Rsq, Rid], writes=[RPS[pb]])
            T.op("act", lambda e: e.activation(out=rstd[:], in_=PSB[pb][:, 0:TB], func=AF.Ln, scale=1.0 / D, bias=EPS),
                 reads=[RPS[pb]], writes=[Rrstd])
            T.op("act", lambda e: e.activation(out=rstd[:], in_=rstd[:], func=AF.Exp, scale=-0.5),
                 reads=[Rrstd], writes=[Rrstd])
            for dc in range(8):
                T.op("dve", lambda e, dc=dc: e.scalar_tensor_tensor(
                    out=XT[:, dc, sl], in0=XT[:, dc, sl], scalar=vec[:, VEC["fing"] + dc:VEC["fing"] + dc + 1],
                    in1=rstd[:], op0=ALU.mult, op1=ALU.mult), reads=[XTr[j][dc], Rrstd, Rvec], writes=[XTr[j][dc]])

        for j in range(0, NB_OWN, 2):
            T.play(T.capture(lambda: fin_block(j)), T.capture(lambda: fin_block(j + 1)))
            if not stage:
                for jj in (j, j + 1):
                    sl = slice(jj * TB, (jj + 1) * TB)
                    out_evs.append(T.dma("sp", out_v[:, :, sl], XT[:, :, sl], reads=XTr[jj]))
        T.barrier()
    if not stage:
        for ev in out_evs:
            T.E["sp"].wait(ev)
        print(f"[kernel] ops={T.nops} waits={T.nwaits}", flush=True)
        return
    return _finish(P, T, XT, XTr, dbg_d, out_d, debug=bool(stage))


def _finish_comb(P, T, combT, dbg_d, out_d, XT, XTr):
    T.barrier()
    ev = T.dma("sp", dbg_d[0:16, :], combT[:, :])
    T.E["sp"].wait(ev)
    _finish(P, T, XT, XTr, dbg_d, out_d, debug=False)


def _finish(P, T, XT, XTr, dbg_d, out_d, debug):
    evs = []
    if debug:
        for dc in range(8):
            evs.append(T.dma("sp", dbg_d[dc * 128:(dc + 1) * 128, :], XT[:, dc, :], reads=[r[dc] for r in XTr]))
    for dc in range(8):
        evs.append(T.dma("sp", out_d[dc * 128:(dc + 1) * 128, :], XT[:, dc, 0:OWN], reads=[r[dc] for r in XTr]))
    for ev in evs:
        T.E["sp"].wait(ev)
    print(f"[kernel] ops={T.nops} waits={T.nwaits}", flush=True)


def _prepare(inputs):
    shared = _shared_weights(inputs)
    w_in = np.asarray(inputs["hg_w_in"][0], np.float32)
    w_in_by_half = [_w_in_tiled(w_in, 0), _w_in_tiled(w_in, 1)]
    return shared, w_in_by_half


def kernel(**inputs):
    inputs = {k: np.asarray(v) for k, v in inputs.items()}
    shared, w_in_by_half = _prepare(inputs)
    n_pmat = _pool_mats_cached(0)[0].shape[0]
    nc = build_program(stage=_DBG["stage"], n_pmat=n_pmat)
    in_maps = [_core_inputs(inputs, shared, w_in_by_half, c) for c in range(8)]
    res = run_bass_kernel_spmd(nc, in_maps, core_ids=list(range(8)))
    out = np.zeros((4, L, D), np.float32)
    for c in range(8):
        b, half = c // 2, c % 2
        o = res.results[c]["outT"].T
        if half == 0:
            out[b, 0:OWN] = o
        else:
            out[b, OWN:] = o[::-1]
    return out
```

```python
import numpy as np
from contextlib import ExitStack
import concourse.bass as bass
import concourse.mybir as mybir
from concourse.bass_utils import run_bass_kernel_spmd

F32 = mybir.dt.float32
BF16 = mybir.dt.bfloat16
AF = mybir.ActivationFunctionType
ALU = mybir.AluOpType

D = 1024
L = 4096
CTX = 256
OWN = 2048
HALO = 512
EXT = OWN + HALO
TB = 256
NB_EXT = EXT // TB
NB_OWN = OWN // TB
EPS = 1e-6
NE = 16
POOL_WINDOWS = (2, 4, 8, 16)


class Res:
    __slots__ = ("name", "w", "r", "tw", "tr")

    def __init__(self, name=""):
        self.name = name
        self.w = None
        self.r = []
        self.tw = 0.0
        self.tr = 0.0


class _Eng:
    def __init__(self, trk, name, handle, sem):
        self.trk = trk
        self.name = name
        self.h = handle
        self.sem = sem
        self.count = 0
        self.seen = {}

    def wait(self, ev):
        if ev is None:
            return
        key, val = ev[0], ev[1]
        if self.seen.get(id(key), 0) >= val:
            return
        self.seen[id(key)] = val
        self.h.wait_ge(key, val)
        self.trk.nwaits += 1


class Tracker:
    def __init__(self, nc, engines):
        self.nc = nc
        self.E = {n: _Eng(self, n, h, s) for n, (h, s) in engines.items()}
        self.nwaits = 0
        self.nops = 0
        self.dma_sems = {}
        self.dma_rr = {}
        self.rec = None
        self.t_eng = {}
        self.strict = True

    def add_dma_sems(self, qname, sems):
        self.dma_sems[qname] = [[s, 0] for s in sems]
        self.dma_rr[qname] = 0

    def _deps(self, eng, reads, writes):
        for r in reads:
            if r.w is not None:
                eng.wait(r.w)
        for w in writes:
            if w.w is not None and (self.strict or w.w[0] is not eng.sem):
                eng.wait(w.w)
            for rd in w.r:
                if self.strict or rd[0] is not eng.sem:
                    eng.wait(rd)

    def _commit(self, ev, reads, writes):
        for r in reads:
            r.r.append(ev)
            if len(r.r) > 48:
                best = {}
                for k, v in r.r:
                    if id(k) not in best or best[id(k)][1] < v:
                        best[id(k)] = (k, v)
                r.r = list(best.values())
        for w in writes:
            w.w = ev
            w.r = []

    LAT = 0.3
    DEF_COST = {"act": 0.45, "dve": 0.4, "pool": 0.7, "pe": 0.22, "sp": 0.05}

    def _est(self, ename, reads, writes):
        t = self.t_eng.get(ename, 0.0)
        for r in reads:
            t = max(t, r.tw + self.LAT)
        for w in writes:
            t = max(t, w.tw + self.LAT, w.tr + self.LAT)
        return t

    def _advance(self, ename, reads, writes, cost, async_cost=None):
        st = self._est(ename, reads, writes)
        fin = st + cost
        self.t_eng[ename] = fin
        done = fin if async_cost is None else st + async_cost
        for r in reads:
            r.tr = max(r.tr, done)
        for w in writes:
            w.tw = done
            w.tr = 0.0

    def _cost_of(self, kind, args):
        if kind == "op":
            return args[4] if args[4] is not None else self.DEF_COST[args[0]]
        if kind == "group":
            return args[4] if args[4] is not None else self.DEF_COST["pe"] * len(args[1])
        return 0.05

    def capture(self, fn):
        prev = self.rec
        self.rec = []
        fn()
        out = self.rec
        self.rec = prev
        return out

    def play(self, *lists):
        lists = [l for l in lists if l]
        if _DBG.get("seq"):
            for l in lists:
                for kind, args in l:
                    getattr(self, kind)(*args)
            return
        pos = [0] * len(lists)
        while True:
            best, bi = None, -1
            for i, l in enumerate(lists):
                if pos[i] < len(l):
                    kind, args = l[pos[i]]
                    ename = args[0]
                    if kind == "dma":
                        st = self._est(ename, args[3], args[4])
                    else:
                        st = self._est(ename, args[2], args[3])
                    if best is None or st < best - 1e-9:
                        best, bi = st, i
            if bi < 0:
                break
            kind, args = lists[bi][pos[bi]]
            pos[bi] += 1
            getattr(self, kind)(*args)

    def op(self, ename, fn, reads=(), writes=(), cost=None):
        if self.rec is not None:
            self.rec.append(("op", (ename, fn, list(reads), list(writes), cost)))
            return
        self._advance(ename, reads, writes, cost if cost is not None else self.DEF_COST[ename])
        eng = self.E[ename]
        self._deps(eng, reads, writes)
        ins = fn(eng.h)
        self.nops += 1
        eng.count += 1
        ins.then_inc(eng.sem, 1)
        self._commit((eng.sem, eng.count), reads, writes)

    def group(self, ename, fns, reads=(), writes=(), cost=None):
        if self.rec is not None:
            self.rec.append(("group", (ename, fns, list(reads), list(writes), cost)))
            return
        self._advance(ename, reads, writes, cost if cost is not None else self.DEF_COST["pe"] * len(fns))
        eng = self.E[ename]
        self._deps(eng, reads, writes)
        ins = None
        for f in fns:
            ins = f(eng.h)
            self.nops += 1
        eng.count += 1
        ins.then_inc(eng.sem, 1)
        self._commit((eng.sem, eng.count), reads, writes)

    def dma(self, qname, out, in_, reads=(), writes=()):
        if self.rec is not None:
            self.rec.append(("dma", (qname, out, in_, list(reads), list(writes))))
            return None
        self._advance(qname, reads, writes, 0.05, async_cost=3.0)
        eng = self.E[qname]
        pool = self.dma_sems[qname]
        i = self.dma_rr[qname]
        self.dma_rr[qname] = (i + 1) % len(pool)
        slot = pool[i]
        sem = slot[0]
        if slot[1] > 0:
            eng.wait((sem, 16 * slot[1]))
        self._deps(eng, reads, writes)
        slot[1] += 1
        eng.h.dma_start(out=out, in_=in_).then_inc(sem, 16)
        self.nops += 1
        ev = (sem, 16 * slot[1])
        self._commit(ev, reads, writes)
        return ev

    def barrier(self):
        tmax = max(self.t_eng.values()) if self.t_eng else 0.0
        for k in self.t_eng:
            self.t_eng[k] = tmax
        evs = [(e.sem, e.count) for e in self.E.values() if e.count > 0]
        for pool in self.dma_sems.values():
            for s, uses in pool:
                if uses > 0:
                    evs.append((s, 16 * uses))
        for e in self.E.values():
            for ev in evs:
                if ev[0] is not e.sem:
                    e.wait(ev)


def _col(v):
    return np.ascontiguousarray(np.asarray(v, np.float32).reshape(-1, 128).T)


VEC = dict(c=0, cctx=8, bmod0=16, bmod1=64, n1g0=112, n2g0=120, n1g1=128, n2g1=136,
           lbF0=144, lbF1=152, lbB0=160, lbB1=168, pscale=176, fing=184, gnorm=192, rb=193)
NV = 256

CST = dict(ident=0, ones=128, scanmask=256, triF=512, triB=576)
NCST = 640


def _pool_plan():
    plan = []
    for w in POOL_WINDOWS:
        reach = w // 2
        dt = (reach + 1) // 2 + (0 if reach % 2 == 0 else 0)
        dt = (reach + 1) // 2 if reach > 1 else 1
        plan.append((-dt, dt, dt))
    return plan


def _pool_mats(half):
    mats = []
    index = {}
    plan = _pool_plan()
    cache = {}
    for wi, w in enumerate(POOL_WINDOWS):
        dmin, dmax, nsp = plan[wi]
        for ti in range(OWN // 128):
            special = ti < nsp
            for d in range(dmin, dmax + 1):
                sj = ti + d
                if sj < 0 or sj >= EXT // 128:
                    continue
                key = (wi, ti if special else -1, d)
                if key in cache:
                    index[(wi, ti, d)] = cache[key]
                    continue
                M = np.zeros((128, 128), np.float64)
                for tl in range(128):
                    f = ti * 128 + tl
                    t = f if half == 0 else L - 1 - f
                    r, c = divmod(t, 64)
                    r0, r1 = max(r - w // 2, 0), min(r - w // 2 + w, 64)
                    c0, c1 = max(c - w // 2, 0), min(c - w // 2 + w, 64)
                    inv = 1.0 / ((r1 - r0) * (c1 - c0))
                    for sl in range(128):
                        fs = sj * 128 + sl
                        ts = fs if half == 0 else L - 1 - fs
                        rs, cs = divmod(ts, 64)
                        if r0 <= rs < r1 and c0 <= cs < c1:
                            M[sl, tl] += inv
                        if ts == t:
                            M[sl, tl] -= 1.0
                cache[key] = len(mats)
                index[(wi, ti, d)] = len(mats)
                mats.append(M.astype(np.float32))
    return np.stack(mats), index


_POOL_CACHE = {}


def _pool_mats_cached(half):
    if half not in _POOL_CACHE:
        _POOL_CACHE[half] = _pool_mats(half)
    return _POOL_CACHE[half]


def _consts():
    c = np.zeros((128, NCST), np.float32)
    c[:, CST["ident"]:CST["ident"] + 128] = np.eye(128, dtype=np.float32)
    c[:, CST["ones"]:CST["ones"] + 128] = 1.0
    m = np.ones((128, TB), np.float32)
    m[:, ::64] = 0.0
    c[:, CST["scanmask"]:CST["scanmask"] + TB] = m
    s = np.arange(64)[:, None]
    cc = np.arange(64)[None, :]
    tf = (s <= cc).astype(np.float32)
    tb = (s >= cc).astype(np.float32)
    c[0:64, CST["triF"]:CST["triF"] + 64] = tf
    c[64:128, CST["triF"]:CST["triF"] + 64] = tf
    c[0:64, CST["triB"]:CST["triB"] + 64] = tb
    c[64:128, CST["triB"]:CST["triB"] + 64] = tb
    sel = np.zeros((16, NE, 128), np.float32)
    for e in range(NE):
        sel[e, e, :] = 1.0
    return c, sel.reshape(16, NE * 128)


def _shared_weights(inp):
    f = lambda a: np.asarray(a, np.float32)
    out = {}
    wm = f(inp["w_mod"])
    out["w_mod_t"] = np.ascontiguousarray(
        wm.reshape(2, 8, 128, 12, 512).transpose(0, 3, 2, 1, 4))
    wg = f(inp["moe_w_gate"]).reshape(2, NE, 8, 128, 256)
    wu = f(inp["moe_w_up"]).reshape(2, NE, 8, 128, 256)
    wgu = np.concatenate([wg, wu], axis=-1)
    out["wgu_t"] = np.ascontiguousarray(wgu.transpose(0, 1, 3, 2, 4))
    wd = f(inp["moe_w_down"]).reshape(2, NE, 2, 128, 1024)
    out["wd_t"] = np.ascontiguousarray(wd.transpose(0, 1, 3, 2, 4))
    out["w_out_t"] = np.ascontiguousarray(
        f(inp["hg_w_out"][0]).reshape(4, 2, 128, 1024).transpose(0, 2, 1, 3))
    out["pw_in_t"] = np.ascontiguousarray(f(inp["pool_w_in"][0]).reshape(8, 128, 1024).transpose(1, 0, 2))
    out["pw_out_t"] = np.ascontiguousarray(f(inp["pool_w_out"][0]).reshape(8, 128, 1024).transpose(1, 0, 2))
    out["pw_grp_t"] = np.ascontiguousarray(
        f(inp["pool_w_grp"][0]).reshape(4, 2, 128, 256).transpose(2, 0, 1, 3))
    out["router_t"] = np.ascontiguousarray(f(inp["router_w"]).reshape(8, 128, NE).transpose(1, 0, 2))
    cst, sel = _consts()
    out["cst"] = cst
    out["sel"] = sel
    return out


def _w_in_tiled(w_in, half):
    q, v, zf, zb, g = [w_in[:, i * D:(i + 1) * D] for i in range(5)]
    zF, zB = (zf, zb) if half == 0 else (zb, zf)
    groups = []
    for gi in range(4):
        cols = []
        for kind in (q, zF, zB, g, v):
            for hh in range(2):
                h = 2 * gi + hh
                cols.append(kind[:, h * 128:(h + 1) * 128])
        Wg = np.concatenate(cols, axis=1)
        groups.append(Wg.reshape(8, 128, 1280).transpose(1, 0, 2))
    return np.ascontiguousarray(np.stack(groups))


def _core_inputs(inp, shared, w_in_by_half, core):
    b, half = core // 2, core % 2
    f = lambda a: np.asarray(a, np.float32)
    x = f(inp["x"][b])
    ctx = f(inp["ctx"][b])
    if half == 1:
        x = x[::-1]
        ctx = ctx[::-1]
    m = dict(shared)
    m["xT"] = np.ascontiguousarray(x.T)
    m["ctxT"] = np.ascontiguousarray(ctx.T)
    vec = np.zeros((128, NV), np.float32)
    vec[:, VEC["c"]:VEC["c"] + 8] = _col(inp["c"][b])
    vec[:, VEC["cctx"]:VEC["cctx"] + 8] = _col(inp["c_ctx"])
    for l in range(2):
        vec[:, VEC[f"bmod{l}"]:VEC[f"bmod{l}"] + 48] = _col(inp["b_mod"][l])
        vec[:, VEC[f"n1g{l}"]:VEC[f"n1g{l}"] + 8] = _col(inp["norm1_g"][l])
        vec[:, VEC[f"n2g{l}"]:VEC[f"n2g{l}"] + 8] = _col(inp["norm2_g"][l])
    lbF, lbB = (inp["hg_lb_fwd"], inp["hg_lb_bwd"]) if half == 0 else (inp["hg_lb_bwd"], inp["hg_lb_fwd"])
    vec[:, VEC["lbF0"]:VEC["lbF0"] + 8] = _col(lbF[0])
    vec[:, VEC["lbF1"]:VEC["lbF1"] + 8] = _col(lbF[1])
    vec[:, VEC["lbB0"]:VEC["lbB0"] + 8] = _col(lbB[0])
    vec[:, VEC["lbB1"]:VEC["lbB1"] + 8] = _col(lbB[1])
    vec[:, VEC["pscale"]:VEC["pscale"] + 8] = _col(inp["pool_scale"][0])
    vec[:, VEC["fing"]:VEC["fing"] + 8] = _col(inp["final_g"])
    vec[:, VEC["gnorm"]] = f(inp["hg_gnorm"][0])
    vec[:, VEC["rb"]:VEC["rb"] + NE] = f(inp["router_b"])[None, :]
    m["vec"] = vec
    m["w_in_t"] = w_in_by_half[half]
    m["pmat"] = _pool_mats_cached(half)[0]
    return m


_DBG = {"stage": None}


class _Prog:
    def __init__(self, stage=None):
        self.stage = stage
        self.nc = bass.Bass("TRN2", target_bir_lowering=False)
        self.es = ExitStack()
        self.nsb = 0

    def sb(self, name, shape, dt=F32, es=None):
        return (es or self.es).enter_context(self.nc.sbuf_tensor("sb_" + name, list(shape), dt))

    def ps(self, name, shape, dt=F32, es=None):
        return (es or self.es).enter_context(self.nc.psum_tensor("pp_" + name, list(shape), dt))

    def sem(self, name):
        return self.es.enter_context(self.nc.semaphore(name))

    def dram_in(self, name, shape, dt=F32):
        return self.nc.dram_tensor(name, list(shape), dt, kind="ExternalInput").ap()

    def dram_out(self, name, shape, dt=F32):
        return self.nc.dram_tensor(name, list(shape), dt, kind="ExternalOutput").ap()


def build_program(stage=None, n_pmat=1):
    P = _Prog(stage)
    nc = P.nc
    with P.es:
        _build(P, n_pmat)
    return nc


def _build(P, n_pmat):
    nc = P.nc
    stage = P.stage
    xT_d = P.dram_in("xT", [D, L])
    ctxT_d = P.dram_in("ctxT", [D, CTX])
    vec_d = P.dram_in("vec", [128, NV])
    cst_d = P.dram_in("cst", [128, NCST])
    sel_d = P.dram_in("sel", [16, NE * 128])
    w_mod_d = P.dram_in("w_mod_t", [2, 12, 128, 8, 512])
    w_in_d = P.dram_in("w_in_t", [4, 128, 8, 1280])
    w_out_d = P.dram_in("w_out_t", [4, 128, 2, 1024])
    wgu_d = P.dram_in("wgu_t", [2, NE, 128, 8, 512])
    wd_d = P.dram_in("wd_t", [2, NE, 128, 2, 1024])
    pw_in_d = P.dram_in("pw_in_t", [128, 8, 1024])
    pw_out_d = P.dram_in("pw_out_t", [128, 8, 1024])
    pw_grp_d = P.dram_in("pw_grp_t", [128, 4, 2, 256])
    router_d = P.dram_in("router_t", [128, 8, NE])
    pmat_d = P.dram_in("pmat", [n_pmat, 128, 128])
    out_d = P.dram_out("outT", [D, OWN])
    dbg_d = P.dram_out("dbg", [D, EXT]) if stage else None

    engs = {"pe": (nc.tensor, P.sem("s_pe")), "act": (nc.scalar, P.sem("s_act")),
            "dve": (nc.vector, P.sem("s_dve")), "pool": (nc.gpsimd, P.sem("s_pool")),
            "sp": (nc.sync, P.sem("s_sp"))}
    T = Tracker(nc, engs)
    T.add_dma_sems("sp", [P.sem(f"dsp{i}") for i in range(8)])
    T.add_dma_sems("pool", [P.sem(f"dpl{i}") for i in range(8)])

    XT = P.sb("XT", [128, 8, EXT], F32)
    XTr = [[Res(f"XT{j}_{dc}") for dc in range(8)] for j in range(NB_EXT)]
    XTall = [r for row in XTr for r in row]
    vec = P.sb("vec", [128, NV], F32)
    Rvec = Res("vec")
    cst32 = P.sb("cst32", [128, NCST], F32)
    Rcst = Res("cst")
    ident = P.sb("ident", [128, 128], BF16)
    ones = P.sb("ones", [128, 128], BF16)
    Rid = Res("ident")
    modv = P.sb("modv", [128, 2, 48, 2], F32)
    Rmod = Res("modv")
    drv = P.sb("drv", [128, 192], F32)
    Rdrv = Res("drv")
    PSB = [P.ps(f"psb{i}", [128, 512], F32) for i in range(7)]
    RPS = [Res(f"ps{i}") for i in range(7)]
    PSH = P.ps("psh", [128, 1024], BF16)
    _rpsh = Res("psh")
    RPSH = [_rpsh, _rpsh]

    def vcol(name, n=8, off=0):
        a = VEC[name] + off
        return vec[:, a:a + n]

    T.dma("sp", vec[:], vec_d[:, :], writes=[Rvec])
    T.dma("sp", cst32[:], cst_d[:, :], writes=[Rcst])
    T.dma("pool", ident[:], cst_d[:, CST["ident"]:CST["ident"] + 128], writes=[Rid])
    T.dma("pool", ones[:], cst_d[:, CST["ones"]:CST["ones"] + 128], writes=[Rid])
    for dc in range(8):
        T.dma("sp", XT[:, dc, :], xT_d[dc * 128:(dc + 1) * 128, 0:EXT], writes=[XTr[j][dc] for j in range(NB_EXT)])

    scanmask = cst32[:, CST["scanmask"]:CST["scanmask"] + TB]
    triF = cst32[:, CST["triF"]:CST["triF"] + 64]
    triB = cst32[:, CST["triB"]:CST["triB"] + 64]

    cs16 = P.sb("cs16", [128, 8, 2], BF16)
    Rcs = Res("cs")
    mps = PSB[0][:, 0:96].rearrange("p (n c) -> p n c", c=2)

    def mod_piece(l, j, wmp, Rwmp):
        buf = wmp[j % 2]
        T.dma("pool", buf[:], w_mod_d[l, j], writes=[Rwmp[j % 2]])
        fns = []
        for cg in range(4):
            n = j * 4 + cg
            for dc in range(8):
                fns.append(lambda e, n=n, cg=cg, dc=dc: e.matmul(
                    mps[:, n, :], buf[:, dc, cg * 128:(cg + 1) * 128], cs16[:, dc, :],
                    start=(dc == 0), stop=(dc == 7)))
        T.group("pe", fns, reads=[Rwmp[j % 2], Rcs], writes=[RPS[0]])

    def mv(l, comp, which=0):
        return modv[:, l, comp * 8:(comp + 1) * 8, which]

    def mod_finish(l):
        bm = vcol(f"bmod{l}", 48)
        T.op("dve", lambda e: e.tensor_tensor(
            out=modv[:, l], in0=mps, in1=bm.unsqueeze(2).to_broadcast([128, 48, 2]), op=ALU.add),
            reads=[RPS[0], Rvec], writes=[Rmod])
        base = l * 64
        n1 = vcol(f"n1g{l}")
        n2 = vcol(f"n2g{l}")

        def gs(dst, sc, ng):
            T.op("dve", lambda e: e.scalar_tensor_tensor(out=dst, in0=sc, scalar=1.0, in1=ng,
                                                          op0=ALU.add, op1=ALU.mult),
                 reads=[Rmod, Rvec], writes=[Rdrv])
        gs(drv[:, base + 0:base + 8], mv(l, 1), n1)
        gs(drv[:, base + 24:base + 32], mv(l, 4), n2)
        gs(drv[:, base + 48:base + 56], mv(l, 1, 1), n1)
        for dst, comp, which in ((8, 0, 0), (16, 2, 0), (32, 3, 0), (40, 5, 0), (56, 0, 1)):
            T.op("dve", lambda e, dst=dst, comp=comp, which=which: e.tensor_copy(
                out=drv[:, base + dst:base + dst + 8], in_=mv(l, comp, which)),
                reads=[Rmod], writes=[Rdrv])

    with ExitStack() as es0:
        cs32 = P.sb("cs32", [128, 16], F32, es0)
        wmp = [P.sb(f"wmp{i}", [128, 8, 512], BF16, es0) for i in range(2)]
        Rwmp = [Res("wmp0"), Res("wmp1")]
        T.op("act", lambda e: e.activation(out=cs32[:], in_=vec[:, 0:16], func=AF.Sigmoid),
             reads=[Rvec], writes=[Rcs])
        T.op("dve", lambda e: e.tensor_tensor(out=cs32[:], in0=cs32[:], in1=vec[:, 0:16], op=ALU.mult),
             reads=[Rcs, Rvec], writes=[Rcs])
        T.op("dve", lambda e: e.tensor_copy(out=cs16[:, :, 0], in_=cs32[:, 0:8]), reads=[Rcs], writes=[Rcs])
        T.op("dve", lambda e: e.tensor_copy(out=cs16[:, :, 1], in_=cs32[:, 8:16]), reads=[Rcs], writes=[Rcs])
        for j in range(12):
            mod_piece(0, j, wmp, Rwmp)
        mod_finish(0)
        for nm, off in (("F", 128), ("B", 144)):
            l0 = vcol(f"lb{nm}0")
            l1 = vcol(f"lb{nm}1")
            T.op("dve", lambda e, off=off, l0=l0, l1=l1: e.tensor_tensor(
                out=drv[:, off + 8:off + 16], in0=l1, in1=l0, op=ALU.subtract), reads=[Rvec], writes=[Rdrv])
            T.op("act", lambda e, off=off: e.activation(out=drv[:, off:off + 8], in_=drv[:, off + 8:off + 16],
                                                        func=AF.Sigmoid), reads=[Rdrv], writes=[Rdrv])
            T.op("dve", lambda e, off=off: e.tensor_scalar(out=drv[:, off + 8:off + 16], in0=drv[:, off:off + 8],
                                                           scalar1=-1.0, scalar2=None, op0=ALU.mult),
                 reads=[Rdrv], writes=[Rdrv])
            xo = 160 if nm == "F" else 168
            T.op("act", lambda e, off=off, xo=xo: e.activation(out=drv[:, xo:xo + 8], in_=drv[:, off:off + 8], func=AF.Ln),
                 reads=[Rdrv], writes=[Rdrv])
            T.op("dve", lambda e, off=off, xo=xo: e.tensor_scalar(out=drv[:, xo + 16:xo + 24], in0=drv[:, off:off + 8],
                                                                  scalar1=-1.0, scalar2=1.0, op0=ALU.mult, op1=ALU.add),
                 reads=[Rdrv], writes=[Rdrv])
        T.barrier()

    def dcol(l, off, dc):
        a = l * 64 + off + dc
        return drv[:, a:a + 1]

    T32ALT = {}
    def modulate(es, src, Rsrc, n, gs_off, sh_off, l, dst16, Rdst, dst32=None, Rdst32=None, tmp=None):
        sq, Rsq, rstd, Rrstd, t32, Rt32, bank, Rbank = tmp
        fns = []
        for dc in range(8):
            if dc % 4 == 3:
                T.op("act", lambda e, dc=dc: e.activation(out=sq[:, dc, 0:n], in_=src(dc), func=AF.Square),
                     reads=[Rsrc[dc] if isinstance(Rsrc, list) else Rsrc], writes=[Rsq])
            else:
                T.op("pool", lambda e, dc=dc: e.tensor_tensor(out=sq[:, dc, 0:n], in0=src(dc), in1=src(dc), op=ALU.mult),
                     reads=[Rsrc[dc] if isinstance(Rsrc, list) else Rsrc], writes=[Rsq])
        for dc in range(8):
            fns.append(lambda e, dc=dc: e.matmul(bank[:, 0:n], ones[:], sq[:, dc, 0:n],
                                                  start=(dc == 0), stop=(dc == 7)))
        T.group("pe", fns, reads=[Rsq, Rid], writes=[Rbank])
        T.op("act", lambda e: e.activation(out=rstd[:, 0:n], in_=bank[:, 0:n], func=AF.Ln,
                                           scale=1.0 / D, bias=EPS), reads=[Rbank], writes=[Rrstd])
        T.op("act", lambda e: e.activation(out=rstd[:, 0:n], in_=rstd[:, 0:n], func=AF.Exp, scale=-0.5),
             reads=[Rrstd], writes=[Rrstd])
        alt = T32ALT.get(id(t32))
        for dc in range(8):
            tb, Rtb = (t32, Rt32) if (alt is None or dc % 2 == 0) else alt
            T.op("dve", lambda e, dc=dc, tb=tb: e.tensor_tensor(out=tb[:, 0:n], in0=src(dc), in1=rstd[:, 0:n],
                                                                 op=ALU.mult),
                 reads=[Rsrc[dc] if isinstance(Rsrc, list) else Rsrc, Rrstd], writes=[Rtb])
            if dst32 is not None:
                Rd32 = Rdst32[dc] if isinstance(Rdst32, list) else Rdst32
                T.op("act", lambda e, dc=dc, tb=tb: e.activation(
                    out=dst32(dc), in_=tb[:, 0:n], func=AF.Identity,
                    scale=dcol(l, gs_off, dc), bias=dcol(l, sh_off, dc)), reads=[Rtb, Rdrv], writes=[Rd32])
                T.op("pool", lambda e, dc=dc: e.tensor_copy(out=dst16(dc), in_=dst32(dc)),
                     reads=[Rd32], writes=[Rdst])
            else:
                T.op("act", lambda e, dc=dc, tb=tb: e.activation(
                    out=dst16(dc), in_=tb[:, 0:n], func=AF.Identity,
                    scale=dcol(l, gs_off, dc), bias=dcol(l, sh_off, dc)), reads=[Rtb, Rdrv], writes=[Rdst])

    with ExitStack() as es1:
        hown_d = nc.dram_tensor("hown", [NB_EXT, 128, 8 * TB], BF16, kind="Internal").ap()
        HTr = [Res(f"HT{j}") for j in range(NB_EXT)]
        sq = P.sb("sq", [128, 8, TB], BF16, es1)
        Rsq = Res("sq")
        rstd = P.sb("rstd", [128, TB], F32, es1)
        Rrstd = Res("rstd")
        t32 = P.sb("t32", [128, TB], F32, es1)
        Rt32 = Res("t32")
        T32ALT[id(t32)] = (P.sb("t32_b", [128, TB], F32, es1), Res("t32_b"))
        mtmp = (sq, Rsq, rstd, Rrstd, t32, Rt32, PSB[0], RPS[0])
        NSO = 1 + (L - EXT) // TB
        hso_d = nc.dram_tensor("hso", [NSO, 128, 8 * TB], BF16, kind="Internal").ap()
        Rhso = [Res(f"hso{i}") for i in range(NSO)]
        hp2 = [P.sb(f"hp{i}", [128, 8, TB], BF16, es1) for i in range(2)]
        Rhp2 = [Res("hp0"), Res("hp1")]
        with ExitStack() as esi:
            NSET = 3
            sets = []
            for k in range(NSET):
                hpk = hp2[k] if k < 2 else P.sb("hp_x", [128, 8, TB], BF16, esi)
                Rhpk = Rhp2[k] if k < 2 else Res("hp_x")
                if k == 0:
                    sqk, Rsqk, rstdk, Rrstdk, t32k, Rt32k = sq, Rsq, rstd, Rrstd, t32, Rt32
                else:
                    sqk = P.sb(f"sq_i{k}", [128, 8, TB], BF16, esi)
                    rstdk = P.sb(f"rstd_i{k}", [128, TB], F32, esi)
                    t32k = P.sb(f"t32_i{k}", [128, TB], F32, esi)
                    Rsqk, Rrstdk, Rt32k = Res(f"sq_i{k}"), Res(f"rstd_i{k}"), Res(f"t32_i{k}")
                    T32ALT[id(t32k)] = (P.sb(f"t32_i{k}b", [128, TB], F32, esi), Res(f"t32_i{k}b"))
                xstk = P.sb(f"xst{k}", [128, 8, 128], F32, esi)
                sets.append(dict(hp=hpk, Rhp=Rhpk, xst=xstk, Rxst=Res(f"xst{k}"),
                                 tmp=(sqk, Rsqk, rstdk, Rrstdk, t32k, Rt32k, PSB[k], RPS[k])))

            def stage_task(k, kind, idx):
                st = sets[k]
                hp, Rhp = st["hp"], st["Rhp"]
                if kind == "so":
                    if idx == 0:
                        src_d, t0, gs_off, sh_off = ctxT_d, 0, 48, 56
                    else:
                        src_d, t0, gs_off, sh_off = xT_d, EXT + (idx - 1) * TB, 0, 8
                    xst, Rxst = st["xst"], st["Rxst"]
                    for hf in range(TB // 128):
                        for dc in range(8):
                            T.dma("sp", xst[:, dc, :],
                                  src_d[dc * 128:(dc + 1) * 128, t0 + hf * 128:t0 + (hf + 1) * 128], writes=[Rxst])
                        modulate(esi, lambda dc: xst[:, dc, :], Rxst, 128, gs_off, sh_off, 0,
                                 lambda dc, hf=hf: hp[:, dc, hf * 128:(hf + 1) * 128], Rhp, tmp=st["tmp"])
                    T.dma("sp", hso_d[idx], hp[:].rearrange("p a b -> p (a b)"), reads=[Rhp], writes=[Rhso[idx]])
                else:
                    sl = slice(idx * TB, (idx + 1) * TB)
                    modulate(esi, lambda dc: XT[:, dc, sl], XTr[idx], TB, 0, 8, 0,
                             lambda dc: hp[:, dc, :], Rhp, tmp=st["tmp"])
                    T.dma("sp", hown_d[idx], hp[:].rearrange("p a b -> p (a b)"), reads=[Rhp], writes=[HTr[idx]])

            tasks = [("own", j) for j in range(NB_EXT)] + [("so", bi) for bi in range(NSO)]
            for r in range(0, len(tasks), NSET):
                T.play(*[T.capture(lambda k=k, t=t: stage_task(k, t[0], t[1]))
                         for k, t in enumerate(tasks[r:r + NSET])])
            T.barrier()

        Wgb = [P.sb("Wg0", [128, 8, 1280], BF16, es1), None]
        RWgb = [Res("Wg0"), Res("Wg1")]
        cur = [0]

        class _WgView:
            def __getitem__(self, _):
                return Wgb[cur[0]]
        Wg = _WgView()

        Wo = P.sb("Wo", [128, 2, 1024], BF16, es1)
        RWo = Res("Wo")
        qd = [[P.sb(f"qd{h}{d}", [128, TB], BF16, es1) for d in range(2)] for h in range(2)]
        kd = [[P.sb(f"kd{h}{d}", [128, TB], BF16, es1) for d in range(2)] for h in range(2)]
        qc = [[P.sb(f"qc{h}{d}", [128, TB], F32, es1) for d in range(2)] for h in range(2)]
        klT = [[P.sb(f"klT{h}{d}", [128, TB], BF16, es1) for d in range(2)] for h in range(2)]
        Rops = [[Res(f"ops{h}{d}") for d in range(2)] for h in range(2)]
        RklT = [[Res(f"klT{h}{d}") for d in range(2)] for h in range(2)]
        decs = P.sb("decs", [128, 2, 2, TB // 64], F32, es1)
        Rdec = [[Res(f"dec{h}{d}") for d in range(2)] for h in range(2)]
        qT = [P.sb(f"qT{h}", [128, TB], F32, es1) for h in range(2)]
        RqT = [Res("qT0"), Res("qT1")]
        sg2 = [P.sb(f"sg{i}", [128, 2, TB], F32, es1) for i in range(3)]
        Rsg2 = [[Res(f"sg{i}{h}") for h in range(2)] for i in range(3)]
        tmpb = [[P.sb(f"gt{h}_{i}", [128, TB], F32, es1) for i in range(7)] for h in range(2)]
        tmpr = [[Res(f"gt{h}_{i}") for i in range(7)] for h in range(2)]
        kl_tm = [P.sb(f"kl_tm{d}", [128, TB // 128, 2, 128], BF16, es1) for d in range(2)]
        Rkl_tm = [Res("kl_tm0"), Res("kl_tm1")]
        v_tm = [P.sb(f"v_tm{i}", [128, TB // 128, 256], BF16, es1) for i in range(2)]
        Rv_tm = [Res("v_tm0"), Res("v_tm1")]
        sTm = [P.sb(f"sTm{i}", [128, 2, 128], BF16, es1) for i in range(2)]
        RsTm = [Res("sTm0"), Res("sTm1")]
        o_sb2 = [P.sb(f"o_sb{i}", [128, 2, TB], F32, es1) for i in range(2)]
        Ro_sb2 = [Res("o_sb0"), Res("o_sb1")]
        yT = P.sb("yT", [128, 2, TB], BF16, es1)
        RyT = Res("yT")
        S = P.sb("S", [128, 2, 2, 128], F32, es1)
        RS = [Res("S_F"), Res("S_B")]
        RS16 = [Res("S16_F"), Res("S16_B")]
        o_b = P.sb("o_b", [128, 2, EXT], BF16, es1)
        Ro_b = [Res(f"o_b{j}") for j in range(NB_EXT)]
        for i in range(2):
            T.op("pool", lambda e, i=i: e.memset(sTm[i][:], 0.0), writes=[RsTm[i]])

        B_PROJ = [1, 2]
        B_SC = 3
        B_KV = 4
        B_O = 5
        B_Y = 6
        proj_rr = [0]

        def next_proj_bank():
            b = B_PROJ[proj_rr[0] % 2]
            proj_rr[0] += 1
            return b

        OML = {0: 128, 1: 144}

        def gate_elementwise(gi, hh, d, hsrc, Rh, n, main, slot=None):
            slot = d if slot is None else slot
            sT, lf, cum, dA, ndL, ee0_, ee1_ = tmpb[hh]
            RsT, Rlf, Rcum, RdA, RndL, Ree0_, Ree1_ = tmpr[hh]
            ee = [ee0_, ee1_]
            Ree = [Ree0_, Ree1_]
            h = 2 * gi + hh
            zcol = (1 + d) * 256 + hh * 128
            pb = 1 + hh
            bank = PSB[pb]
            T.group("pe", [lambda e, dc=dc: e.matmul(bank[:, 0:n], Wg[0][:, dc, zcol:zcol + 128], hsrc(dc),
                                                      start=(dc == 0), stop=(dc == 7)) for dc in range(8)],
                    reads=[RWgb[cur[0]], Rh], writes=[RPS[pb]])
            lnoml = drv[:, 160 + 8 * d + h:160 + 8 * d + h + 1]
            lbc = drv[:, 176 + 8 * d + h:176 + 8 * d + h + 1]
            nch = n // 64
            l1 = ee[1]
            Rl1 = Ree[1]
            T.op("act", lambda e: e.activation(out=sT[:, 0:n], in_=bank[:, 0:n], func=AF.Exp),
                 reads=[RPS[pb]], writes=[RsT])
            T.op("act", lambda e: e.activation(out=l1[:, 0:n], in_=sT[:, 0:n], func=AF.Ln, bias=1.0),
                 reads=[RsT], writes=[Rl1])
            T.op("act", lambda e: e.activation(out=lf[:, 0:n], in_=sT[:, 0:n], func=AF.Ln, bias=lbc),
                 reads=[RsT, Rdrv], writes=[Rlf])
            T.op("pool", lambda e: e.tensor_tensor(out=lf[:, 0:n], in0=lf[:, 0:n], in1=l1[:, 0:n], op=ALU.subtract),
                 reads=[Rlf, Rl1], writes=[Rlf])
            T.op("dve", lambda e: e.tensor_tensor_scan(out=cum[:, 0:n], data0=scanmask[:, 0:n], data1=lf[:, 0:n],
                                                        initial=0.0, op0=ALU.mult, op1=ALU.add),
                 reads=[Rlf, Rcst], writes=[Rcum])
            cum3 = cum[:, 0:n].rearrange("p (c t) -> p c t", t=64)
            T.op("act", lambda e: e.activation(out=decs[:, hh, slot, 0:nch], in_=cum3[:, :, 63], func=AF.Exp),
                 reads=[Rcum], writes=[Rdec[hh][slot]])
            T.op("pool", lambda e: e.tensor_tensor(
                out=ndL[:, 0:n].rearrange("p (c t) -> p c t", t=64), in0=cum3,
                in1=cum3[:, :, 63:64].to_broadcast([128, nch, 64]), op=ALU.subtract),
                reads=[Rcum], writes=[RndL])
            if d == 1:
                T.op("pool", lambda e: e.tensor_tensor(out=ndL[:, 0:n], in0=ndL[:, 0:n], in1=lf[:, 0:n],
                                                       op=ALU.subtract), reads=[RndL, Rlf], writes=[RndL])
                T.op("pool", lambda e: e.tensor_tensor(out=cum[:, 0:n], in0=cum[:, 0:n], in1=lf[:, 0:n],
                                                       op=ALU.subtract), reads=[Rcum, Rlf], writes=[Rcum])
            A = cum
            RA = Rcum
            if d == 0:
                T.op("dve", lambda e: e.scalar_tensor_tensor(out=ee[0][:, 0:n], in0=ndL[:, 0:n], scalar=-1.0,
                                                             in1=l1[:, 0:n], op0=ALU.mult, op1=ALU.subtract),
                     reads=[RndL, Rl1], writes=[Ree[0]])
            else:
                T.op("dve", lambda e: e.tensor_tensor(out=ee[0][:, 0:n], in0=A[:, 0:n], in1=l1[:, 0:n], op=ALU.subtract),
                     reads=[RA, Rl1], writes=[Ree[0]])
            T.op("act", lambda e: e.activation(out=klT[hh][slot][:, 0:n], in_=ee[0][:, 0:n], func=AF.Exp, bias=lnoml),
                 reads=[Ree[0], Rdrv], writes=[RklT[hh][slot]])
            if not main:
                return
            refi = 31 if d == 0 else 32
            A3 = A[:, 0:n].rearrange("p (c t) -> p c t", t=64)
            T.op("pool", lambda e: e.tensor_tensor(
                out=dA[:, 0:n].rearrange("p (c t) -> p c t", t=64), in0=A3,
                in1=A3[:, :, refi:refi + 1].to_broadcast([128, nch, 64]), op=ALU.subtract),
                reads=[RA], writes=[RdA])
            sq_, sk_ = (1.0, -1.0) if d == 0 else (-1.0, 1.0)
            T.op("dve", lambda e: e.scalar_tensor_tensor(out=ee[0][:, 0:n], in0=dA[:, 0:n], scalar=sk_,
                                                         in1=l1[:, 0:n], op0=ALU.mult, op1=ALU.subtract),
                 reads=[RdA, Rl1], writes=[Ree[0]])
            T.op("act", lambda e: e.activation(out=kd[hh][slot][:, 0:n], in_=ee[0][:, 0:n], func=AF.Exp, bias=lnoml),
                 reads=[Ree[0], Rdrv], writes=[Rops[hh][slot]])
            T.op("act", lambda e: e.activation(out=sT[:, 0:n], in_=dA[:, 0:n], func=AF.Exp, scale=sq_),
                 reads=[RdA], writes=[RsT])
            T.op("dve", lambda e: e.tensor_tensor(out=qd[hh][slot][:, 0:n], in0=qT[hh][:, 0:n], in1=sT[:, 0:n],
                                                  op=ALU.mult), reads=[RqT[hh], RsT], writes=[Rops[hh][slot]])
            if d == 0:
                T.op("act", lambda e: e.activation(out=ndL[:, 0:n], in_=A[:, 0:n], func=AF.Exp),
                     reads=[RA], writes=[RndL])
            else:
                T.op("act", lambda e: e.activation(out=ndL[:, 0:n], in_=ndL[:, 0:n], func=AF.Exp, scale=-1.0),
                     reads=[RndL], writes=[RndL])
            T.op("pool", lambda e: e.tensor_tensor(out=qc[hh][slot][:, 0:n], in0=qT[hh][:, 0:n], in1=ndL[:, 0:n],
                                                   op=ALU.mult), reads=[RqT[hh], RndL], writes=[Rops[hh][slot]])

        def v_project(hsrc, Rh, n, vs=0):
            nt = n // 128
            pb = 1
            bank = PSB[pb]
            fns = []
            for ti in range(nt):
                for dc in range(8):
                    fns.append(lambda e, ti=ti, dc=dc: e.matmul(
                        bank[:, ti * 256:(ti + 1) * 256], hsrc(dc)[:, ti * 128:(ti + 1) * 128],
                        Wg[0][:, dc, 1024:1280], start=(dc == 0), stop=(dc == 7)))
            T.group("pe", fns, reads=[RWgb[cur[0]], Rh], writes=[RPS[pb]])
            T.op("act", lambda e: e.activation(
                out=v_tm[vs][:, 0:nt, :], in_=bank[:, 0:nt * 256].rearrange("p (a b) -> p a b", b=256), func=AF.Copy),
                reads=[RPS[pb]], writes=[Rv_tm[vs]])

        def kl_transpose(d, n):
            nt = n // 128
            half = RPSH[d]
            base = d * 512
            fns = []
            for ti in range(nt):
                for hh in range(2):
                    o0 = base + (ti * 2 + hh) * 128
                    fns.append(lambda e, ti=ti, hh=hh, o0=o0: e.transpose(
                        PSH[:, o0:o0 + 128], klT[hh][d][:, ti * 128:(ti + 1) * 128], ident[:]))
            T.group("pe", fns, reads=[RklT[0][d], RklT[1][d], Rid], writes=[half])
            T.op("act", lambda e: e.activation(
                out=kl_tm[d][:, 0:nt].rearrange("p a h k -> p (a h k)"), in_=PSH[:, base:base + nt * 256],
                func=AF.Copy), reads=[half], writes=[Rkl_tm[d]])

        def kv_bank(chunk):
            if chunk % 2 == 0:
                return PSB[B_KV][:, 0:256].rearrange("p (h v) -> p h v", v=128), RPS[B_KV]
            return PSB[0][:, 256:512].rearrange("p (h v) -> p h v", v=128), RPS[0]

        def kv_mm(ti, j, chunk, slot, vs):
            kvb, Rkv = kv_bank(chunk)
            fns = []
            for hh in range(2):
                fns.append(lambda e, hh=hh: e.matmul(
                    kvb[:, hh, :], kl_tm[slot][64 * j:64 * j + 64, ti, hh, :],
                    v_tm[vs][64 * j:64 * j + 64, ti, hh * 128:(hh + 1) * 128], start=True, stop=True))
            T.group("pe", fns, reads=[Rkl_tm[slot], Rv_tm[vs]], writes=[Rkv])

        def s_update(d, chunk, slot):
            kvb, Rkv = kv_bank(chunk)
            for hh in range(2):
                T.op("dve", lambda e, hh=hh: e.scalar_tensor_tensor(
                    out=S[:, d, hh, :], in0=S[:, d, hh, :], scalar=decs[:, hh, slot, chunk:chunk + 1],
                    in1=kvb[:, hh, :], op0=ALU.mult, op1=ALU.add),
                    reads=[RS[d], Rkv, Rdec[hh][slot]], writes=[RS[d]])

        def state_step(d, ti, j, chunk, slot=None, vs=0):
            slot = d if slot is None else slot
            kv_mm(ti, j, chunk, slot, vs)
            s_update(d, chunk, slot)

        def zero_state(d):
            T.op("pool", lambda e: e.memset(S[:, d].rearrange("p h v -> p (h v)"), 0.0), writes=[RS[d]])

        def so_prep(gi, d, slot, bi_so, part):
            hp, Rhp = hp2[slot], Rhp2[slot]
            hsrc = lambda dc: hp[:, dc, :]
            if part == "pre":
                T.dma("sp", hp[:].rearrange("p a b -> p (a b)"), hso_d[bi_so], reads=[Rhso[bi_so]], writes=[Rhp])
            elif part == "post":
                kl_transpose(slot, TB)
            elif part == 0:
                v_project(hsrc, Rhp, TB, slot)
                gate_elementwise(gi, 0, d, hsrc, Rhp, TB, main=False, slot=slot)
            else:
                gate_elementwise(gi, 1, d, hsrc, Rhp, TB, main=False, slot=slot)

        def so_steps(d, slot):
            nch = TB // 64
            order = range(nch) if d == 0 else range(nch - 1, -1, -1)
            for chunk in order:
                state_step(d, chunk // 2, chunk % 2, chunk, slot=slot, vs=slot)

        def m_prep(gi, j, dd, slot, part):
            hp, Rh = hp2[slot], Rhp2[slot]
            hsrc = lambda dc: hp[:, dc, :]
            n = TB
            sg = sg2[j % 3]
            if part == "pre":
                T.dma("sp", hp[:].rearrange("p a b -> p (a b)"), hown_d[j], reads=[HTr[j]], writes=[Rh])
                return
            if part == "post":
                kl_transpose(slot, n)
                return
            hh = part
            if part == 0:
                v_project(hsrc, Rh, n, slot)
            kinds = [0] if dd == 1 else [0, 3]
            for kind in kinds:
                dstT, Rdst = (qT[hh], RqT[hh]) if kind == 0 else (None, Rsg2[j % 3][hh])
                col = kind * 256 + hh * 128
                pb = 1 + hh
                bank = PSB[pb]
                T.group("pe", [lambda e, dc=dc, col=col, bank=bank: e.matmul(
                    bank[:, 0:n], Wg[0][:, dc, col:col + 128], hsrc(dc), start=(dc == 0), stop=(dc == 7))
                    for dc in range(8)], reads=[RWgb[cur[0]], Rh], writes=[RPS[pb]])
                dst = dstT[:, 0:n] if dstT is not None else sg[:, hh, 0:n]
                T.op("act", lambda e, dst=dst, bank=bank: e.activation(out=dst, in_=bank[:, 0:n],
                                                                        func=AF.Exp, scale=-1.0),
                     reads=[RPS[pb]], writes=[Rdst])
                T.op("act", lambda e, dst=dst: e.activation(out=dst, in_=dst, func=AF.Ln, bias=1.0),
                     reads=[Rdst], writes=[Rdst])
                T.op("act", lambda e, dst=dst: e.activation(out=dst, in_=dst, func=AF.Exp, scale=-1.0),
                     reads=[Rdst], writes=[Rdst])
                T.op("dve", lambda e, dst=dst, bank=bank: e.tensor_tensor(out=dst, in0=dst, in1=bank[:, 0:n],
                                                                           op=ALU.mult),
                     reads=[Rdst, RPS[pb]], writes=[Rdst])
            gate_elementwise(gi, hh, dd, hsrc, Rh, n, main=True, slot=slot)

        def m_loop(gi, j, dd, slot):
            o_sb, Ro_sb = o_sb2[j % 2], Ro_sb2[j % 2]
            n = TB
            nch = n // 64
            vs = slot
            tri = triF if dd == 0 else triB
            order = list(range(nch)) if dd == 0 else list(range(nch - 1, -1, -1))
            done_tiles = set()
            for chunk in order:
                ti, jj = chunk // 2, chunk % 2
                sb_i = ti % 2
                if ti not in done_tiles:
                    done_tiles.add(ti)
                    scb = PSB[B_SC][:, sb_i * 256:(sb_i + 1) * 256].rearrange("p (h c) -> p h c", c=128)
                    T.group("pe", [lambda e, hh=hh, ti=ti, scb=scb: e.matmul(
                        scb[:, hh, :], kd[hh][slot][:, ti * 128:(ti + 1) * 128],
                        qd[hh][slot][:, ti * 128:(ti + 1) * 128], start=True, stop=True) for hh in range(2)],
                        reads=[Rops[0][slot], Rops[1][slot]], writes=[RPS[B_SC]])
                    for blk in range(2):
                        p0 = blk * 64
                        T.op("dve", lambda e, p0=p0, sb_i=sb_i, scb=scb: e.tensor_tensor(
                            out=sTm[sb_i][p0:p0 + 64, :, p0:p0 + 64], in0=scb[p0:p0 + 64, :, p0:p0 + 64],
                            in1=tri[p0:p0 + 64, :].unsqueeze(1).to_broadcast([64, 2, 64]), op=ALU.mult),
                            reads=[RPS[B_SC], Rcst], writes=[RsTm[sb_i]])
                kv_mm(ti, jj, chunk, slot, vs)
                qtr = chunk % 4
                ob = PSB[B_O][:, qtr * 128:(qtr + 1) * 128].rearrange("p (h c) -> p h c", c=64)
                fns = []
                p0 = jj * 64
                for hh in range(2):
                    fns.append(lambda e, hh=hh, p0=p0, ti=ti, sb_i=sb_i, ob=ob: e.matmul(
                        ob[:, hh, :], v_tm[vs][p0:p0 + 64, ti, hh * 128:(hh + 1) * 128],
                        sTm[sb_i][p0:p0 + 64, hh, p0:p0 + 64], start=True, stop=False))
                    fns.append(lambda e, hh=hh, chunk=chunk, ob=ob: e.matmul(
                        ob[:, hh, :], S[:, dd, hh, :], qc[hh][slot][:, chunk * 64:(chunk + 1) * 64],
                        start=False, stop=True))
                T.group("pe", fns, reads=[Rv_tm[vs], RsTm[sb_i], RS[dd], Rops[0][slot], Rops[1][slot]],
                        writes=[RPS[B_O]])
                if dd == 0:
                    osl = o_sb[:, :, chunk * 64:(chunk + 1) * 64]
                    T.op("act", lambda e, osl=osl, ob=ob: e.activation(out=osl, in_=ob, func=AF.Copy),
                         reads=[RPS[B_O]], writes=[Ro_sb])
                else:
                    osl = o_b[:, :, j * TB + chunk * 64:j * TB + (chunk + 1) * 64]
                    T.op("act", lambda e, osl=osl, ob=ob: e.activation(out=osl, in_=ob, func=AF.Copy),
                         reads=[RPS[B_O]], writes=[Ro_b[j]])
                s_update(dd, chunk, slot)

        def m_readout(gi, j):
            o_sb, Ro_sb = o_sb2[j % 2], Ro_sb2[j % 2]
            n = TB
            sl = slice(j * TB, (j + 1) * TB)
            sg = sg2[j % 3]
            T.op("dve", lambda e: e.tensor_tensor(out=o_sb[:], in0=o_sb[:], in1=o_b[:, :, sl], op=ALU.add),
                 reads=[Ro_sb, Ro_b[j]], writes=[Ro_sb])
            for hh in range(2):
                T.op("act", lambda e, hh=hh: e.activation(out=sq[:, hh, 0:n], in_=o_sb[:, hh, :], func=AF.Square),
                     reads=[Ro_sb], writes=[Rsq])
            for hh in range(2):
                bank = PSB[B_Y]
                T.group("pe", [lambda e, hh=hh, bank=bank: e.matmul(bank[:, 0:n], ones[:], sq[:, hh, 0:n],
                                                                     start=True, stop=True)],
                        reads=[Rsq, Rid], writes=[RPS[B_Y]])
                T.op("act", lambda e, bank=bank: e.activation(out=rstd[:, 0:n], in_=bank[:, 0:n], func=AF.Ln,
                                                               scale=1.0 / 128, bias=EPS),
                     reads=[RPS[B_Y]], writes=[Rrstd])
                T.op("act", lambda e: e.activation(out=rstd[:, 0:n], in_=rstd[:, 0:n], func=AF.Exp, scale=-0.5),
                     reads=[Rrstd], writes=[Rrstd])
                T.op("dve", lambda e, hh=hh: e.tensor_tensor(out=t32[:, 0:n], in0=o_sb[:, hh, :], in1=rstd[:, 0:n],
                                                              op=ALU.mult), reads=[Ro_sb, Rrstd], writes=[Rt32])
                T.op("dve", lambda e, hh=hh: e.scalar_tensor_tensor(
                    out=yT[:, hh, :], in0=t32[:, 0:n], scalar=vec[:, VEC["gnorm"]:VEC["gnorm"] + 1],
                    in1=sg[:, hh, 0:n], op0=ALU.mult, op1=ALU.mult), reads=[Rt32, Rsg2[j % 3][hh], Rvec], writes=[RyT])
            for oc in range(8):
                by = B_Y
                bank = PSB[by][:, (oc % 2) * 256:(oc % 2) * 256 + 256]
                T.group("pe", [lambda e, hh=hh, oc=oc, bank=bank: e.matmul(
                    bank[:, 0:n], Wo[:, hh, oc * 128:(oc + 1) * 128], yT[:, hh, :], start=(hh == 0), stop=(hh == 1))
                    for hh in range(2)], reads=[RWo, RyT], writes=[RPS[by]])
                T.op("dve", lambda e, oc=oc, bank=bank: e.scalar_tensor_tensor(
                    out=XT[:, oc, sl], in0=bank[:, 0:n], scalar=dcol(0, 16, oc), in1=XT[:, oc, sl],
                    op0=ALU.mult, op1=ALU.add), reads=[RPS[by], XTr[j][oc], Rdrv], writes=[XTr[j][oc]])

        def zero_state(d):
            T.op("pool", lambda e: e.memset(S[:, d].rearrange("p h v -> p (h v)"), 0.0), writes=[RS[d]])

        cap = T.capture
        esw = ExitStack()
        wmp1 = [P.sb(f"wmpL1_{i}", [128, 8, 512], BF16, esw) for i in range(2)]
        Rwmp1 = [Res("wmpL1_0"), Res("wmpL1_1")]
        T.dma("pool", Wgb[0][:], w_in_d[0], writes=[RWgb[0]])
        for gi in range(4):
            cur[0] = gi % 2
            if gi >= 1 and gi + 1 < 4:
                T.dma("pool", Wgb[(gi + 1) % 2][:], w_in_d[gi + 1], writes=[RWgb[(gi + 1) % 2]])
            T.dma("pool", Wo[:], w_out_d[gi], writes=[RWo])
            for d in (1, 0):
                if gi == 0 and d == 0:
                    T.barrier()
                    esw.close()
                    Wgb[1] = P.sb("Wg1", [128, 8, 1280], BF16, es1)
                    T.dma("pool", Wgb[1][:], w_in_d[1], writes=[RWgb[1]])
                zero_state(d)
                stages = [("so", 0)]
                if d == 1:
                    stages += [("so", 1 + (j - NB_EXT)) for j in range(L // TB - 1, NB_EXT - 1, -1)]
                    stages += [("m", j) for j in range(NB_EXT - 1, -1, -1)]
                else:
                    stages += [("m", j) for j in range(NB_EXT)]

                def prep(i, part):
                    kind, a = stages[i]
                    slot = i % 2
                    if kind == "so":
                        so_prep(gi, d, slot, a, part)
                    else:
                        m_prep(gi, a, d, slot, part)

                def body(i):
                    kind, a = stages[i]
                    slot = i % 2
                    if kind == "so":
                        so_steps(d, slot)
                    else:
                        m_loop(gi, a, d, slot)

                prep(0, "pre")
                T.play(cap(lambda: prep(0, 0)), cap(lambda: prep(0, 1)))
                prep(0, "post")
                pending = None
                for i in range(len(stages)):
                    streams = [cap(lambda: body(i))]
                    if pending is not None:
                        streams.append(cap(lambda: m_readout(gi, pending)))
                    if i + 1 < len(stages):
                        prep(i + 1, "pre")
                        streams += [cap(lambda: prep(i + 1, 0)), cap(lambda: prep(i + 1, 1))]
                        if gi == 0 and d == 1 and i < 12:
                            streams.append(cap(lambda: mod_piece(1, i, wmp1, Rwmp1)))
                    T.play(*streams)
                    if i + 1 < len(stages):
                        prep(i + 1, "post")
                    if gi == 0 and d == 1 and i == 11:
                        mod_finish(1)
                    pending = stages[i][1] if (d == 0 and stages[i][0] == "m") else None
                if pending is not None:
                    m_readout(gi, pending)
        T.barrier()

    if stage == "x1":
        return _finish(P, T, XT, XTr, dbg_d, out_d, debug=True)

    def moe_phase(l, ntok):
        nb = ntok // TB
        MB = 512
        nmb = ntok // MB
        with ExitStack() as es2:
            H2 = P.sb(f"H2_{l}", [128, 8, ntok], BF16, es2)
            RH2 = [Res(f"H2_{j}") for j in range(nmb)]
            h32b = [P.sb(f"h32_{l}_{i}", [128, 8, TB], F32, es2) for i in range(2)]
            Rh32b = [[Res(f"h32_{i}_{dc}") for dc in range(8)] for i in range(2)]
            sq = P.sb(f"msq_{l}", [128, 8, TB], BF16, es2)
            rstd = P.sb(f"mrstd_{l}", [128, TB], F32, es2)
            t32 = P.sb(f"mt32_{l}", [128, TB], F32, es2)
            T32ALT[id(t32)] = (P.sb(f"mt32b_{l}", [128, TB], F32, es2), Res("mt32b"))
            mtmp = (sq, Res("msq"), rstd, Res("mrstd"), t32, Res("mt32"), PSB[0], RPS[0])
            rw = P.sb(f"rw_{l}", [128, 8, NE], F32, es2)
            Rrw = Res("rw")
            combT = P.sb(f"combT_{l}", [16, ntok], F32, es2)
            RcombT = [Res(f"combT{j}") for j in range(nmb)]
            rs = P.sb(f"rs_{l}", [128, 2, NE], F32, es2)
            rsel = P.sb(f"rsel_{l}", [128, 2, NE], F32, es2)
            req = P.sb(f"req_{l}", [128, 2, NE], F32, es2)
            rsel2 = P.sb(f"rsel2_{l}", [128, 2, NE], F32, es2)
            rmk = P.sb(f"rmk_{l}", [128, 2, NE], F32, es2)
            rm1 = P.sb(f"rm1_{l}", [128, 8], F32, es2)
            rm2 = P.sb(f"rm2_{l}", [128, 8], F32, es2)
            rgs = P.sb(f"rgs_{l}", [128, 8], F32, es2)
            rgm = P.sb(f"rgm_{l}", [128, 8], F32, es2)
            rgx = P.sb(f"rgx_{l}", [128, 2], F32, es2)
            rws = P.sb(f"rws_{l}", [128, 2], F32, es2)
            comb = P.sb(f"comb_{l}", [128, 2, NE], F32, es2)
            Rr = Res("route")
            T.dma("sp", rw[:], router_d[:, :, :], writes=[Rrw])
            gs_off, sh_off = 24, 32
            rb = vec[:, VEC["rb"]:VEC["rb"] + NE]
            def rt_mod(j):
                sl = slice(j * TB, (j + 1) * TB)
                mbi = (j * TB) // MB
                h32, Rh32 = h32b[j % 2], Rh32b[j % 2]
                modulate(es2, lambda dc: XT[:, dc, sl], XTr[j], TB, gs_off, sh_off, l,
                         lambda dc: H2[:, dc, sl], RH2[mbi],
                         dst32=lambda dc: h32[:, dc, :], Rdst32=Rh32, tmp=mtmp)

            def rt_route(j):
                sl = slice(j * TB, (j + 1) * TB)
                mbi = (j * TB) // MB
                h32, Rh32 = h32b[j % 2], Rh32b[j % 2]
                lg = PSB[1][:, 0:2 * NE].rearrange("p (a e) -> p a e", e=NE)
                fns = []
                for ti in range(2):
                    for dc in range(8):
                        fns.append(lambda e, ti=ti, dc=dc: e.matmul(
                            lg[:, ti, :], h32[:, dc, ti * 128:(ti + 1) * 128], rw[:, dc, :],
                            start=(dc == 0), stop=(dc == 7)))
                T.group("pe", fns, reads=Rh32 + [Rrw], writes=[RPS[1]])
                T.op("act", lambda e: e.activation(out=rs[:], in_=lg, func=AF.Exp, scale=-1.0), reads=[RPS[1]], writes=[Rr])
                T.op("dve", lambda e: e.tensor_scalar(out=rs[:], in0=rs[:], scalar1=1.0, scalar2=None, op0=ALU.add),
                     reads=[Rr], writes=[Rr])
                T.op("dve", lambda e: e.reciprocal(out=rs[:], in_=rs[:]), reads=[Rr], writes=[Rr])
                dv = lambda fn, rd=(), : T.op("dve", fn, reads=[Rr] + list(rd), writes=[Rr])
                g4 = lambda t: t[:].rearrange("p a (g k) -> p (a g) k", k=4)
                dv(lambda e: e.tensor_tensor(out=rsel[:], in0=rs[:], in1=rb.unsqueeze(1).to_broadcast([128, 2, NE]),
                                             op=ALU.add), rd=[Rvec])
                dv(lambda e: e.tensor_reduce(out=rm1[:], in_=g4(rsel), axis=mybir.AxisListType.X, op=ALU.max))
                dv(lambda e: e.tensor_tensor(out=g4(req), in0=g4(rsel), in1=rm1[:].unsqueeze(2).to_broadcast([128, 8, 4]),
                                             op=ALU.is_equal))
                dv(lambda e: e.scalar_tensor_tensor(out=rsel2[:], in0=req[:], scalar=-1.0e9, in1=rsel[:],
                                                    op0=ALU.mult, op1=ALU.add))
                dv(lambda e: e.tensor_reduce(out=rm2[:], in_=g4(rsel2), axis=mybir.AxisListType.X, op=ALU.max))
                dv(lambda e: e.tensor_tensor(out=rgs[:], in0=rm1[:], in1=rm2[:], op=ALU.add))
                dv(lambda e: e.tensor_reduce(out=rgx[:], in_=rgs[:].rearrange("p (a g) -> p a g", g=4),
                                             axis=mybir.AxisListType.X, op=ALU.max))
                dv(lambda e: e.tensor_tensor(out=rgm[:].rearrange("p (a g) -> p a g", g=4),
                                             in0=rgs[:].rearrange("p (a g) -> p a g", g=4),
                                             in1=rgx[:].unsqueeze(2).to_broadcast([128, 2, 4]), op=ALU.is_equal))
                dv(lambda e: e.tensor_tensor(out=g4(rmk), in0=g4(rsel), in1=rm2[:].unsqueeze(2).to_broadcast([128, 8, 4]),
                                             op=ALU.is_ge))
                dv(lambda e: e.tensor_tensor(out=g4(rmk), in0=g4(rmk), in1=rgm[:].unsqueeze(2).to_broadcast([128, 8, 4]),
                                             op=ALU.mult))
                dv(lambda e: e.tensor_tensor(out=rmk[:], in0=rmk[:], in1=rs[:], op=ALU.mult))
                dv(lambda e: e.tensor_reduce(out=rws[:], in_=rmk[:], axis=mybir.AxisListType.X, op=ALU.add))
                dv(lambda e: e.reciprocal(out=rws[:], in_=rws[:]))
                dv(lambda e: e.tensor_tensor(out=comb[:], in0=rmk[:], in1=rws[:].unsqueeze(2).to_broadcast([128, 2, NE]),
                                             op=ALU.mult))
                tp = PSB[2][0:16, 0:256]
                T.group("pe", [lambda e, ti=ti: e.transpose(tp[:, ti * 128:(ti + 1) * 128], comb[:, ti, :],
                                                            cst32[:, CST["ident"]:CST["ident"] + 128])
                               for ti in range(2)], reads=[Rr, Rcst], writes=[RPS[2]])
                T.op("act", lambda e: e.activation(out=combT[:, sl], in_=tp, func=AF.Copy),
                     reads=[RPS[2]], writes=[RcombT[mbi]])
            rt_mod(0)
            for j in range(nb):
                nxt = T.capture(lambda: rt_mod(j + 1)) if j + 1 < nb else []
                T.play(T.capture(lambda: rt_route(j)), nxt)
            if stage == f"comb{l}":
                return "comb", combT
            cmb_d = nc.dram_tensor(f"cmb_scratch{l}", [16, ntok], F32, kind="Internal").ap()
            Rcmb_d = Res("cmb_d")
            T.dma("sp", cmb_d[:, :], combT[:, :], reads=RcombT, writes=[Rcmb_d])
            wgu = [P.sb(f"wgu{i}_{l}", [128, 8, 512], BF16, es2) for i in range(2)]
            wdn = [P.sb(f"wdn{i}_{l}", [128, 2, 1024], BF16, es2) for i in range(2)]
            Rwgu = [Res("wgu0"), Res("wgu1")]
            Rwdn = [Res("wdn0"), Res("wdn1")]
            cmb_sb = P.sb(f"cmb_sb_{l}", [128, MB], F32, es2)
            Rcmb = Res("cmb")
            s_sb = [P.sb(f"s_sb{i}_{l}", [128, MB], F32, es2) for i in range(2)]
            Rs_sb = [Res("s_sb0"), Res("s_sb1")]
            t_sb = [P.sb(f"t_sb{i}_{l}", [128, MB], F32, es2) for i in range(2)]
            Rt_sb = [Res("t_sb0"), Res("t_sb1")]
            a_sb = [P.sb(f"a_sb{i}_{l}", [128, 2, MB], BF16, es2) for i in range(2)]
            Ra_sb = [[Res(f"a{i}{f}") for f in range(2)] for i in range(2)]
            g2_off = 40
            cmb2 = [cmb_sb, P.sb(f"cmb_sb2_{l}", [128, MB], F32, es2)]
            Rcmb2 = [Rcmb, Res("cmb2")]
            iters = [(ex, mb) for ex in range(NE) for mb in range(nmb)]

            def load_w(ex):
                wb = ex % 2
                T.dma("pool", wgu[wb][:], wgu_d[l, ex], writes=[Rwgu[wb]])
                T.dma("pool", wdn[wb][:], wd_d[l, ex], writes=[Rwdn[wb]])

            def emit_gu(it):
                ex, mb = iters[it]
                wb = ex % 2
                ab = it % 2
                msl = slice(mb * MB, (mb + 1) * MB)
                T.dma("sp", cmb2[ab][:], cmb_d[ex:ex + 1, msl].to_broadcast([128, MB]),
                      reads=[Rcmb_d], writes=[Rcmb2[ab]])
                for fc in range(2):
                    bg, bu = 1 + fc, 3 + fc
                    T.group("pe", [lambda e, dc=dc, fc=fc, bg=bg: e.matmul(
                        PSB[bg][:, :], wgu[wb][:, dc, fc * 128:(fc + 1) * 128], H2[:, dc, msl],
                        start=(dc == 0), stop=(dc == 7)) for dc in range(8)],
                        reads=[Rwgu[wb], RH2[mb]], writes=[RPS[bg]], cost=1.8)
                    T.group("pe", [lambda e, dc=dc, fc=fc, bu=bu: e.matmul(
                        PSB[bu][:, :], wgu[wb][:, dc, 256 + fc * 128:256 + (fc + 1) * 128], H2[:, dc, msl],
                        start=(dc == 0), stop=(dc == 7)) for dc in range(8)],
                        reads=[Rwgu[wb], RH2[mb]], writes=[RPS[bu]], cost=1.8)
                    T.op("act", lambda e, fc=fc, bg=bg: e.activation(out=s_sb[fc][:], in_=PSB[bg][:, :], func=AF.Silu),
                         reads=[RPS[bg]], writes=[Rs_sb[fc]], cost=0.65)
                    T.op("pool", lambda e, fc=fc: e.tensor_tensor(out=t_sb[fc][:], in0=s_sb[fc][:], in1=cmb2[ab][:], op=ALU.mult),
                         reads=[Rs_sb[fc], Rcmb2[ab]], writes=[Rt_sb[fc]], cost=1.3)
                    T.op("dve", lambda e, fc=fc, bu=bu: e.tensor_tensor(out=a_sb[ab][:, fc, :], in0=PSB[bu][:, :], in1=t_sb[fc][:],
                                                          op=ALU.mult),
                         reads=[RPS[bu], Rt_sb[fc]], writes=[Ra_sb[ab][fc]], cost=0.7)

            ysb = [P.sb(f"ysb{i}_{l}", [128, MB], F32, es2) for i in range(2)]
            Rysb = [Res("ysb0"), Res("ysb1")]

            def emit_down(it):
                ex, mb = iters[it]
                wb = ex % 2
                ab = it % 2
                msl = slice(mb * MB, (mb + 1) * MB)
                for oc in range(8):
                    by = (5, 6, 0)[oc % 3]
                    T.group("pe", [lambda e, fc=fc, oc=oc, by=by: e.matmul(
                        PSB[by][:, :], wdn[wb][:, fc, oc * 128:(oc + 1) * 128], a_sb[ab][:, fc, :],
                        start=(fc == 0), stop=(fc == 1)) for fc in range(2)],
                        reads=[Rwdn[wb], Ra_sb[ab][0], Ra_sb[ab][1]], writes=[RPS[by]], cost=0.45)
                    xr = [XTr[mb * 2][oc], XTr[mb * 2 + 1][oc]]
                    if oc not in (1, 5):
                        T.op("dve", lambda e, oc=oc, by=by: e.scalar_tensor_tensor(
                            out=XT[:, oc, msl], in0=PSB[by][:, :], scalar=dcol(l, g2_off, oc),
                            in1=XT[:, oc, msl], op0=ALU.mult, op1=ALU.add),
                            reads=[RPS[by], Rdrv] + xr, writes=xr, cost=0.8)
                    else:
                        yi = (oc // 2) % 2
                        T.op("act", lambda e, oc=oc, by=by, yi=yi: e.activation(
                            out=ysb[yi][:], in_=PSB[by][:, :], func=AF.Copy, scale=dcol(l, g2_off, oc)),
                            reads=[RPS[by], Rdrv], writes=[Rysb[yi]], cost=0.7)
                        T.op("pool", lambda e, oc=oc, yi=yi: e.tensor_tensor(
                            out=XT[:, oc, msl], in0=XT[:, oc, msl], in1=ysb[yi][:], op=ALU.add),
                            reads=[Rysb[yi]] + xr, writes=xr, cost=1.3)

            load_w(0)
            load_w(1)
            emit_gu(0)
            for it in range(len(iters)):
                nxt = T.capture(lambda: emit_gu(it + 1)) if it + 1 < len(iters) else []
                T.play(nxt, T.capture(lambda: emit_down(it)))
                ex, mb = iters[it]
                if mb == nmb - 1 and ex + 2 < NE:
                    load_w(ex + 2)
            T.barrier()
        return None

    r = moe_phase(0, EXT)
    if stage == "comb0":
        return _finish_comb(P, T, r[1], dbg_d, out_d, XT, XTr)
    if stage == "x2":
        return _finish(P, T, XT, XTr, dbg_d, out_d, debug=True)

    pidx = _pool_mats_cached(0)[1]
    plan = _pool_plan()
    with ExitStack() as es3:
        u_tm = P.sb("u_tm", [128, EXT // 128, D], BF16, es3)
        Ru = [Res(f"u{t}") for t in range(EXT // 128)]
        pwi = P.sb("pwi", [128, 8, D], BF16, es3)
        pwo = P.sb("pwo", [128, 8, D], BF16, es3)
        pwg = P.sb("pwg", [128, 4, 2, 256], BF16, es3)
        pm = P.sb("pm", [128, n_pmat, 128], BF16, es3)
        Rpw = Res("pw")
        hb = P.sb("hb", [128, 8, TB], BF16, es3)
        Rhb = Res("hb")
        sq = P.sb("psq", [128, 8, TB], BF16, es3)
        rstd = P.sb("prstd", [128, TB], F32, es3)
        t32 = P.sb("pt32", [128, TB], F32, es3)
        T32ALT[id(t32)] = (P.sb("pt32b", [128, TB], F32, es3), Res("pt32b"))
        mtmp = (sq, Res("psq"), rstd, Res("prstd"), t32, Res("pt32"), PSB[0], RPS[0])
        dT = P.sb("dT", [128, 8, TB], BF16, es3)
        RdT = Res("dT")
        yin = P.sb("yin", [128, 8, TB], BF16, es3)
        Ryin = Res("yin")
        T.dma("pool", pwi[:], pw_in_d[:, :, :], writes=[Rpw])
        T.dma("pool", pwo[:], pw_out_d[:, :, :], writes=[Rpw])
        T.dma("pool", pwg[:], pw_grp_d[:, :, :, :], writes=[Rpw])
        T.dma("pool", pm[:], pmat_d.rearrange("m s t -> s m t"), writes=[Rpw])
        def pl_u(j):
            sl = slice(j * TB, (j + 1) * TB)
            modulate(es3, lambda dc, sl=sl: XT[:, dc, sl], XTr[j], TB, 0, 8, 1,
                     lambda dc: hb[:, dc, :], Rhb, tmp=mtmp)
            for ti in range(2):
                tile_i = j * 2 + ti
                for hf in range(2):
                    pb = 1 + (ti * 2 + hf) % 4
                    T.group("pe", [lambda e, dc=dc, ti=ti, hf=hf, pb=pb: e.matmul(
                        PSB[pb][:, :], hb[:, dc, ti * 128:(ti + 1) * 128], pwi[:, dc, hf * 512:(hf + 1) * 512],
                        start=(dc == 0), stop=(dc == 7)) for dc in range(8)], reads=[Rhb, Rpw], writes=[RPS[pb]])
                    eng = "act" if hf == 0 else "dve"
                    if eng == "act":
                        T.op("act", lambda e, tile_i=tile_i, hf=hf, pb=pb: e.activation(
                            out=u_tm[:, tile_i, hf * 512:(hf + 1) * 512], in_=PSB[pb][:, :], func=AF.Copy),
                            reads=[RPS[pb]], writes=[Ru[tile_i]])
                    else:
                        T.op("dve", lambda e, tile_i=tile_i, hf=hf, pb=pb: e.tensor_copy(
                            out=u_tm[:, tile_i, hf * 512:(hf + 1) * 512], in_=PSB[pb][:, :]),
                            reads=[RPS[pb]], writes=[Ru[tile_i]])
        def pl_mix(j):
            sl = slice(j * TB, (j + 1) * TB)
            for ti in range(2):
                tile_i = j * 2 + ti
                for wi in range(4):
                    dmin, dmax, nsp = plan[wi]
                    ds = [d for d in range(dmin, dmax + 1) if (wi, tile_i, d) in pidx]
                    pb = 1 + wi % 2
                    dps = PSB[pb][:, 0:256].rearrange("p (f t) -> p f t", t=128)
                    fns = []
                    srcs = []
                    for fc in range(2):
                        for k, d in enumerate(ds):
                            mi = pidx[(wi, tile_i, d)]
                            fns.append(lambda e, fc=fc, d=d, mi=mi, k=k, wi=wi, tile_i=tile_i, dps=dps, nds=len(ds): e.matmul(
                                dps[:, fc, :], u_tm[:, tile_i + d, wi * 256 + fc * 128:wi * 256 + (fc + 1) * 128],
                                pm[:, mi, :], start=(k == 0), stop=(k == nds - 1)))
                    for d in ds:
                        srcs.append(Ru[tile_i + d])
                    T.group("pe", fns, reads=srcs + [Rpw], writes=[RPS[pb]])
                    T.op("act", lambda e, wi=wi, ti=ti, dps=dps: e.activation(
                        out=dT[:, wi * 2:wi * 2 + 2, ti * 128:(ti + 1) * 128], in_=dps, func=AF.Copy),
                        reads=[RPS[pb]], writes=[RdT])
            for wi in range(4):
                for fo in range(2):
                    pb = 3 + (wi * 2 + fo) % 2
                    T.group("pe", [lambda e, fc=fc, wi=wi, fo=fo, pb=pb: e.matmul(
                        PSB[pb][:, 0:TB], pwg[:, wi, fc, fo * 128:(fo + 1) * 128], dT[:, wi * 2 + fc, :],
                        start=(fc == 0), stop=(fc == 1)) for fc in range(2)], reads=[RdT, Rpw], writes=[RPS[pb]])
                    ci = wi * 2 + fo
                    T.op("act", lambda e, ci=ci, pb=pb: e.activation(
                        out=yin[:, ci, :], in_=PSB[pb][:, 0:TB], func=AF.Copy,
                        scale=vec[:, VEC["pscale"] + ci:VEC["pscale"] + ci + 1]),
                        reads=[RPS[pb], Rvec], writes=[Ryin])
            for oc in range(8):
                pb = 5 + oc % 2
                T.group("pe", [lambda e, fcx=fcx, oc=oc, pb=pb: e.matmul(
                    PSB[pb][:, 0:TB], pwo[:, fcx, oc * 128:(oc + 1) * 128], yin[:, fcx, :],
                    start=(fcx == 0), stop=(fcx == 7)) for fcx in range(8)], reads=[Ryin, Rpw], writes=[RPS[pb]])
                T.op("dve", lambda e, oc=oc, pb=pb, sl=sl: e.scalar_tensor_tensor(
                    out=XT[:, oc, sl], in0=PSB[pb][:, 0:TB], scalar=dcol(1, 16, oc), in1=XT[:, oc, sl],
                    op0=ALU.mult, op1=ALU.add), reads=[RPS[pb], XTr[j][oc], Rdrv], writes=[XTr[j][oc]])
        for j in range(NB_EXT):
            pl_u(j)
        for j in range(NB_OWN):
            pl_mix(j)
        T.barrier()
    if stage == "x3":
        return _finish(P, T, XT, XTr, dbg_d, out_d, debug=True)

    moe_phase(1, OWN)
    if stage == "x4":
        return _finish(P, T, XT, XTr, dbg_d, out_d, debug=True)

    out_evs = []
    with ExitStack() as es4:
        sq2 = [P.sb(f"fsq{i}", [128, 8, TB], BF16, es4) for i in range(2)]
        Rsq2 = [Res("fsq0"), Res("fsq1")]
        rstd2 = [P.sb(f"frstd{i}", [128, TB], F32, es4) for i in range(2)]
        Rrstd2 = [Res("frstd0"), Res("frstd1")]
        out_v = out_d.rearrange("(dc p) t -> p dc t", p=128)

        def fin_block(j):
            sl = slice(j * TB, (j + 1) * TB)
            sq, Rsq, rstd, Rrstd = sq2[j % 2], Rsq2[j % 2], rstd2[j % 2], Rrstd2[j % 2]
            for dc in range(8):
                if dc % 2 == 0:
                    T.op("act", lambda e, dc=dc: e.activation(out=sq[:, dc, :], in_=XT[:, dc, sl], func=AF.Square),
                         reads=[XTr[j][dc]], writes=[Rsq])
                else:
                    T.op("pool", lambda e, dc=dc: e.tensor_tensor(out=sq[:, dc, :], in0=XT[:, dc, sl], in1=XT[:, dc, sl],
                                                                  op=ALU.mult), reads=[XTr[j][dc]], writes=[Rsq])
            pb = 1 + j % 2
            T.group("pe", [lambda e, dc=dc: e.matmul(PSB[pb][:, 0:TB], ones[:], sq[:, dc, :],
                                                      start=(dc == 0), stop=(dc == 7)) for dc in range(8)],
                    reads=[Rsq, Rid], writes=[RPS[pb]])
            T.op("act", lambda e: e.activation(out=rstd[:], in_=PSB[pb][:, 0:TB], func=AF.Ln, scale=1.0 / D, bias=EPS),
                 reads=[RPS[pb]], writes=[Rrstd])
            T.op("act", lambda e: e.activation(out=rstd[:], in_=rstd[:], func=AF.Exp, scale=-0.5),
                 reads=[Rrstd], writes=[Rrstd])
            for dc in range(8):
                T.op("dve", lambda e, dc=dc: e.scalar_tensor_tensor(
                    out=XT[:, dc, sl], in0=XT[:, dc, sl], scalar=vec[:, VEC["fing"] + dc:VEC["fing"] + dc + 1],
                    in1=rstd[:], op0=ALU.mult, op1=ALU.mult), reads=[XTr[j][dc], Rrstd, Rvec], writes=[XTr[j][dc]])

        for j in range(0, NB_OWN, 2):
            T.play(T.capture(lambda: fin_block(j)), T.capture(lambda: fin_block(j + 1)))
            if not stage:
                for jj in (j, j + 1):
                    sl = slice(jj * TB, (jj + 1) * TB)
                    out_evs.append(T.dma("sp", out_v[:, :, sl], XT[:, :, sl], reads=XTr[jj]))
        T.barrier()
    if not stage:
        for ev in out_evs:
            T.E["sp"].wait(ev)
        print(f"[kernel] ops={T.nops} waits={T.nwaits}", flush=True)
        return
    return _finish(P, T, XT, XTr, dbg_d, out_d, debug=bool(stage))


def _finish_comb(P, T, combT, dbg_d, out_d, XT, XTr):
    T.barrier()
    ev = T.dma("sp", dbg_d[0:16, :], combT[:, :])
    T.E["sp"].wait(ev)
    _finish(P, T, XT, XTr, dbg_d, out_d, debug=False)


def _finish(P, T, XT, XTr, dbg_d, out_d, debug):
    evs = []
    if debug:
        for dc in range(8):
            evs.append(T.dma("sp", dbg_d[dc * 128:(dc + 1) * 128, :], XT[:, dc, :], reads=[r[dc] for r in XTr]))
    for dc in range(8):
        evs.append(T.dma("sp", out_d[dc * 128:(dc + 1) * 128, :], XT[:, dc, 0:OWN], reads=[r[dc] for r in XTr]))
    for ev in evs:
        T.E["sp"].wait(ev)
    print(f"[kernel] ops={T.nops} waits={T.nwaits}", flush=True)


def _prepare(inputs):
    shared = _shared_weights(inputs)
    w_in = np.asarray(inputs["hg_w_in"][0], np.float32)
    w_in_by_half = [_w_in_tiled(w_in, 0), _w_in_tiled(w_in, 1)]
    return shared, w_in_by_half


def kernel(**inputs):
    inputs = {k: np.asarray(v) for k, v in inputs.items()}
    shared, w_in_by_half = _prepare(inputs)
    n_pmat = _pool_mats_cached(0)[0].shape[0]
    nc = build_program(stage=_DBG["stage"], n_pmat=n_pmat)
    in_maps = [_core_inputs(inputs, shared, w_in_by_half, c) for c in range(8)]
    res = run_bass_kernel_spmd(nc, in_maps, core_ids=list(range(8)))
    out = np.zeros((4, L, D), np.float32)
    for c in range(8):
        b, half = c // 2, c % 2
        o = res.results[c]["outT"].T
        if half == 0:
            out[b, 0:OWN] = o
        else:
            out[b, OWN:] = o[::-1]
    return out
```
